# Optimizing a Trainium2 kernel written in Bass

```python
import math
import jax
import jax.numpy as jnp
from jax import lax
import numpy as np

D_MODEL = 1024
BATCH = 8
SEQ = 4096
DEPTH = 2

CTX_LEN = 256
GRID_W = 64
HEAD_DIM = 64
H_RET = 4
H_GDN = 4
H_NA = 8
D_RET = H_RET * HEAD_DIM
D_GDN = H_GDN * HEAD_DIM
D_NA = H_NA * HEAD_DIM
D_MIX = D_RET + D_GDN + D_NA
RET_COLS = 4 * D_RET
GDN_COLS = 4 * D_GDN + 4 * H_GDN
NA_COLS = 3 * D_NA
D_IN = RET_COLS + GDN_COLS + NA_COLS
RET_CHUNK = 128
GDN_CHUNK = 64
SHORT_CONV = 5
NA_KH = 8
NA_KW = 16
NA_QBLK = 16
NA_KBLK = 32
N_COLBLK = GRID_W // NA_QBLK
ROPE_BASE = 10000.0
N_FREQ = HEAD_DIM // 4
D_FF = 2816
N_EXPERTS = 8
TOP_K = 2
MOE_BLOCK = 128
EPS = 1e-6
NEG_INF = -1e30

kernel_name = 'hybrid_dit_retention_gdn_natten_moe'


def rms_norm(x, g):
    xf = x.astype(jnp.float32)
    y = xf * lax.rsqrt(jnp.mean(xf * xf, axis=-1, keepdims=True) + EPS)
    return (y * g.astype(jnp.float32)).astype(x.dtype)


def modulate(h, shift, scale):
    return h * (1 + scale) + shift


def head_rms(o):
    return o * lax.rsqrt(jnp.mean(o * o, axis=-1, keepdims=True) + EPS)


def l2_normalise(t):
    return t * lax.rsqrt(jnp.sum(t * t, axis=-1, keepdims=True) + EPS)


def split_heads(t, n_heads):
    return t.reshape(t.shape[:-1] + (n_heads, HEAD_DIM))


def merge_heads(t):
    return t.reshape(t.shape[:-2] + (-1,))


def axial_rope_tables(n_tok):
    t = jnp.arange(n_tok, dtype=jnp.int32)
    row = (t // GRID_W).astype(jnp.float32)
    col = (t % GRID_W).astype(jnp.float32)
    inv_freq = ROPE_BASE ** (-jnp.arange(N_FREQ, dtype=jnp.float32) / N_FREQ)
    ang = jnp.concatenate([row[:, None] * inv_freq, col[:, None] * inv_freq], axis=-1)
    return jnp.cos(ang)[:, None, :], jnp.sin(ang)[:, None, :]


def apply_rope(t, cos, sin):
    t1, t2 = jnp.split(t, 2, axis=-1)
    return jnp.concatenate([t1 * cos - t2 * sin, t1 * sin + t2 * cos], axis=-1)


def centred_depthwise_conv(t, w):
    k_w, ch = w.shape
    return lax.conv_general_dilated(t, w[:, None, :].astype(t.dtype), window_strides=(1,),
                                    padding=[(k_w // 2, k_w // 2)],
                                    dimension_numbers=('NWC', 'WIO', 'NWC'), feature_group_count=ch)


def to_chunks(t, chunk):
    b, l, h = t.shape[:3]
    t = t.reshape((b, l // chunk, chunk, h) + t.shape[3:])
    return jnp.moveaxis(t, (1, 3), (0, 2))


def from_chunks(t):
    t = jnp.moveaxis(t, (0, 2), (1, 3))
    return t.reshape((t.shape[0], t.shape[1] * t.shape[2]) + t.shape[3:])


def retention_scan(q, k, v, log_gamma, s0):
    pos = jnp.arange(RET_CHUNK, dtype=jnp.float32)
    diff = pos[:, None] - pos[None, :]
    intra_decay = jnp.where(diff >= 0, jnp.exp(log_gamma[:, None, None] * jnp.maximum(diff, 0.0)), 0.0)
    q_decay = jnp.exp(log_gamma[:, None] * (pos + 1.0))[..., None]
    k_decay = jnp.exp(log_gamma[:, None] * (RET_CHUNK - 1.0 - pos))[..., None]
    chunk_decay = jnp.exp(log_gamma * RET_CHUNK)[:, None, None]

    def step(s, inp):
        qi, ki, vi = inp
        scores = jnp.einsum('bhid,bhjd->bhij', qi, ki) * intra_decay
        o = jnp.einsum('bhij,bhje->bhie', scores, vi) + jnp.einsum('bhid,bhde->bhie', qi * q_decay, s)
        s = s * chunk_decay + jnp.einsum('bhjd,bhje->bhde', ki * k_decay, vi)
        return s, o

    s_fin, o = lax.scan(step, s0, (to_chunks(q, RET_CHUNK), to_chunks(k, RET_CHUNK), to_chunks(v, RET_CHUNK)))
    return from_chunks(o), s_fin


def gated_delta_scan(q, k, v, g, beta, s0):
    c = GDN_CHUNK
    pos = jnp.arange(c)
    incl = pos[:, None] >= pos[None, :]
    strict = pos[:, None] > pos[None, :]

    def step(s, inp):
        qi, ki, vi, gi, bi = inp
        diff = gi[..., :, None] - gi[..., None, :]
        decay = jnp.where(incl, jnp.exp(jnp.where(incl, diff, 0.0)), 0.0)
        kb = ki * bi[..., None]
        a = jnp.where(strict, jnp.einsum('bhid,bhjd->bhij', kb, ki) * decay, 0.0)
        rhs = jnp.concatenate([vi * bi[..., None], kb * jnp.exp(gi)[..., None]], axis=-1)
        sol = lax.linalg.triangular_solve(a, rhs, left_side=True, lower=True, unit_diagonal=True)
        u, w = jnp.split(sol, 2, axis=-1)
        v_new = u - jnp.einsum('bhcd,bhde->bhce', w, s)
        attn = jnp.einsum('bhid,bhjd->bhij', qi, ki) * decay
        o = (jnp.einsum('bhid,bhde->bhie', qi * jnp.exp(gi)[..., None], s)
             + jnp.einsum('bhij,bhje->bhie', attn, v_new))
        g_last = gi[..., -1:]
        s = (s * jnp.exp(g_last)[..., None]
             + jnp.einsum('bhjd,bhje->bhde', ki * jnp.exp(g_last - gi)[..., None], v_new))
        return s, o

    g_cum = jnp.cumsum(to_chunks(g, c), axis=-1)
    s_fin, o = lax.scan(step, s0, (to_chunks(q, c), to_chunks(k, c), to_chunks(v, c), g_cum, to_chunks(beta, c)))
    return from_chunks(o), s_fin


def bidirectional(scan_fw, scan_bw, lat, ctx, s0):
    rev = lambda ts: tuple(jnp.flip(t, axis=1) for t in ts)
    oc_f, sc_f = scan_fw(ctx, s0)
    oc_b, sc_b = scan_bw(rev(ctx), s0)
    ol_f, _ = scan_fw(lat, sc_f)
    ol_b, _ = scan_bw(rev(lat), sc_b)
    return ol_f + jnp.flip(ol_b, axis=1), oc_f + jnp.flip(oc_b, axis=1)


def retention_group(pl, pc, decay_param, cos, sin, need_ctx):
    log_gamma = jnp.log1p(-jnp.exp2(-decay_param.astype(jnp.float32)))

    def prep(p, rotate):
        q, k, v, gate = [split_heads(t.astype(jnp.float32), H_RET) for t in jnp.split(p, 4, axis=-1)]
        if rotate:
            q, k = apply_rope(q, cos, sin), apply_rope(k, cos, sin)
        return (q, k * HEAD_DIM ** -0.5, v), gate

    lat, gate_l = prep(pl, True)
    ctx, gate_c = prep(pc, False)
    s0 = jnp.zeros((pl.shape[0], H_RET, HEAD_DIM, HEAD_DIM), jnp.float32)
    o_l, o_c = bidirectional(lambda seq, s: retention_scan(*seq, log_gamma[0], s),
                             lambda seq, s: retention_scan(*seq, log_gamma[1], s), lat, ctx, s0)
    finish = lambda o, gate: merge_heads(head_rms(o) * jax.nn.silu(gate))
    out_c = finish(o_c, gate_c).astype(pc.dtype) if need_ctx else None
    return finish(o_l, gate_l).astype(pl.dtype), out_c


def gdn_group(pl, pc, conv_w, a_log, dt_bias, norm_g, need_ctx):
    a_log = a_log.astype(jnp.float32)
    dt_bias = dt_bias.astype(jnp.float32)

    def prep(p):
        p = p.astype(jnp.float32)
        qkv = jax.nn.silu(centred_depthwise_conv(p[..., :3 * D_GDN], conv_w))
        q, k, v = [split_heads(t, H_GDN) for t in jnp.split(qkv, 3, axis=-1)]
        gate = split_heads(p[..., 3 * D_GDN:4 * D_GDN], H_GDN)
        ab = p[..., 4 * D_GDN:].reshape(p.shape[:2] + (2, 2, H_GDN))
        g = -jnp.exp(a_log) * jax.nn.softplus(ab[:, :, 0] + dt_bias)
        beta = jax.nn.sigmoid(ab[:, :, 1])
        return (l2_normalise(q) * HEAD_DIM ** -0.5, l2_normalise(k), v, g, beta), gate

    lat, gate_l = prep(pl)
    ctx, gate_c = prep(pc)
    s0 = jnp.zeros((pl.shape[0], H_GDN, HEAD_DIM, HEAD_DIM), jnp.float32)

    def scan_dir(dr):
        return lambda seq, s: gated_delta_scan(seq[0], seq[1], seq[2], seq[3][:, :, dr], seq[4][:, :, dr], s)

    o_l, o_c = bidirectional(scan_dir(0), scan_dir(1), lat, ctx, s0)
    finish = lambda o, gate: merge_heads(head_rms(o) * norm_g.astype(jnp.float32) * jax.nn.silu(gate))
    out_c = finish(o_c, gate_c).astype(pc.dtype) if need_ctx else None
    return finish(o_l, gate_l).astype(pl.dtype), out_c


def neighbourhood_attention(ql, kl, vl, kc, vc, rpb):
    b, l, h, d = ql.shape
    rows = l // GRID_W
    kh = min(NA_KH, rows)
    scale = d ** -0.5
    qg = ql.reshape(b, rows, GRID_W, h, d)
    kg = kl.reshape(b, rows, GRID_W, h, d)
    vg = vl.reshape(b, rows, GRID_W, h, d)
    kcf = kc.astype(jnp.float32)
    vcf = vc.astype(jnp.float32)
    qcol = jnp.arange(GRID_W, dtype=jnp.int32).reshape(N_COLBLK, NA_QBLK)
    win_start = jnp.clip(qcol - NA_KW // 2, 0, GRID_W - NA_KW)
    band_start = jnp.clip(qcol[:, 0] - NA_KW // 2, 0, GRID_W - NA_KBLK)
    band_cols = band_start[:, None] + jnp.arange(NA_KBLK, dtype=jnp.int32)
    kcol = band_cols[:, None, :]
    col_ok = (kcol >= win_start[..., None]) & (kcol < win_start[..., None] + NA_KW)
    col_idx = jnp.clip(kcol - qcol[..., None] + NA_KW - 1, 0, 2 * NA_KW - 2)
    rpb_c = rpb.astype(jnp.float32)[:, :, col_idx]

    def one_row(r):
        r0 = jnp.clip(r - kh // 2, 0, rows - kh)
        qb = lax.dynamic_index_in_dim(qg, r, axis=1, keepdims=False)
        qb = qb.reshape(b, N_COLBLK, NA_QBLK, h, d).astype(jnp.float32)
        kr = lax.dynamic_slice_in_dim(kg, r0, kh, axis=1)[:, :, band_cols].astype(jnp.float32)
        vr = lax.dynamic_slice_in_dim(vg, r0, kh, axis=1)[:, :, band_cols].astype(jnp.float32)
        s_loc = jnp.einsum('bnqhd,binkhd->bhnqik', qb, kr) * scale
        row_idx = r0 + jnp.arange(kh, dtype=jnp.int32) - r + NA_KH - 1
        bias = jnp.transpose(rpb_c[:, row_idx], (0, 2, 3, 1, 4))
        s_loc = jnp.where(col_ok[:, :, None, :], s_loc + bias, NEG_INF)
        s_ctx = jnp.einsum('bnqhd,bchd->bhnqc', qb, kcf) * scale
        s = jnp.concatenate([s_loc.reshape(b, h, N_COLBLK, NA_QBLK, kh * NA_KBLK), s_ctx], axis=-1)
        p = jax.nn.softmax(s, axis=-1)
        p_loc = p[..., :kh * NA_KBLK].reshape(b, h, N_COLBLK, NA_QBLK, kh, NA_KBLK)
        p_ctx = p[..., kh * NA_KBLK:]
        o = jnp.einsum('bhnqik,binkhd->bnqhd', p_loc, vr) + jnp.einsum('bhnqc,bchd->bnqhd', p_ctx, vcf)
        return o.reshape(b, GRID_W, h, d)

    out = lax.map(one_row, jnp.arange(rows, dtype=jnp.int32))
    return jnp.transpose(out, (1, 0, 2, 3, 4)).reshape(b, l, h, d)


def context_attention(q, k, v):
    s = jnp.einsum('bqhd,bkhd->bhqk', q.astype(jnp.float32), k.astype(jnp.float32)) * HEAD_DIM ** -0.5
    p = jax.nn.softmax(s, axis=-1)
    return jnp.einsum('bhqk,bkhd->bqhd', p, v.astype(jnp.float32))


def na_group(pl, pc, rpb, need_ctx):
    ql, kl, vl = [split_heads(t, H_NA) for t in jnp.split(pl, 3, axis=-1)]
    qc, kc, vc = [split_heads(t, H_NA) for t in jnp.split(pc, 3, axis=-1)]
    out_l = merge_heads(neighbourhood_attention(ql, kl, vl, kc, vc, rpb)).astype(pl.dtype)
    out_c = merge_heads(context_attention(qc, kc, vc)).astype(pc.dtype) if need_ctx else None
    return out_l, out_c


def token_mixer(hl, hc, w_in, w_out, conv_w, ret_decay, gdn_a_log, gdn_dt_bias, gdn_norm_g, na_rpb,
                cos, sin, need_ctx):
    pl = hl @ w_in
    pc = hc @ w_in
    c1, c2 = RET_COLS, RET_COLS + GDN_COLS
    r_l, r_c = retention_group(pl[..., :c1], pc[..., :c1], ret_decay, cos, sin, need_ctx)
    g_l, g_c = gdn_group(pl[..., c1:c2], pc[..., c1:c2], conv_w, gdn_a_log, gdn_dt_bias, gdn_norm_g, need_ctx)
    n_l, n_c = na_group(pl[..., c2:], pc[..., c2:], na_rpb, need_ctx)
    yl = jnp.concatenate([r_l, g_l, n_l], axis=-1) @ w_out
    yc = jnp.concatenate([r_c, g_c, n_c], axis=-1) @ w_out if need_ctx else None
    return yl, yc


def swiglu(h, w1, w3, w2):
    return (jax.nn.silu(h @ w1) * (h @ w3)) @ w2


def moe_swiglu(h, w_router, w1, w3, w2):
    b, l, d = h.shape
    n_tok = b * l
    hf = h.reshape(n_tok, d)
    logits = jnp.dot(hf, w_router).astype(jnp.float32)
    top_logit, top_e = lax.top_k(logits, TOP_K)
    gates = jax.nn.softmax(top_logit, axis=-1).astype(h.dtype)
    e_flat = top_e.reshape(-1).astype(jnp.int32)
    tok_flat = jnp.repeat(jnp.arange(n_tok, dtype=jnp.int32), TOP_K)
    w_flat = gates.reshape(-1)
    order = jnp.argsort(e_flat)
    e_sorted, tok_sorted, w_sorted = e_flat[order], tok_flat[order], w_flat[order]
    counts = jax.ops.segment_sum(jnp.ones_like(e_flat), e_flat, num_segments=N_EXPERTS)
    starts = jnp.cumsum(counts) - counts
    padded = (counts + MOE_BLOCK - 1) // MOE_BLOCK * MOE_BLOCK
    pad_ends = jnp.cumsum(padded)
    pad_starts = pad_ends - padded
    dest = pad_starts[e_sorted] + (jnp.arange(n_tok * TOP_K, dtype=jnp.int32) - starts[e_sorted])
    n_blocks = (n_tok * TOP_K + MOE_BLOCK - 1) // MOE_BLOCK + N_EXPERTS
    n_rows = n_blocks * MOE_BLOCK
    buf_tok = jnp.zeros((n_rows,), jnp.int32).at[dest].set(tok_sorted)
    buf_w = jnp.zeros((n_rows,), h.dtype).at[dest].set(w_sorted)
    block_start = jnp.arange(n_blocks, dtype=jnp.int32) * MOE_BLOCK
    block_e = jnp.minimum(jnp.searchsorted(pad_ends, block_start, side='right'), N_EXPERTS - 1)

    def expert_block(args):
        idx, e, wt = args
        return swiglu(hf[idx], w1[e], w3[e], w2[e]) * wt[:, None]

    y = lax.map(expert_block, (buf_tok.reshape(n_blocks, MOE_BLOCK), block_e, buf_w.reshape(n_blocks, MOE_BLOCK)))
    out = jnp.zeros_like(hf).at[buf_tok].add(y.reshape(n_rows, d))
    return out.reshape(b, l, d)


def setup_inputs(seed: int = 0) -> dict:
    key = jax.random.key(seed)
    ks = jax.random.split(key, 24)
    f32 = jnp.float32
    n_dense = (DEPTH + 1) // 2
    n_moe = DEPTH // 2

    def nrm(k, shape, scale):
        return jax.random.normal(k, shape, f32) * scale

    dt = jnp.exp(jax.random.uniform(ks[13], (DEPTH, 2, H_GDN), f32, math.log(1e-3), math.log(1e-1)))
    return {
        'x': nrm(ks[0], (BATCH, SEQ, D_MODEL), 1.0),
        'c': nrm(ks[1], (BATCH, D_MODEL), 1.0),
        'ctx': nrm(ks[2], (BATCH, CTX_LEN, D_MODEL), 1.0),
        'c_ctx': nrm(ks[3], (D_MODEL,), 1.0),
        'ada_w': nrm(ks[4], (DEPTH, D_MODEL, 6 * D_MODEL), 0.5 * D_MODEL ** -0.5),
        'ada_b': nrm(ks[5], (DEPTH, 6 * D_MODEL), 0.02),
        'norm1_g': 1.0 + nrm(ks[6], (DEPTH, D_MODEL), 0.05),
        'norm2_g': 1.0 + nrm(ks[7], (DEPTH, D_MODEL), 0.05),
        'w_in': nrm(ks[8], (DEPTH, D_MODEL, D_IN), D_MODEL ** -0.5),
        'w_out': nrm(ks[9], (DEPTH, D_MIX, D_MODEL), D_MIX ** -0.5),
        'conv_w': nrm(ks[10], (DEPTH, SHORT_CONV, 3 * D_GDN), SHORT_CONV ** -0.5),
        'ret_decay': 5.0 + jnp.arange(H_RET, dtype=f32) + nrm(ks[11], (DEPTH, 2, H_RET), 0.1),
        'gdn_a_log': jnp.log(jax.random.uniform(ks[12], (DEPTH, 2, H_GDN), f32, 1.0, 16.0)),
        'gdn_dt_bias': dt + jnp.log(-jnp.expm1(-dt)),
        'gdn_norm_g': 1.0 + nrm(ks[14], (DEPTH, HEAD_DIM), 0.05),
        'na_rpb': nrm(ks[15], (DEPTH, H_NA, 2 * NA_KH - 1, 2 * NA_KW - 1), 0.1),
        'ffn_w1': nrm(ks[16], (n_dense, D_MODEL, D_FF), D_MODEL ** -0.5),
        'ffn_w3': nrm(ks[17], (n_dense, D_MODEL, D_FF), D_MODEL ** -0.5),
        'ffn_w2': nrm(ks[18], (n_dense, D_FF, D_MODEL), D_FF ** -0.5),
        'moe_router': nrm(ks[19], (n_moe, D_MODEL, N_EXPERTS), D_MODEL ** -0.5),
        'moe_w1': nrm(ks[20], (n_moe, N_EXPERTS, D_MODEL, D_FF), D_MODEL ** -0.5),
        'moe_w3': nrm(ks[21], (n_moe, N_EXPERTS, D_MODEL, D_FF), D_MODEL ** -0.5),
        'moe_w2': nrm(ks[22], (n_moe, N_EXPERTS, D_FF, D_MODEL), D_FF ** -0.5),
        'final_g': 1.0 + nrm(ks[23], (D_MODEL,), 0.05),
    }


def reference(x, c, ctx, c_ctx, ada_w, ada_b, norm1_g, norm2_g, w_in, w_out, conv_w, ret_decay,
              gdn_a_log, gdn_dt_bias, gdn_norm_g, na_rpb, ffn_w1, ffn_w3, ffn_w2,
              moe_router, moe_w1, moe_w3, moe_w2, final_g):
    cos, sin = axial_rope_tables(x.shape[1])
    y = ctx
    sc = jax.nn.silu(c)
    scc = jax.nn.silu(c_ctx)
    for l in range(DEPTH):
        need_ctx = l < DEPTH - 1
        ml = [m[:, None, :] for m in jnp.split(sc @ ada_w[l] + ada_b[l], 6, axis=-1)]
        mc = jnp.split(scc @ ada_w[l] + ada_b[l], 6, axis=-1)
        hl = modulate(rms_norm(x, norm1_g[l]), ml[0], ml[1])
        hc = modulate(rms_norm(y, norm1_g[l]), mc[0], mc[1])
        ol, oc = token_mixer(hl, hc, w_in[l], w_out[l], conv_w[l], ret_decay[l], gdn_a_log[l], gdn_dt_bias[l],
                             gdn_norm_g[l], na_rpb[l], cos, sin, need_ctx)
        x = x + ml[2] * ol
        if need_ctx:
            y = y + mc[2] * oc
        j = l // 2
        if l % 2 == 0:
            ffn = lambda h: swiglu(h, ffn_w1[j], ffn_w3[j], ffn_w2[j])
        else:
            ffn = lambda h: moe_swiglu(h, moe_router[j], moe_w1[j], moe_w3[j], moe_w2[j])
        x = x + ml[5] * ffn(modulate(rms_norm(x, norm2_g[l]), ml[3], ml[4]))
        if need_ctx:
            y = y + mc[5] * ffn(modulate(rms_norm(y, norm2_g[l]), mc[3], mc[4]))
    return rms_norm(x, final_g)
```

```python
import os
import numpy as np
import ml_dtypes
import concourse.bass as bass
import concourse.mybir as mybir
from contextlib import ExitStack
from concourse.bass_utils import run_bass_kernel_spmd

F32 = mybir.dt.float32
BF16 = mybir.dt.bfloat16
F32R = mybir.dt.float32r
ALU = mybir.AluOpType
AF = mybir.ActivationFunctionType
AX = mybir.AxisListType

EPOCH = 16000
N_DMA_SEMS = 40

D = 1024
SEQ = 4096
CTX = 256
NTOK = SEQ + CTX
NT = NTOK // 128
DEPTH = 2
D_IN = 3600
D_FF = 2816
NE = 8
EPS = 1e-6


class Buf:
    __slots__ = ("name", "last_w", "reads", "excl")

    def __init__(self, name="", excl=False):
        self.name = name
        self.last_w = None
        self.reads = []
        self.excl = excl


class Eng:
    def __init__(self, name):
        self.name = name
        self.ops = []
        self.n = 0
        self.sems = []
        self.waited = {}


class Ker:
    def __init__(self, nc, stack):
        self.nc = nc
        self.stack = stack
        self.eng = {n: Eng(n) for n in ("pe", "act", "dve", "pool", "sp")}
        self.dma_sems = []
        self.dma_sem_val = []
        self.dma_rr = 0
        for i in range(N_DMA_SEMS):
            s = stack.enter_context(nc.semaphore(f"dq{i}"))
            self.dma_sems.append(s)
            self.dma_sem_val.append(0)

    def _need(self, e, waits, tok):
        if tok is None:
            return
        sem, val, key = tok[0], tok[1], tok[2]
        if e.waited.get(key, 0) >= val:
            return
        e.waited[key] = val
        waits.append((sem, val))

    def _deps(self, e, reads, writes, pe_acc=False):
        waits = []
        for b in reads:
            self._need(e, waits, b.last_w)
            if b.excl:
                for t in b.reads:
                    if t[3] != e.name:
                        self._need(e, waits, t)
        for b in writes:
            if not (pe_acc and b.last_w is not None and b.last_w[3] == "pe"):
                self._need(e, waits, b.last_w)
            for t in b.reads:
                self._need(e, waits, t)
        return waits

    def _commit(self, tok, reads, writes):
        for b in reads:
            b.reads.append(tok)
            if len(b.reads) > 12:
                d = {}
                for t in b.reads:
                    if t[2] not in d or d[t[2]][1] < t[1]:
                        d[t[2]] = t
                b.reads = list(d.values())
        for b in writes:
            b.last_w = tok
            b.reads = []

    def op(self, engname, fn, reads=(), writes=(), pe_acc=False):
        e = self.eng[engname]
        waits = self._deps(e, reads, writes, pe_acc)
        ep = e.n // EPOCH
        while len(e.sems) <= ep:
            e.sems.append(self.stack.enter_context(self.nc.semaphore(f"s_{engname}{len(e.sems)}")))
        sem = e.sems[ep]
        val = e.n % EPOCH + 1
        tok = (sem, val, (engname, ep), engname)
        e.n += 1
        e.ops.append((waits, fn, (sem, 1)))
        self._commit(tok, reads, writes)
        return tok

    def dma(self, qname, out_ap, in_ap, reads=(), writes=(), **kw):
        e = self.eng[qname]
        waits = self._deps(e, reads, writes)
        i = self.dma_rr
        self.dma_rr = (self.dma_rr + 1) % N_DMA_SEMS
        sem = self.dma_sems[i]
        if self.dma_sem_val[i] > 0:
            self._need(e, waits, (sem, self.dma_sem_val[i], ("dq", i), "dma"))
        self.dma_sem_val[i] += 16
        val = self.dma_sem_val[i]
        tok = (sem, val, ("dq", i), "dma")

        def fn(h, out_ap=out_ap, in_ap=in_ap, kw=kw):
            return h.dma_start(out=out_ap, in_=in_ap, **kw)
        e.ops.append((waits, fn, (sem, 16)))
        self._commit(tok, reads, writes)
        return tok

    def barrier(self):
        toks = []
        for n, e in self.eng.items():
            if e.n > 0:
                ep = (e.n - 1) // EPOCH
                toks.append((e.sems[ep], (e.n - 1) % EPOCH + 1, (n, ep), n))
        for i in range(N_DMA_SEMS):
            if self.dma_sem_val[i] > 0:
                toks.append((self.dma_sems[i], self.dma_sem_val[i], ("dq", i), "dma"))
        for n, e in self.eng.items():
            waits = []
            for t in toks:
                self._need(e, waits, t)
            e.ops.append((waits, None, None))

    def finish(self, final_tokens):
        e = self.eng["sp"]
        waits = []
        for t in final_tokens:
            self._need(e, waits, t)
        e.ops.append((waits, None, None))

    def emit(self):
        nc = self.nc
        with nc.Block() as block:
            def run(e):
                def body(h):
                    for waits, fn, inc in e.ops:
                        for (sem, val) in waits:
                            h.wait_ge(sem, val)
                        if fn is not None:
                            ins = fn(h)
                            if inc is not None:
                                ins.then_inc(inc[0], inc[1])
                return body
            block.tensor(run(self.eng["pe"]))
            block.scalar(run(self.eng["act"]))
            block.vector(run(self.eng["dve"]))
            block.gpsimd(run(self.eng["pool"]))
            block.sync(run(self.eng["sp"]))


_CUR = {"K": None}


class SS(ExitStack):
    def __exit__(self, *a):
        if _CUR["K"] is not None and a[0] is None:
            _CUR["K"].barrier()
        return super().__exit__(*a)


class Tile:
    def __init__(self, t, nslots=1):
        self.t = t
        self.b = [Buf() for _ in range(nslots)]

    def __getitem__(self, k):
        return self.t[k]


class Gen:
    def __init__(self, nc, stack):
        self.nc = nc
        self.st = stack
        self.K = Ker(nc, stack)
        self.psum = []
        self.ps_rr = 0
        for i in range(8):
            t = stack.enter_context(nc.psum_tensor(f"ps{i}", [128, 512], F32))
            tl = Tile(t)
            tl.b[0].excl = True
            self.psum.append(tl)

    def ps(self):
        p = self.psum[self.ps_rr]
        self.ps_rr = (self.ps_rr + 1) % 8
        return p

    def sb(self, name, shape, dt, nslots=1, stack=None):
        self.uid = getattr(self, "uid", 0) + 1
        t = (stack or self.st).enter_context(self.nc.sbuf_tensor(f"{name}_{self.uid}", shape, dt))
        return Tile(t, nslots)

    def mm(self, out, lhsT, rhs, start, stop, R, W):
        return self.K.op("pe", lambda h: h.matmul(out, lhsT, rhs, start=start, stop=stop), reads=R, writes=W,
                         pe_acc=not start)

    def tr(self, out, in_, ident, R, W):
        return self.K.op("pe", lambda h: h.transpose(out, in_, ident), reads=R, writes=W, pe_acc=True)

    def act(self, out, in_, func, R, W, bias=None, scale=1.0, accum_out=None, eng="act"):
        kw = {}
        if bias is not None:
            kw["bias"] = bias
        if accum_out is not None:
            kw["accum_out"] = accum_out
        return self.K.op(eng, lambda h: h.activation(out=out, in_=in_, func=func, scale=scale, **kw), reads=R, writes=W)

    def tt(self, out, in0, in1, op, R, W, eng="dve"):
        return self.K.op(eng, lambda h: h.tensor_tensor(out=out, in0=in0, in1=in1, op=op), reads=R, writes=W)

    def ts(self, out, in0, s1, op0, R, W, s2=None, op1=None, eng="dve", accum_out=None):
        kw = {}
        if op1 is not None:
            kw["op1"] = op1
        if accum_out is not None:
            kw["accum_out"] = accum_out
        return self.K.op(eng, lambda h: h.tensor_scalar(out=out, in0=in0, scalar1=s1, scalar2=s2, op0=op0, **kw),
                         reads=R, writes=W)

    def stt(self, out, in0, scalar, in1, op0, op1, R, W, eng="dve"):
        return self.K.op(eng, lambda h: h.scalar_tensor_tensor(out=out, in0=in0, scalar=scalar, in1=in1, op0=op0, op1=op1),
                         reads=R, writes=W)

    def cp(self, out, in_, R, W, eng="dve"):
        if eng == "act":
            return self.K.op("act", lambda h: h.copy(out=out, in_=in_), reads=R, writes=W)
        return self.K.op(eng, lambda h: h.tensor_copy(out=out, in_=in_), reads=R, writes=W)

    def memset(self, ap, val, W, eng="pool"):
        return self.K.op(eng, lambda h: h.memset(ap, val), writes=W)

    def recip(self, out, in_, R, W):
        return self.K.op("dve", lambda h: h.reciprocal(out=out, in_=in_), reads=R, writes=W)

    def dma(self, out, in_, R=(), W=(), q="sp", **kw):
        return self.K.dma(q, out, in_, reads=R, writes=W, **kw)


class Ctx:
    pass


def phase_adaln(C, nlayers):
    g, I = C.g, C.I
    with SS() as s1:
        craw = g.sb("craw", [128, 2, 8], F32, stack=s1)
        for r in range(2):
            g.dma(craw[:, r, :], I["cvec"][r].rearrange("(p k) -> p k", k=8), W=[craw.b[0]])
        scT = g.sb("scT", [128, 8, 2], F32, stack=s1)
        g.act(scT[:].rearrange("p k r -> p r k"), craw[:], AF.Silu, [craw.b[0]], [scT.b[0]])
        adab = g.sb("adab", [2, 6 * D], F32, stack=s1)
        modsb = g.sb("modsb", [2, 6 * D], F32, stack=s1)
        wch = g.sb("wch", [128, 2, 8, 512], F32, nslots=2, stack=s1)
        it = 0
        for l in range(nlayers):
            g.dma(adab[:], I["ada_b"][l:l + 1, :].partition_broadcast(2), W=[adab.b[0]])
            for n in range(12):
                sl = it % 2
                it += 1
                g.dma(wch[:, sl], I["ada_w"][l].rearrange("(p k) n -> p k n", k=8)[:, :, n * 512:(n + 1) * 512],
                      W=[wch.b[sl]])
                p = g.ps()
                for k in range(8):
                    g.mm(p[0:2, :], scT[:, k, :], wch[:, sl, k, :], k == 0, k == 7, [scT.b[0], wch.b[sl]], [p.b[0]])
                g.tt(modsb[:, n * 512:(n + 1) * 512], p[0:2, :], adab[:, n * 512:(n + 1) * 512], ALU.add,
                     [p.b[0], adab.b[0]], [modsb.b[0]])
            g.dma(C.MOD[l], modsb[:], R=[modsb.b[0]], W=[C.B_MOD[l]])


def norm_mod_tiles(C, l, which, s1):
    g, I = C.g, C.I
    so, co = (0, D) if which == 1 else (3 * D, 4 * D)
    gname = "norm1_g" if which == 1 else "norm2_g"
    Abc = g.sb("Abc", [128, 2, D], F32, stack=s1)
    Sbc = g.sb("Sbc", [128, 2, D], F32, stack=s1)
    gbc = g.sb("gbc", [128, D], F32, stack=s1)
    g.dma(gbc[:], I[gname][l:l + 1, :].partition_broadcast(128), W=[gbc.b[0]])
    for r in range(2):
        g.dma(Abc[:, r, :], C.MOD[l, r:r + 1, co:co + D].partition_broadcast(128), R=[C.B_MOD[l]], W=[Abc.b[0]])
        g.dma(Sbc[:, r, :], C.MOD[l, r:r + 1, so:so + D].partition_broadcast(128), R=[C.B_MOD[l]], W=[Sbc.b[0]])
        g.stt(Abc[:, r, :], Abc[:, r, :], 1.0, gbc[:], ALU.add, ALU.mult, [Abc.b[0], gbc.b[0]], [Abc.b[0]])
    return Abc, Sbc


def norm_mod_transpose(C, xt_ap, xt_b, r, Abc, Sbc, T, sl, dstHT, dstB):
    g = C.g
    sq, ssq, hf, hb, hT = T
    g.act(sq[:], xt_ap, AF.Square, [xt_b], [sq.b[0], ssq.b[sl]], accum_out=ssq[:, sl:sl + 1])
    g.act(ssq[:, sl:sl + 1], ssq[:, sl:sl + 1], AF.Sqrt, [ssq.b[sl]], [ssq.b[sl]], bias=C.epsc[:, 0:1], scale=1.0 / D)
    g.recip(ssq[:, sl:sl + 1], ssq[:, sl:sl + 1], [ssq.b[sl]], [ssq.b[sl]])
    g.stt(hf[:, sl], xt_ap, ssq[:, sl:sl + 1], Abc[:, r, :], ALU.mult, ALU.mult,
          [xt_b, ssq.b[sl], Abc.b[0]], [hf.b[sl]])
    g.tt(hb[:, sl], hf[:, sl], Sbc[:, r, :], ALU.add, [hf.b[sl], Sbc.b[0]], [hb.b[sl]], eng="pool")
    p = g.ps()
    pb = p[:].bitcast(BF16)
    for k in range(8):
        g.tr(pb[:, k * 128:(k + 1) * 128], hb[:, sl, k * 128:(k + 1) * 128], C.ident[:],
             [hb.b[sl], C.ident.b[0]], [p.b[0]])
    g.cp(hT[:, sl], pb, [p.b[0]], [hT.b[sl]], eng="act")
    g.dma(dstHT, hT[:, sl], R=[hT.b[sl]], W=[dstB])


def nm_scratch(C, s1):
    g = C.g
    sq = g.sb("sq", [128, D], F32, stack=s1)
    ssq = g.sb("ssq", [128, 2], F32, nslots=2, stack=s1)
    hf = g.sb("hf", [128, 2, D], F32, nslots=2, stack=s1)
    hb = g.sb("hb", [128, 2, D], BF16, nslots=2, stack=s1)
    hT = g.sb("hT", [128, 2, D], BF16, nslots=2, stack=s1)
    return (sq, ssq, hf, hb, hT)


def phase_norm1(C, l):
    g, I = C.g, C.I
    xsrc = I["xin"] if l == 0 else C.XS
    with SS() as s1:
        Abc, Sbc = norm_mod_tiles(C, l, 1, s1)
        xt = g.sb("xt", [128, 2, D], F32, nslots=2, stack=s1)
        T = nm_scratch(C, s1)
        for t in range(NT):
            sl = t % 2
            r = 1 if t < 2 else 0
            g.dma(xt[:, sl], xsrc[t * 128:(t + 1) * 128, :], R=[C.B_XS[t]] if l > 0 else [], W=[xt.b[sl]])
            norm_mod_transpose(C, xt[:, sl], xt.b[sl], r, Abc, Sbc, T, sl, C.HT[t], C.B_HT[t])


def load_ht_all(C, s1):
    g = C.g
    hts = g.sb("hts", [128, 8, NTOK], BF16, nslots=NT, stack=s1)
    for t in range(NT):
        g.dma(hts[:, :, t * 128:(t + 1) * 128], C.HT[t].rearrange("p (k c) -> p k c", k=8), R=[C.B_HT[t]],
              W=[hts.b[t]])
    return hts


def load_w_bf16(C, name, l, c0, c1, tname, s1, krows=8):
    g = C.g
    w = g.sb(tname, [128, krows, c1 - c0], BF16, stack=s1)
    src = C.I[name][l].rearrange("(k p) n -> p k n", p=128)
    for k0 in range(0, krows, 4):
        k1 = min(krows, k0 + 4)
        g.dma(w[:, k0:k1, :], src[:, k0:k1, c0:c1], W=[w.b[0]], q="pool")
    return w


def phase_na(C, l, need_ctx):
    g, I = C.g, C.I
    QC, KC, VC = 2064, 2576, 3088
    with SS() as s1:
        hts = load_ht_all(C, s1)
        wna = load_w_bf16(C, "w_in", l, QC, D_IN, "wna", s1)
        mask = g.sb("namask", [128, 5, 640], F32, stack=s1)
        g.dma(mask[:], I["na_mask"], W=[mask.b[0]])
        braw = g.sb("nabraw", [128, 5, 640], F32, stack=s1)
        bias = g.sb("nabias", [128, 2, 5, 640], BF16, nslots=2, stack=s1)
        qT = g.sb("naqT", [128, NTOK], BF16, nslots=9, stack=s1)
        kT = g.sb("nakT", [128, NTOK], BF16, nslots=9, stack=s1)
        vx = g.sb("navx", [128, NT, 2, 65], BF16, nslots=NT, stack=s1)
        pT = g.sb("napT", [128, 2, 7, 128], BF16, nslots=2, stack=s1)
        rden = g.sb("narden", [128, 2, 2], F32, nslots=2, stack=s1)
        ona = g.sb("naout", [128, 2, 128], BF16, nslots=2, stack=s1)
        g.memset(vx[:], 1.0, vx.b)
        groups = [(0, 256)] + [(256 + 512 * i, 512) for i in range(8)]
        unit = 0
        for hp in range(4):
            for h2 in range(2):
                h = 2 * hp + h2
                g.dma(braw[:], I["na_bias"][l, h], W=[braw.b[0]])
                g.tt(braw[:], braw[:], mask[:], ALU.add, [braw.b[0], mask.b[0]], [braw.b[0]], eng="pool")
                g.act(bias[:, h2], braw[:], AF.Copy, [braw.b[0]], [bias.b[h2]], scale=8.0)
            for gi, (t0, n) in enumerate(groups):
                tl = list(range(t0 // 128, (t0 + n) // 128))
                for (dst, c0) in ((qT, hp * 128), (kT, (KC - QC) + hp * 128)):
                    p = g.ps()
                    for k in range(8):
                        g.mm(p[:, 0:n], wna[:, k, c0:c0 + 128], hts[:, k, t0:t0 + n], k == 0, k == 7,
                             [wna.b[0]] + [hts.b[t] for t in tl], [p.b[0]])
                    g.cp(dst[:, t0:t0 + n], p[:, 0:n], [p.b[0]], [dst.b[gi]], eng="act" if dst is qT else "dve")
                p = g.ps()
                c0 = (VC - QC) + hp * 128
                for j, t in enumerate(tl):
                    for k in range(8):
                        g.mm(p[:, j * 128:(j + 1) * 128], hts[:, k, t * 128:(t + 1) * 128], wna[:, k, c0:c0 + 128],
                             k == 0, k == 7, [wna.b[0], hts.b[t]], [p.b[0]])
                g.cp(vx[:, tl[0]:tl[-1] + 1, :, 0:64],
                     p[:, 0:n // 128 * 128].rearrange("p (a b c) -> p a b c", b=2, c=64),
                     [p.b[0]], [vx.b[t] for t in tl], eng="dve")

            def tok_group(t):
                return 0 if t < 2 else 1 + (t - 2) // 4

            def attend(qt, local_tiles, cls, h2list=(0, 1)):
                nonlocal unit
                sl = unit % 2
                unit += 1
                po = g.ps()
                for h2 in h2list:
                    base = h2 * 64
                    keyt = list(local_tiles) + [0, 1]
                    nk = len(keyt)
                    banks = [g.ps(), g.ps()] if nk > 4 else [g.ps()]
                    for ci, kt in enumerate(keyt):
                        pb = banks[ci // 4]
                        reg = pb[:, (ci % 4) * 128:(ci % 4 + 1) * 128]
                        has_b = ci < len(local_tiles)
                        g.mm(reg, kT[base:base + 64, kt * 128:(kt + 1) * 128], qT[base:base + 64, qt * 128:(qt + 1) * 128],
                             True, not has_b, [kT.b[tok_group(kt)], qT.b[tok_group(qt)]], [pb.b[0]])
                        if has_b:
                            g.mm(reg, C.ident[:], bias[:, h2, cls, ci * 128:(ci + 1) * 128], False, True,
                                 [C.ident.b[0], bias.b[h2]], [pb.b[0]])
                    for bi, pb in enumerate(banks):
                        n = min(4, nk - 4 * bi) * 128
                        g.act(pT[:, sl, 4 * bi:4 * bi + n // 128, :].rearrange("p a b -> p (a b)") if False else
                              pT[:, sl].rearrange("p a b -> p (a b)")[:, bi * 512:bi * 512 + n],
                              pb[:, 0:n], AF.Exp, [pb.b[0]], [pT.b[sl]], scale=0.125)
                    for ci, kt in enumerate(keyt):
                        g.mm(po[:, h2 * 65:(h2 + 1) * 65], pT[:, sl, ci, :], vx[:, kt, h2, :], ci == 0, ci == nk - 1,
                             [pT.b[sl], vx.b[kt]], [po.b[0]])
                    g.recip(rden[:, sl, h2:h2 + 1], po[:, h2 * 65 + 64:h2 * 65 + 65], [po.b[0]], [rden.b[sl]])
                    g.ts(ona[:, sl, h2 * 64:(h2 + 1) * 64], po[:, h2 * 65:h2 * 65 + 64], rden[:, sl, h2:h2 + 1], ALU.mult,
                         [po.b[0], rden.b[sl]], [ona.b[sl]])
                g.dma(C.O[qt * 128:(qt + 1) * 128, 512 + hp * 128:512 + (hp + 1) * 128], ona[:, sl, :],
                      R=[ona.b[sl]], W=[C.B_O[qt]])

            for m in range(32):
                kp0 = na_kp0(m)
                attend(2 + m, [2 + kp0 + j for j in range(5)], na_cls(m))
            if need_ctx:
                for qt in range(2):
                    attend(qt, [], 0)


def silu_from(g, out, outW, x, xR, tmp, tmpB):
    g.act(tmp, x, AF.Exp, xR, [tmpB], scale=-1.0)
    g.ts(tmp, tmp, 1.0, ALU.add, [tmpB], [tmpB])
    g.recip(tmp, tmp, [tmpB], [tmpB])
    g.tt(out, x, tmp, ALU.mult, list(xR) + [tmpB], outW)


def phase_ret(C, l, need_ctx):
    g, I = C.g, C.I
    with SS() as s1:
        lgb = g.sb("lgb", [128, 8], F32, stack=s1)
        g.dma(lgb[:], I["ret_decay"][l:l + 1].rearrange("o a b -> o (a b)").partition_broadcast(128), W=[lgb.b[0]])
        g.act(lgb[:], lgb[:], AF.Exp, [lgb.b[0]], [lgb.b[0]], scale=-float(np.log(2.0)))
        g.act(lgb[:], lgb[:], AF.Ln, [lgb.b[0]], [lgb.b[0]], scale=-1.0, bias=C.onec[:, 0:1])
        cst = g.sb("retc", [128, 6, 128], F32, stack=s1)
        g.dma(cst[:], I["ret_const"], W=[cst.b[0]])
        pcol = g.sb("retpc", [128, 2, 64], F32, stack=s1)
        g.dma(pcol[:], I["ret_pcol"], W=[pcol.b[0]])
        c128 = g.sb("retc128", [128, 128], F32, stack=s1)
        g.memset(c128[:], 128.0, [c128.b[0]])
        BD = g.sb("retBD", [128, 2, 128], F32, stack=s1)
        g.memset(BD[:], 0.0, [BD.b[0]])
        g.memset(BD[0:64, :, 0:64], 1.0, [BD.b[0]])
        g.memset(BD[64:128, :, 64:128], 1.0, [BD.b[0]])
        MT = g.sb("retMT", [128, 2, 2, 128], BF16, stack=s1)
        g.memset(MT[:], 0.0, [MT.b[0]])
        g.memset(MT[0:64, 0], 1.0, [MT.b[0]])
        g.memset(MT[64:128, 1], 1.0, [MT.b[0]])
        D2 = g.sb("retD2", [128, 4, 128], F32, stack=s1)
        tmpd = g.sb("rettmp", [128, 128], F32, stack=s1)
        QF = g.sb("retQF", [128, 2, 128], F32, stack=s1)
        QB = g.sb("retQB", [128, 2, 128], F32, stack=s1)
        KF = g.sb("retKF", [128, 256], F32, stack=s1)
        KB = g.sb("retKB", [128, 256], F32, stack=s1)
        CDF = g.sb("retCDF", [128, 2, 128], F32, stack=s1)
        CDB = g.sb("retCDB", [128, 2, 128], F32, stack=s1)
        R0 = [lgb.b[0], cst.b[0]]
        for h in range(4):
            f, b = lgb[:, h:h + 1], lgb[:, 4 + h:5 + h]
            g.act(D2[:, h, :], cst[:, 0, :], AF.Exp, R0, [D2.b[0]], scale=f)
            g.tt(D2[:, h, :], D2[:, h, :], cst[:, 2, :], ALU.mult, [D2.b[0], cst.b[0]], [D2.b[0]])
            g.act(tmpd[:], cst[:, 1, :], AF.Exp, R0, [tmpd.b[0]], scale=b)
            g.tt(tmpd[:], tmpd[:], cst[:, 3, :], ALU.mult, [tmpd.b[0], cst.b[0]], [tmpd.b[0]])
            g.tt(D2[:, h, :], D2[:, h, :], tmpd[:], ALU.add, [D2.b[0], tmpd.b[0]], [D2.b[0]])
            g.ts(D2[:, h, :], D2[:, h, :], 0.125, ALU.mult, [D2.b[0]], [D2.b[0]])
            pr, bs = h // 2, (h % 2) * 64
            g.act(QF[bs:bs + 64, pr, :], cst[bs:bs + 64, 4, :], AF.Exp, R0, [QF.b[0]], scale=lgb[bs:bs + 64, h:h + 1])
            g.act(QB[bs:bs + 64, pr, :], cst[bs:bs + 64, 5, :], AF.Exp, R0, [QB.b[0]], scale=lgb[bs:bs + 64, 4 + h:5 + h])
            g.act(KF[:, h * 64:(h + 1) * 64], pcol[:, 0, :], AF.Exp, [lgb.b[0], pcol.b[0]], [KF.b[0]], scale=f)
            g.act(KB[:, h * 64:(h + 1) * 64], pcol[:, 1, :], AF.Exp, [lgb.b[0], pcol.b[0]], [KB.b[0]], scale=b)
            g.act(CDF[bs:bs + 64, pr, :], c128[bs:bs + 64, :], AF.Exp, [lgb.b[0], c128.b[0]], [CDF.b[0]],
                  scale=lgb[bs:bs + 64, h:h + 1])
            g.act(CDB[bs:bs + 64, pr, :], c128[bs:bs + 64, :], AF.Exp, [lgb.b[0], c128.b[0]], [CDB.b[0]],
                  scale=lgb[bs:bs + 64, 4 + h:5 + h])
        g.ts(KF[:], KF[:], 0.125, ALU.mult, [KF.b[0]], [KF.b[0]])
        g.ts(KB[:], KB[:], 0.125, ALU.mult, [KB.b[0]], [KB.b[0]])

        import os
        RS = int(os.environ.get("RET_STOP", "9"))
        if RS <= 1:
            return
        qkT = g.sb("retqkT", [128, NT, 4, 128], BF16, nslots=NT, stack=s1)
        vall = g.sb("retv", [128, NT, 256], BF16, nslots=NT, stack=s1)
        sg = g.sb("retsg", [128, NT, 256], BF16, nslots=NT, stack=s1)
        SinF = g.sb("retSinF", [128, NT, 2, 128], BF16, nslots=NT, stack=s1)
        SinB = g.sb("retSinB", [128, NT, 2, 128], BF16, nslots=NT, stack=s1)
        with SS() as s2:
            wret = load_w_bf16(C, "w_in", l, 0, 1024, "wret", s2)
            kdf = g.sb("retkdf", [128, NT, 256], BF16, nslots=NT, stack=s2)
            kdb = g.sb("retkdb", [128, NT, 256], BF16, nslots=NT, stack=s2)
            ht = g.sb("retht", [128, 2, 8, 128], BF16, nslots=2, stack=s2)
            rope = g.sb("retrope", [128, 2, 2, 256], F32, nslots=2, stack=s2)
            qk32 = g.sb("retqk32", [128, 512], F32, stack=s2)
            ra = g.sb("retra", [128, 4, 256], F32, nslots=2, stack=s2)
            qkr = g.sb("retqkr", [128, 2, 512], BF16, nslots=2, stack=s2)
            for t in range(NT):
                sl = t % 2
                g.dma(ht[:, sl], C.HT[t].rearrange("p (k c) -> p k c", k=8), R=[C.B_HT[t]], W=[ht.b[sl]])
                p0, p1 = g.ps(), g.ps()
                for (p, n0) in ((p0, 0), (p1, 512)):
                    for k in range(8):
                        g.mm(p[:, :], ht[:, sl, k, :], wret[:, k, n0:n0 + 512], k == 0, k == 7,
                             [ht.b[sl], wret.b[0]], [p.b[0]])
                SUB = int(os.environ.get("RET_SUB", "9"))
                if SUB <= 0:
                    continue
                if os.environ.get("RET_V", "1") == "1":
                    g.cp(vall[:, t, :], p1[:, 0:256], [p1.b[0]], [vall.b[t]], eng="dve")
                if os.environ.get("RET_G", "1") == "1":
                    silu_from(g, sg[:, t, :], [sg.b[t]], p1[:, 256:512], [p1.b[0]], qk32[:, 0:256], qk32.b[0])
                if SUB <= 1:
                    continue
                if t >= 2:
                    g.dma(rope[:, sl], I["rope"][(t - 2) * 128:(t - 1) * 128], W=[rope.b[sl]])
                    g.cp(qk32[:], p0[:, :], [p0.b[0]], [qk32.b[0]], eng="act")
                    v4 = qk32[:].rearrange("p (h a c) -> p h a c", a=2, c=32)
                    t1, t2 = v4[:, :, 0, :], v4[:, :, 1, :]
                    cs = rope[:, sl, 0, :].rearrange("p (h c) -> p h c", c=32)
                    sn = rope[:, sl, 1, :].rearrange("p (h c) -> p h c", c=32)
                    rv = [ra[:, i, :].rearrange("p (h c) -> p h c", c=32) for i in range(4)]
                    RR = [qk32.b[0], rope.b[sl]]
                    g.tt(rv[0], t1, cs, ALU.mult, RR, [ra.b[0]], eng="dve")
                    g.tt(rv[1], t2, sn, ALU.mult, RR, [ra.b[0]], eng="pool")
                    g.tt(rv[2], t1, sn, ALU.mult, RR, [ra.b[1]], eng="dve")
                    g.tt(rv[3], t2, cs, ALU.mult, RR, [ra.b[1]], eng="pool")
                    o4 = qkr[:, sl, :].rearrange("p (h a c) -> p h a c", a=2, c=32)
                    g.tt(o4[:, :, 0, :], rv[0], rv[1], ALU.subtract, [ra.b[0]], [qkr.b[sl]], eng="dve")
                    g.tt(o4[:, :, 1, :], rv[2], rv[3], ALU.add, [ra.b[1]], [qkr.b[sl]], eng="pool")
                else:
                    g.cp(qkr[:, sl, :], p0[:, :], [p0.b[0]], [qkr.b[sl]], eng="act")
                if SUB <= 2:
                    continue
                g.tt(kdf[:, t, :], qkr[:, sl, 256:512], KF[:], ALU.mult, [qkr.b[sl], KF.b[0]], [kdf.b[t]], eng="dve")
                g.tt(kdb[:, t, :], qkr[:, sl, 256:512], KB[:], ALU.mult, [qkr.b[sl], KB.b[0]], [kdb.b[t]], eng="pool")
                if SUB <= 3:
                    continue
                pt = g.ps()
                ptb = pt[:].bitcast(BF16)
                for c4 in range(4):
                    g.tr(ptb[:, c4 * 128:(c4 + 1) * 128], qkr[:, sl, c4 * 128:(c4 + 1) * 128], C.ident[:],
                         [qkr.b[sl], C.ident.b[0]], [pt.b[0]])
                g.cp(qkT[:, t].rearrange("p a b -> p (a b)"), ptb[:, 0:512], [pt.b[0]], [qkT.b[t]], eng="act")

            if RS <= 2:
                return
            S = g.sb("retS", [128, 2, 2, 128], F32, nslots=2, stack=s2)
            tS = g.sb("rettS", [128, 2, 2, 128], F32, nslots=2, stack=s2)
            g.memset(S[:], 0.0, S.b)
            order_f = list(range(NT))
            order_b = [1, 0] + list(range(NT - 1, 1, -1))
            for step in range(NT):
                for d, (order, kd, Sin, CDt) in enumerate(((order_f, kdf, SinF, CDF), (order_b, kdb, SinB, CDB))):
                    t = order[step]
                    p = g.ps()
                    for pr in range(2):
                        g.mm(p[:, pr * 128:(pr + 1) * 128], kd[:, t, pr * 128:(pr + 1) * 128], vall[:, t, pr * 128:(pr + 1) * 128],
                             True, True, [kd.b[t], vall.b[t]], [p.b[0]])
                    g.tt(tS[:, d].rearrange("p a b -> p (a b)"), p[:, 0:256], BD[:].rearrange("p a b -> p (a b)"), ALU.mult,
                         [p.b[0], BD.b[0]], [tS.b[d]], eng="dve")
                    g.cp(Sin[:, t], S[:, d], [S.b[d]], [Sin.b[t]], eng="act")
                    g.tt(S[:, d], S[:, d], CDt[:], ALU.mult, [S.b[d], CDt.b[0]], [S.b[d]], eng="pool")
                    g.tt(S[:, d], S[:, d], tS[:, d], ALU.add, [S.b[d], tS.b[d]], [S.b[d]], eng="pool")

        if RS <= 3:
            return
        with SS() as s2:
            AT = g.sb("retAT", [128, 2, 4, 128], BF16, nslots=2, stack=s2)
            qm = g.sb("retqm", [128, 2, 4, 128], BF16, nslots=2, stack=s2)
            qsf = g.sb("retqsf", [128, 2, 2, 128], BF16, nslots=2, stack=s2)
            qsb = g.sb("retqsb", [128, 2, 2, 128], BF16, nslots=2, stack=s2)
            o32 = g.sb("reto32", [128, 2, 256], F32, nslots=2, stack=s2)
            osq = g.sb("retosq", [128, 256], F32, stack=s2)
            rs = g.sb("retrs", [128, 2, 4], F32, nslots=2, stack=s2)
            ob = g.sb("retob", [128, 2, 256], BF16, nslots=2, stack=s2)
            for t in range(NT):
                if t < 2 and not need_ctx:
                    continue
                sl = t % 2
                for par in range(2):
                    g.tt(qm[:, sl, 2 * par:2 * par + 2, :], qkT[:, t, 0:2, :], MT[:, par], ALU.mult, [qkT.b[t], MT.b[0]],
                         [qm.b[sl]], eng="pool")
                ps_ = g.ps()
                for h in range(4):
                    pr, par = h // 2, h % 2
                    g.mm(ps_[:, h * 128:(h + 1) * 128], qkT[:, t, 2 + pr, :], qm[:, sl, 2 * par + pr, :], True, True,
                         [qkT.b[t], qm.b[sl]], [ps_.b[0]])
                g.tt(AT[:, sl].rearrange("p a b -> p (a b)"), ps_[:, :], D2[:].rearrange("p a b -> p (a b)"), ALU.mult,
                     [ps_.b[0], D2.b[0]], [AT.b[sl]], eng="dve")
                g.tt(qsf[:, sl], qkT[:, t, 0:2, :], QF[:], ALU.mult, [qkT.b[t], QF.b[0]], [qsf.b[sl]], eng="pool")
                g.tt(qsb[:, sl], qkT[:, t, 0:2, :], QB[:], ALU.mult, [qkT.b[t], QB.b[0]], [qsb.b[sl]], eng="pool")
                po = g.ps()
                for pr in range(2):
                    reg = po[:, pr * 128:(pr + 1) * 128]
                    g.mm(reg, qsf[:, sl, pr, :], SinF[:, t, pr, :], True, False, [qsf.b[sl], SinF.b[t]], [po.b[0]])
                    g.mm(reg, qsb[:, sl, pr, :], SinB[:, t, pr, :], False, False, [qsb.b[sl], SinB.b[t]], [po.b[0]])
                    for par in range(2):
                        h = 2 * pr + par
                        g.mm(po[:, h * 64:(h + 1) * 64], AT[:, sl, h, :], vall[:, t, h * 64:(h + 1) * 64], False, par == 1,
                             [AT.b[sl], vall.b[t]], [po.b[0]])
                g.cp(o32[:, sl, :], po[:, 0:256], [po.b[0]], [o32.b[sl]], eng="act")
                g.tt(osq[:], o32[:, sl, :], o32[:, sl, :], ALU.mult, [o32.b[sl]], [osq.b[0]], eng="pool")
                g.K.op("dve", lambda h_, o_=rs[:, sl, :], i_=osq[:].rearrange("p (h c) -> p h c", c=64):
                       h_.tensor_reduce(out=o_, in_=i_, axis=AX.X, op=ALU.add), reads=[osq.b[0]], writes=[rs.b[sl]])
                g.act(rs[:, sl, :], rs[:, sl, :], AF.Sqrt, [rs.b[sl]], [rs.b[sl]], bias=C.epsc[:, 0:1], scale=1.0 / 64)
                g.recip(rs[:, sl, :], rs[:, sl, :], [rs.b[sl]], [rs.b[sl]])
                for h in range(4):
                    g.stt(ob[:, sl, h * 64:(h + 1) * 64], o32[:, sl, h * 64:(h + 1) * 64], rs[:, sl, h:h + 1],
                          sg[:, t, h * 64:(h + 1) * 64], ALU.mult, ALU.mult, [o32.b[sl], rs.b[sl], sg.b[t]], [ob.b[sl]])
                g.dma(C.O[t * 128:(t + 1) * 128, 0:256], ob[:, sl, :], R=[ob.b[sl]], W=[C.B_O[t]])


def phase_gdn(C, l, need_ctx):
    g, I = C.g, C.I
    NCH = NTOK // 64
    order = [list(range(NCH)), [3, 2, 1, 0] + list(range(NCH - 1, 3, -1))]
    id64 = C.ident[0:64, 0:64]
    with SS() as s1:
        cst = g.sb("gdnc", [64, 13, 64], F32, stack=s1)
        g.dma(cst[:], I["gdn_const"], W=[cst.b[0]])
        CB = cst.b[0]
        Y4, SU4, I4 = cst[:, 0:4, :], cst[:, 4:8, :], cst[:, 8:12, :]
        idf, ones = cst[:, 8, :], cst[:, 12, :]
        one64 = C.onec[0:64, 0:1]
        eps64 = C.epsc[0:64, 0:1]
        ab = g.sb("gab", [64, 16, NCH], F32, stack=s1)
        with SS() as s2:
            ht = g.sb("ght", [128, 2, 8, 128], BF16, nslots=2, stack=s2)
            wab = load_w_bf16(C, "w_in", l, 2048, 2064, "wab", s2)
            p = None
            for t in range(NT):
                sl = t % 2
                g.dma(ht[:, sl], C.HT[t].rearrange("p (k c) -> p k c", k=8), R=[C.B_HT[t]], W=[ht.b[sl]])
                for half in range(2):
                    c = 2 * t + half
                    if c % 32 == 0:
                        p = g.ps()
                        c0 = c
                    reg = p[0:64, (c - c0) * 16:(c - c0 + 1) * 16]
                    for k in range(8):
                        g.mm(reg, ht[:, sl, k, half * 64:(half + 1) * 64], wab[:, k, 0:16], k == 0, k == 7,
                             [ht.b[sl], wab.b[0]], [p.b[0]])
                    if c % 32 == 31 or c == NCH - 1:
                        n = c - c0 + 1
                        g.cp(ab[:, :, c0:c0 + n].rearrange("p k c -> p c k"),
                             p[0:64, 0:n * 16].rearrange("p (c k) -> p c k", k=16), [p.b[0]], [ab.b[0]], eng="dve")
        prm = g.sb("gprm", [64, 2, 8], F32, stack=s1)
        g.dma(prm[:, 0, :], I["gdn_a_log"][l:l + 1].rearrange("o a b -> o (a b)").partition_broadcast(64), W=[prm.b[0]])
        g.dma(prm[:, 1, :], I["gdn_dt_bias"][l:l + 1].rearrange("o a b -> o (a b)").partition_broadcast(64), W=[prm.b[0]])
        sc = {}
        for nm in ("G", "beta", "nbeta", "egi", "erem", "egl", "begi"):
            sc[nm] = g.sb("g" + nm, [64, 8, NCH], F32, stack=s1)
        G, beta, nbeta, egi, erem, egl, begi = [sc[k] for k in ("G", "beta", "nbeta", "egi", "erem", "egl", "begi")]
        bc8 = lambda ap: ap.unsqueeze(2).to_broadcast([64, 8, NCH])
        g.tt(G[:], ab[:, 0:8, :], bc8(prm[:, 1, :]), ALU.add, [ab.b[0], prm.b[0]], [G.b[0]])
        g.act(G[:], G[:], AF.Exp, [G.b[0]], [G.b[0]])
        g.act(G[:], G[:], AF.Ln, [G.b[0]], [G.b[0]], bias=one64)
        g.act(prm[:, 0, :], prm[:, 0, :], AF.Exp, [prm.b[0]], [prm.b[0]])
        g.ts(prm[:, 0, :], prm[:, 0, :], -1.0, ALU.mult, [prm.b[0]], [prm.b[0]])
        g.tt(G[:], G[:], bc8(prm[:, 0, :]), ALU.mult, [G.b[0], prm.b[0]], [G.b[0]])
        g.act(beta[:], ab[:, 8:16, :], AF.Exp, [ab.b[0]], [beta.b[0]], scale=-1.0)
        g.ts(beta[:], beta[:], 1.0, ALU.add, [beta.b[0]], [beta.b[0]])
        g.recip(beta[:], beta[:], [beta.b[0]], [beta.b[0]])
        g.ts(nbeta[:], beta[:], -1.0, ALU.mult, [beta.b[0]], [nbeta.b[0]])
        for d in range(2):
            rhs = G[:, d * 4:(d + 1) * 4, :].rearrange("p a c -> p (a c)")
            for (dst, lhs) in ((egi, cst[:, d, :]), (erem, cst[:, 4 + d, :]), (egl, ones)):
                p = g.ps()
                g.mm(p[0:64, 0:4 * NCH], lhs, rhs, True, True, [CB, G.b[0]], [p.b[0]])
                g.act(dst[:, d * 4:(d + 1) * 4, :].rearrange("p a c -> p (a c)"), p[0:64, 0:4 * NCH], AF.Exp, [p.b[0]],
                      [dst.b[0]])
        g.tt(begi[:], beta[:], egi[:], ALU.mult, [beta.b[0], egi.b[0]], [begi.b[0]])
        ngb = g.sb("gngb", [64, 64], F32, stack=s1)
        g.dma(ngb[:], I["gdn_norm_g"][l:l + 1, :].partition_broadcast(64), W=[ngb.b[0]])

        for hp in range(2):
            with SS() as s2:
                qT = g.sb("gqT", [64, 2, NTOK], BF16, nslots=2, stack=s2)
                kT = g.sb("gkT", [64, 2, NTOK], BF16, nslots=2, stack=s2)
                ktm = g.sb("gktm", [64, 2, NCH, 64], BF16, nslots=2, stack=s2)
                vtm = g.sb("gvtm", [64, 2, NCH, 64], BF16, nslots=2, stack=s2)
                sgt = g.sb("gsgt", [64, 2, NCH, 64], BF16, stack=s2)
                with SS() as s3:
                    wg = load_w_bf16(C, "w_in", l, 1024, 2048, "wg", s3)
                    raw = g.sb("graw", [128, NTOK + 8], F32, stack=s3)
                    acc = g.sb("gacc", [128, NTOK], F32, stack=s3)
                    o128 = g.sb("go128", [128, NTOK], BF16, stack=s3)
                    hg = g.sb("ghg", [128, 2, 8, 512], BF16, nslots=2, stack=s3)
                    cw = g.sb("gcw", [128, 3, 5], F32, stack=s3)
                    sq = g.sb("gsq", [128, 2, 512], F32, nslots=2, stack=s3)
                    bd1 = g.sb("gbd1", [128, 128], F32, stack=s3)
                    g.dma(bd1[:], I["gdn_bd"], W=[bd1.b[0]])
                    g.memset(raw[:], 0.0, [raw.b[0]])
                    g.dma(cw[:], I["gdn_convw"][l, 2 * hp:2 * hp + 2].rearrange("h d t k -> (h d) t k"), W=[cw.b[0]])
                    groups = [(0, 256)] + [(256 + 512 * i, 512) for i in range(8)]
                    git = 0
                    for typ in range(3):
                        wc0 = typ * 256 + hp * 128
                        for (t0, n) in groups:
                            sl = git % 2
                            git += 1
                            for q in range(n // 128):
                                t = t0 // 128 + q
                                g.dma(hg[:, sl, :, q * 128:(q + 1) * 128], C.HT[t].rearrange("p (k c) -> p k c", k=8),
                                      R=[C.B_HT[t]], W=[hg.b[sl]])
                            p = g.ps()
                            for k in range(8):
                                g.mm(p[:, 0:n], wg[:, k, wc0:wc0 + 128], hg[:, sl, k, 0:n], k == 0, k == 7,
                                     [wg.b[0], hg.b[sl]], [p.b[0]])
                            off = t0 + 2 if t0 < 256 else t0 + 6
                            g.cp(raw[:, off:off + n], p[:, 0:n], [p.b[0]], [raw.b[0]], eng="act")
                        for (a0, n, r0) in ((0, 256, 0), (256, 2048, 260), (2304, 2048, 2308)):
                            for k in range(5):
                                src = raw[:, r0 + k:r0 + k + n]
                                if k == 0:
                                    g.ts(acc[:, a0:a0 + n], src, cw[:, typ, 0:1], ALU.mult, [raw.b[0], cw.b[0]], [acc.b[0]])
                                else:
                                    g.stt(acc[:, a0:a0 + n], src, cw[:, typ, k:k + 1], acc[:, a0:a0 + n], ALU.mult, ALU.add,
                                          [raw.b[0], cw.b[0], acc.b[0]], [acc.b[0]])
                        g.act(acc[:], acc[:], AF.Silu, [acc.b[0]], [acc.b[0]])
                        if typ == 2:
                            g.cp(o128[:], acc[:], [acc.b[0]], [o128.b[0]], eng="pool")
                        else:
                            for gi2, (t0, n) in enumerate(groups):
                                ss = gi2 % 2
                                g.tt(sq[:, ss, 0:n], acc[:, t0:t0 + n], acc[:, t0:t0 + n], ALU.mult, [acc.b[0]], [sq.b[ss]],
                                     eng="pool")
                                p = g.ps()
                                g.mm(p[:, 0:n], bd1[:], sq[:, ss, 0:n], True, True, [bd1.b[0], sq.b[ss]], [p.b[0]])
                                g.act(sq[:, ss, 0:n], p[:, 0:n], AF.Sqrt, [p.b[0]], [sq.b[ss]], bias=C.epsc[:, 0:1])
                                g.recip(sq[:, ss, 0:n], sq[:, ss, 0:n], [sq.b[ss]], [sq.b[ss]])
                                g.stt(o128[:, t0:t0 + n], acc[:, t0:t0 + n], 0.125 if typ == 0 else 1.0, sq[:, ss, 0:n],
                                      ALU.mult, ALU.mult, [acc.b[0], sq.b[ss]], [o128.b[0]])
                            dst = qT if typ == 0 else kT
                            for hh in range(2):
                                g.dma(dst[:, hh, :], o128[hh * 64:(hh + 1) * 64, :], R=[o128.b[0]], W=[dst.b[hh]])
                        if typ >= 1:
                            dst_tm = ktm if typ == 1 else vtm
                            for c0 in range(0, NCH, 8):
                                nn = min(8, NCH - c0)
                                p = g.ps()
                                pb = p[:].bitcast(BF16)
                                for ci in range(nn):
                                    c = c0 + ci
                                    g.tr(pb[0:64, ci * 128:(ci + 1) * 128], o128[:, c * 64:(c + 1) * 64], C.ident[:],
                                         [o128.b[0], C.ident.b[0]], [p.b[0]])
                                g.cp(dst_tm[:, :, c0:c0 + nn, :], pb[0:64, 0:nn * 128].rearrange("p (c h d) -> p h c d", h=2, d=64),
                                     [p.b[0]], [dst_tm.b[0], dst_tm.b[1]], eng="act")
                    ht2 = g.sb("ght2", [128, 2, 8, 128], BF16, nslots=2, stack=s3)
                    sg32 = g.sb("gsg32", [64, 512], F32, stack=s3)
                    p = None
                    for t in range(NT):
                        sl = t % 2
                        g.dma(ht2[:, sl], C.HT[t].rearrange("p (k c) -> p k c", k=8), R=[C.B_HT[t]], W=[ht2.b[sl]])
                        for half in range(2):
                            c = 2 * t + half
                            if c % 4 == 0:
                                p = g.ps()
                                c0 = c
                            reg = p[0:64, (c - c0) * 128:(c - c0 + 1) * 128]
                            for k in range(8):
                                g.mm(reg, ht2[:, sl, k, half * 64:(half + 1) * 64], wg[:, k, 768 + hp * 128:768 + (hp + 1) * 128],
                                     k == 0, k == 7, [ht2.b[sl], wg.b[0]], [p.b[0]])
                            if c % 4 == 3:
                                g.cp(sg32[:], p[0:64, :], [p.b[0]], [sg32.b[0]], eng="act")
                                g.act(sgt[:, :, c0:c0 + 4, :], sg32[:].rearrange("p (c h d) -> p h c d", h=2, d=64), AF.Silu,
                                      [sg32.b[0]], [sgt.b[0]])

                Oacc = g.sb("gOacc", [64, 2, NCH, 64], F32, nslots=2, stack=s2)
                with SS() as s3:
                    NSET, RS_ = 3, 4

                    def tn(nm, dt, ns, w=64, extra=()):
                        return g.sb(nm, [64, ns] + list(extra) + [4, w] if not extra else [64, ns] + list(extra), dt, nslots=ns, stack=s3)
                    X4 = tn("gX4", F32, NSET)
                    E8 = g.sb("gE8", [64, NSET, 4, 2, 64], F32, nslots=NSET, stack=s3)
                    DT4, DS4, P0, P0T, TT = [tn(n_, F32, NSET) for n_ in ("gDT4", "gDS4", "gP0", "gP0T", "gTT")]
                    PP = g.sb("gPP", [64, NSET * 2, 4, 2, 64], F32, nslots=NSET * 2, stack=s3)
                    TTb, vb4, kbeg4 = [tn(n_, BF16, NSET) for n_ in ("gTTb", "gvb4", "gkbeg4")]
                    attn4, wTb, kdec4 = [tn(n_, BF16, RS_) for n_ in ("gattn4", "gwTb", "gkdec4")]
                    U4 = tn("gU4", F32, RS_)
                    S = g.sb("gS", [64, 4, 64], F32, stack=s3)
                    Sb = g.sb("gSb", [64, 4, 64], BF16, stack=s3)
                    vn4 = g.sb("gvn4", [64, 4, 64], BF16, stack=s3)
                    qs4 = g.sb("gqs4", [64, 4, 64], F32, stack=s3)
                    g.memset(S[:], 0.0, [S.b[0]])
                    g.memset(Sb[:], 0.0, [Sb.b[0]])
                    idr = g.sb("gidr", [64, 64], F32, stack=s3)
                    g.cp(idr[:].bitcast(F32R), idf, [CB], [idr.b[0]])
                    RR_ = lambda ap: ap.bitcast(F32R)
                    visited = set()
                    units = [(hh, d) for hh in range(2) for d in range(2)]

                    def col(tile_, u, step):
                        hh, d = units[u]
                        c = order[d][step]
                        dh = d * 4 + 2 * hp + hh
                        return tile_[:, dh, c:c + 1]

                    def indep(step):
                        ts_ = step % NSET
                        sl = step % RS_
                        cs = [order[d][step] for (hh, d) in units]
                        for u in range(4):
                            g.ts(X4[:, ts_, u, :], SU4[:, u, :], col(G, u, step), ALU.mult, [CB, G.b[0]], [X4.b[ts_]])
                        yield
                        pa, pbk = g.ps(), g.ps()
                        pav = pa[0:64, :].rearrange("p (u t i) -> p u t i", u=4, t=2)
                        pbv = pbk[0:64, :].rearrange("p (u t i) -> p u t i", u=4, t=2)
                        for u, (hh, d) in enumerate(units):
                            g.mm(pav[:, u, 0, :], X4[:, ts_, u, :], cst[:, d, :], True, True, [X4.b[ts_], CB], [pa.b[0]])
                            g.mm(pav[:, u, 1, :], cst[:, d, :], X4[:, ts_, u, :], True, True, [X4.b[ts_], CB], [pa.b[0]])
                        for u, (hh, d) in enumerate(units):
                            c = cs[u]
                            kc, qc = kT[:, hh, c * 64:(c + 1) * 64], qT[:, hh, c * 64:(c + 1) * 64]
                            g.mm(pbv[:, u, 0, :], kc, qc, True, True, [kT.b[hh], qT.b[hh]], [pbk.b[0]])
                            g.mm(pbv[:, u, 1, :], kc, kc, True, True, [kT.b[hh]], [pbk.b[0]])
                        yield
                        g.act(E8[:, ts_].rearrange("p u t i -> p (u t i)"), pa[0:64, :], AF.Exp, [pa.b[0]], [E8.b[ts_]])
                        yield
                        g.tt(DT4[:, ts_], E8[:, ts_, :, 0, :], Y4, ALU.mult, [E8.b[ts_], CB], [DT4.b[ts_]], eng="pool")
                        g.tt(DS4[:, ts_], E8[:, ts_, :, 1, :], SU4, ALU.mult, [E8.b[ts_], CB], [DS4.b[ts_]], eng="pool")
                        yield
                        for u in range(4):
                            g.stt(RR_(P0[:, ts_, u, :]), pbv[:, u, 1, :], col(nbeta, u, step), DS4[:, ts_, u, :], ALU.mult, ALU.mult,
                                  [pbk.b[0], nbeta.b[0], DS4.b[ts_]], [P0.b[ts_]])
                        g.tt(attn4[:, sl], pbv[:, :, 0, :], DT4[:, ts_], ALU.mult, [pbk.b[0], DT4.b[ts_]], [attn4.b[sl]])
                        yield
                        pc = g.ps()
                        pcv = pc[0:64, 0:256].rearrange("p (u i) -> p u i", u=4)
                        for u in range(4):
                            g.mm(pcv[:, u, :], RR_(P0[:, ts_, u, :]), RR_(idr[:]), True, True, [P0.b[ts_], idr.b[0]], [pc.b[0]])
                        yield
                        g.cp(RR_(P0T[:, ts_].rearrange("p u i -> p (u i)")), pc[0:64, 0:256], [pc.b[0]], [P0T.b[ts_]], eng="act")
                        yield
                        g.tt(RR_(TT[:, ts_]), P0T[:, ts_], I4, ALU.add, [P0T.b[ts_], CB], [TT.b[ts_]], eng="pool")
                        Pk, PkT, PkB = P0[:, ts_], P0T[:, ts_], [P0.b[ts_], P0T.b[ts_]]

                        def sq_mm(k, Pk, PkT, PkB):
                            pd = g.ps()
                            pdv = pd[0:64, :].rearrange("p (u t i) -> p u t i", u=4, t=2)
                            for u in range(4):
                                g.mm(pdv[:, u, 0, :], RR_(PkT[:, u, :]), RR_(Pk[:, u, :]), True, True, PkB, [pd.b[0]])
                                if k < 4:
                                    g.mm(pdv[:, u, 1, :], RR_(Pk[:, u, :]), RR_(PkT[:, u, :]), True, True, PkB, [pd.b[0]])
                            return pd

                        pd = sq_mm(0, Pk, PkT, PkB)
                        yield
                        for k in range(5):
                            ps_ = ts_ * 2 + k % 2
                            g.cp(RR_(PP[:, ps_].rearrange("p u t i -> p (u t i)")), pd[0:64, :], [pd.b[0]], [PP.b[ps_]], eng="act")
                            yield
                            Pk, PkT, PkB = PP[:, ps_, :, 0, :], PP[:, ps_, :, 1, :], [PP.b[ps_]]
                            if k < 4:
                                pd = sq_mm(k + 1, Pk, PkT, PkB)
                            pe_ = g.ps()
                            pev = pe_[0:64, 0:256].rearrange("p (u i) -> p u i", u=4)
                            for u in range(4):
                                g.mm(pev[:, u, :], RR_(Pk[:, u, :]), RR_(TT[:, ts_, u, :]), True, True, PkB + [TT.b[ts_]], [pe_.b[0]])
                            yield
                            g.tt(RR_(TT[:, ts_]), TT[:, ts_], pev, ALU.add, [TT.b[ts_], pe_.b[0]], [TT.b[ts_]])
                        g.cp(TTb[:, ts_], TT[:, ts_], [TT.b[ts_]], [TTb.b[ts_]], eng="act")
                        for u, (hh, d) in enumerate(units):
                            c = cs[u]
                            g.ts(vb4[:, ts_, u, :], vtm[:, hh, c, :], col(beta, u, step), ALU.mult, [vtm.b[hh], beta.b[0]], [vb4.b[ts_]])
                            g.ts(kbeg4[:, ts_, u, :], ktm[:, hh, c, :], col(begi, u, step), ALU.mult, [ktm.b[hh], begi.b[0]],
                                 [kbeg4.b[ts_]])
                            g.ts(kdec4[:, sl, u, :], ktm[:, hh, c, :], col(erem, u, step), ALU.mult, [ktm.b[hh], erem.b[0]],
                                 [kdec4.b[sl]])
                        yield
                        pf = g.ps()
                        pfv = pf[0:64, :].rearrange("p (u t i) -> p u t i", u=4, t=2)
                        for u in range(4):
                            g.mm(pfv[:, u, 0, :], TTb[:, ts_, u, :], vb4[:, ts_, u, :], True, True, [TTb.b[ts_], vb4.b[ts_]], [pf.b[0]])
                            g.mm(pfv[:, u, 1, :], kbeg4[:, ts_, u, :], TTb[:, ts_, u, :], True, True, [TTb.b[ts_], kbeg4.b[ts_]],
                                 [pf.b[0]])
                        yield
                        g.cp(U4[:, sl], pfv[:, :, 0, :], [pf.b[0]], [U4.b[sl]], eng="act")
                        g.cp(wTb[:, sl], pfv[:, :, 1, :], [pf.b[0]], [wTb.b[sl]], eng="act")

                    def dep(step):
                        sl = step % RS_
                        cs = [order[d][step] for (hh, d) in units]
                        pg = g.ps()
                        pgv = pg[0:64, :].rearrange("p (u t i) -> p u t i", u=4, t=2)
                        for u, (hh, d) in enumerate(units):
                            c = cs[u]
                            g.mm(pgv[:, u, 0, :], wTb[:, sl, u, :], Sb[:, u, :], True, True, [wTb.b[sl], Sb.b[0]], [pg.b[0]])
                            g.mm(pgv[:, u, 1, :], qT[:, hh, c * 64:(c + 1) * 64], Sb[:, u, :], True, True, [qT.b[hh], Sb.b[0]],
                                 [pg.b[0]])
                        g.tt(vn4[:], U4[:, sl], pgv[:, :, 0, :], ALU.subtract, [U4.b[sl], pg.b[0]], [vn4.b[0]])
                        for u in range(4):
                            g.act(qs4[:, u, :], pgv[:, u, 1, :], AF.Copy, [pg.b[0], egi.b[0]], [qs4.b[0]], scale=col(egi, u, step))
                        ph = g.ps()
                        phv = ph[0:64, :].rearrange("p (u t i) -> p u t i", u=4, t=2)
                        for u in range(4):
                            g.mm(phv[:, u, 0, :], kdec4[:, sl, u, :], vn4[:, u, :], True, True, [kdec4.b[sl], vn4.b[0]], [ph.b[0]])
                            g.mm(phv[:, u, 1, :], attn4[:, sl, u, :], vn4[:, u, :], True, True, [attn4.b[sl], vn4.b[0]], [ph.b[0]])
                        for u in range(4):
                            g.stt(S[:, u, :], S[:, u, :], col(egl, u, step), phv[:, u, 0, :], ALU.mult, ALU.add,
                                  [S.b[0], egl.b[0], ph.b[0]], [S.b[0]])
                        g.cp(Sb[:], S[:], [S.b[0]], [Sb.b[0]], eng="act")
                        for u, (hh, d) in enumerate(units):
                            c = cs[u]
                            if (hh, c) not in visited:
                                visited.add((hh, c))
                                g.tt(Oacc[:, hh, c, :], qs4[:, u, :], phv[:, u, 1, :], ALU.add, [qs4.b[0], ph.b[0]], [Oacc.b[hh]])
                            else:
                                g.tt(qs4[:, u, :], qs4[:, u, :], phv[:, u, 1, :], ALU.add, [qs4.b[0], ph.b[0]], [qs4.b[0]])
                                g.tt(Oacc[:, hh, c, :], Oacc[:, hh, c, :], qs4[:, u, :], ALU.add, [qs4.b[0], Oacc.b[hh]],
                                     [Oacc.b[hh]], eng="pool")

                    NS = int(os.environ.get("GDN_STEPS", str(NCH)))
                    gens = []
                    done = set()
                    nxt_i, nxt_d = 0, 0
                    while nxt_d < NS:
                        while len(gens) < NSET and nxt_i < NS and nxt_i < nxt_d + RS_:
                            gens.append((nxt_i, indep(nxt_i)))
                            nxt_i += 1
                        still = []
                        for (st_, ge_) in gens:
                            try:
                                next(ge_)
                                still.append((st_, ge_))
                            except StopIteration:
                                done.add(st_)
                        gens = still
                        while nxt_d in done:
                            dep(nxt_d)
                            nxt_d += 1
                    if C.DBG is not None and hp == 0 and NS < NCH:
                        o_ = 0
                        for nm_, tl_, ap_ in (("S", S, S[:]), ("qs4", qs4, qs4[:])):
                            g.dma(C.DBG[:, o_:o_ + 256].rearrange("p (u i) -> p u i", u=4), ap_, R=tl_.b, W=[C.B_DBG], q="pool")
                            o_ += 256

                if C.DBG is not None and hp == 0 and "GDN_STEPS" not in os.environ:
                    o_ = 0
                    for tl_ in (G, beta, egi, erem, egl):
                        g.dma(C.DBG[:, o_:o_ + 8 * NCH], tl_[:].rearrange("p a c -> p (a c)"), R=[tl_.b[0]], W=[C.B_DBG], q="pool")
                        o_ += 8 * NCH
                    g.dma(C.DBG[:, 3000:3512], qT[:, 0, 0:512], R=[qT.b[0]], W=[C.B_DBG], q="pool")
                    g.dma(C.DBG[:, 3512:4024], kT[:, 0, 0:512], R=[kT.b[0]], W=[C.B_DBG], q="pool")
                    g.dma(C.DBG[:, 4024:4536], ktm[:, 0, 0:8, :].rearrange("p c d -> p (c d)"), R=[ktm.b[0]], W=[C.B_DBG], q="pool")
                    g.dma(C.DBG[:, 4536:5048], vtm[:, 0, 0:8, :].rearrange("p c d -> p (c d)"), R=[vtm.b[0]], W=[C.B_DBG], q="pool")
                    g.dma(C.DBG[:, 5048:5560], Oacc[:, 0, 0:8, :].rearrange("p c d -> p (c d)"), R=[Oacc.b[0]], W=[C.B_DBG], q="pool")
                    g.dma(C.DBG[:, 5560:6072], Oacc[:, 0, 60:68, :].rearrange("p c d -> p (c d)"), R=[Oacc.b[0]], W=[C.B_DBG], q="pool")
                    g.dma(C.DBG[:, 6072:6584], sgt[:, 0, 0:8, :].rearrange("p c d -> p (c d)"), R=[sgt.b[0]], W=[C.B_DBG], q="pool")
                with SS() as s3:
                    osq = g.sb("gosq", [64, 2 * NCH, 64], F32, stack=s3)
                    rs = g.sb("grs", [64, 2 * NCH], F32, stack=s3)
                    ob = vtm
                    OB = [Oacc.b[0], Oacc.b[1]]
                    Ov = Oacc[:].rearrange("p h c d -> p (h c) d")
                    g.tt(osq[:], Ov, Ov, ALU.mult, OB, [osq.b[0]], eng="pool")
                    g.K.op("dve", lambda h_: h_.tensor_reduce(out=rs[:], in_=osq[:], axis=AX.X, op=ALU.add), reads=[osq.b[0]],
                           writes=[rs.b[0]])
                    g.act(rs[:], rs[:], AF.Sqrt, [rs.b[0]], [rs.b[0]], bias=eps64, scale=1.0 / 64)
                    g.recip(rs[:], rs[:], [rs.b[0]], [rs.b[0]])
                    g.tt(osq[:], Ov, rs[:].unsqueeze(2).to_broadcast([64, 2 * NCH, 64]), ALU.mult, OB + [rs.b[0]], [osq.b[0]])
                    g.tt(osq[:], osq[:], ngb[:].unsqueeze(1).to_broadcast([64, 2 * NCH, 64]), ALU.mult, [osq.b[0], ngb.b[0]],
                         [osq.b[0]])
                    g.tt(ob[:].rearrange("p h c d -> p (h c) d"), osq[:], sgt[:].rearrange("p h c d -> p (h c) d"), ALU.mult,
                         [osq.b[0], sgt.b[0]], [ob.b[0], ob.b[1]])
                    for hh in range(2):
                        h = 2 * hp + hh
                        g.dma(C.O.rearrange("(c p) n -> p c n", p=64)[:, :, 256 + h * 64:256 + (h + 1) * 64], ob[:, hh],
                              R=[ob.b[0], ob.b[1]], W=C.B_O)


def phase_wout(C, l, need_ctx):
    g, I = C.g, C.I
    xsrc = I["xin"] if l == 0 else C.XS
    with SS() as s1:
        wout = load_w_bf16(C, "w_out", l, 0, D, "wout", s1)
        Abc, Sbc = norm_mod_tiles(C, l, 2, s1)
        G1 = g.sb("G1bc", [128, 2, D], F32, stack=s1)
        for r in range(2):
            g.dma(G1[:, r, :], C.MOD[l, r:r + 1, 2 * D:3 * D].partition_broadcast(128), R=[C.B_MOD[l]], W=[G1.b[0]])
        T = nm_scratch(C, s1)
        xt = g.sb("xt", [128, 2, D], F32, nslots=2, stack=s1)
        ot = g.sb("ot", [128, 2, D], BF16, nslots=2, stack=s1)
        oT = g.sb("oT", [128, 2, D], BF16, nslots=2, stack=s1)
        tmp = g.sb("wtmp", [128, D], F32, stack=s1)
        for t in range(NT):
            if t < 2 and not need_ctx:
                continue
            sl = t % 2
            r = 1 if t < 2 else 0
            g.dma(xt[:, sl], xsrc[t * 128:(t + 1) * 128, :], R=[C.B_XS[t]] if l > 0 else [], W=[xt.b[sl]])
            g.dma(ot[:, sl], C.O[t * 128:(t + 1) * 128, :], R=[C.B_O[t]], W=[ot.b[sl]])
            p = g.ps()
            pb = p[:].bitcast(BF16)
            for k in range(8):
                g.tr(pb[:, k * 128:(k + 1) * 128], ot[:, sl, k * 128:(k + 1) * 128], C.ident[:],
                     [ot.b[sl], C.ident.b[0]], [p.b[0]])
            g.cp(oT[:, sl], pb, [p.b[0]], [oT.b[sl]], eng="act")
            for nh in range(2):
                p = g.ps()
                for k in range(8):
                    g.mm(p[:, :], oT[:, sl, k * 128:(k + 1) * 128], wout[:, k, nh * 512:(nh + 1) * 512], k == 0, k == 7,
                         [oT.b[sl], wout.b[0]], [p.b[0]])
                g.tt(tmp[:, nh * 512:(nh + 1) * 512], p[:, :], G1[:, r, nh * 512:(nh + 1) * 512], ALU.mult,
                     [p.b[0], G1.b[0]], [tmp.b[0]], eng="dve")
            g.tt(xt[:, sl], xt[:, sl], tmp[:], ALU.add, [xt.b[sl], tmp.b[0]], [xt.b[sl]], eng="pool")
            g.dma(C.XS[t * 128:(t + 1) * 128, :], xt[:, sl], R=[xt.b[sl]], W=[C.B_XS[t]])
            norm_mod_transpose(C, xt[:, sl], xt.b[sl], r, Abc, Sbc, T, sl, C.H2T[t], C.B_H2T[t])


def phase_ffn(C, l, need_ctx, last):
    g, I = C.g, C.I
    moe = (l % 2 == 1)
    j = l // 2
    E = NE if moe else 1
    if moe:
        W1 = lambda e: I["moe_w1"][j, e]
        W3 = lambda e: I["moe_w3"][j, e]
        W2 = lambda e: I["moe_w2"][j, e]
    else:
        W1 = lambda e: I["ffn_w1"][j]
        W3 = lambda e: I["ffn_w3"][j]
        W2 = lambda e: I["ffn_w2"][j]
    tiles = [t for t in range(NT) if t >= 2 or need_ctx]
    groups = []
    if need_ctx:
        groups.append([0, 1])
    for i in range(8):
        groups.append([2 + 4 * i + q for q in range(4)])
    with SS() as s1:
        G2 = g.sb("G2bc", [128, 2, D], F32, stack=s1)
        for r in range(2):
            g.dma(G2[:, r, :], C.MOD[l, r:r + 1, 5 * D:6 * D].partition_broadcast(128), R=[C.B_MOD[l]], W=[G2.b[0]])
        if last:
            fg = g.sb("fgbc", [128, D], F32, stack=s1)
            g.dma(fg[:], I["final_g"].rearrange("(o d) -> o d", o=1).partition_broadcast(128), W=[fg.b[0]])
        if moe:
            wr = g.sb("wrouter", [128, 8, NE], BF16, stack=s1)
            g.dma(wr[:], I["moe_router"][j].rearrange("(k p) e -> p k e", p=128), W=[wr.b[0]], q="pool")
        h2 = g.sb("h2", [128, 2, 8, 512], BF16, nslots=2, stack=s1)
        aT = g.sb("aT", [128, 22, 512], BF16, nslots=22, stack=s1)
        Y = g.sb("Yacc", [128, 4, D], F32, nslots=4, stack=s1)
        w13 = g.sb("w13", [128, 3, 2, 8, 256], BF16, nslots=3, stack=s1)
        w2 = g.sb("w2", [128, 4, 11, D], BF16, nslots=4, stack=s1)
        sil = g.sb("sil", [128, 2, 512], F32, nslots=2, stack=s1)
        gates = g.sb("gates", [128, 4, NE], F32, nslots=4, stack=s1)
        rt = g.sb("rt", [128, 8, NE], F32, stack=s1)
        rc = g.sb("rc", [128, 8], F32, stack=s1)
        xt = g.sb("xt", [128, 2, D], F32, nslots=2, stack=s1)
        ft = g.sb("ftmp", [128, D], F32, stack=s1)
        fs = g.sb("fssq", [128, 2], F32, nslots=2, stack=s1)
        w13_it = 0
        w2_it = 0
        for gi_, tl in enumerate(groups):
            hs = gi_ % 2
            n = 128 * len(tl)
            for q, t in enumerate(tl):
                g.dma(h2[:, hs, :, q * 128:(q + 1) * 128], C.H2T[t].rearrange("p (k c) -> p k c", k=8), R=[C.B_H2T[t]],
                      W=[h2.b[hs]])
            for q, t in enumerate(tl):
                if not moe:
                    g.memset(gates[:, q, :], 1.0, [gates.b[q]])
                    continue
                p = g.ps()
                for k in range(8):
                    g.mm(p[:, 0:NE], h2[:, hs, k, q * 128:(q + 1) * 128], wr[:, k, :], k == 0, k == 7, [h2.b[hs], wr.b[0]],
                         [p.b[0]])
                RT, RC = [rt.b[0]], [rc.b[0]]
                lg, eq1, msk, eq2 = rt[:, 0, :], rt[:, 1, :], rt[:, 2, :], rt[:, 3, :]
                g.cp(lg, p[:, 0:NE], [p.b[0]], RT, eng="dve")
                red = lambda o_, i_: g.K.op("dve", lambda h_: h_.tensor_reduce(out=o_, in_=i_, axis=AX.X, op=ALU.max),
                                            reads=RT + RC, writes=RC)
                red(rc[:, 0:1], lg)
                g.ts(eq1, lg, rc[:, 0:1], ALU.is_equal, RT + RC, RT)
                g.stt(msk, eq1, -1e30, lg, ALU.mult, ALU.add, RT, RT)
                red(rc[:, 1:2], msk)
                g.ts(eq2, msk, rc[:, 1:2], ALU.is_equal, RT + RC, RT)
                g.tt(rc[:, 2:3], rc[:, 1:2], rc[:, 0:1], ALU.subtract, RC, RC)
                g.act(rc[:, 3:4], rc[:, 2:3], AF.Exp, RC, RC)
                g.ts(rc[:, 4:5], rc[:, 3:4], 1.0, ALU.add, RC, RC)
                g.recip(rc[:, 4:5], rc[:, 4:5], RC, RC)
                g.tt(rc[:, 5:6], rc[:, 3:4], rc[:, 4:5], ALU.mult, RC, RC)
                g.ts(eq1, eq1, rc[:, 4:5], ALU.mult, RT + RC, RT)
                g.stt(gates[:, q, :], eq2, rc[:, 5:6], eq1, ALU.mult, ALU.add, RT + RC, [gates.b[q]])
            for e in range(E):
                w1v = W1(e).rearrange("(k p) n -> p k n", p=128)
                w3v = W3(e).rearrange("(k p) n -> p k n", p=128)
                w2v = W2(e).rearrange("(c p) n -> p c n", p=128)
                w2s = []
                for hf_ in range(2):
                    ws = w2_it % 4
                    w2_it += 1
                    g.dma(w2[:, ws], w2v[:, hf_ * 11:(hf_ + 1) * 11, :], W=[w2.b[ws]], q="pool")
                    w2s.append(ws)
                for cb in range(11):
                    wsl = w13_it % 3
                    w13_it += 1
                    g.dma(w13[:, wsl, 0], w1v[:, :, cb * 256:(cb + 1) * 256], W=[w13.b[wsl]], q="pool")
                    g.dma(w13[:, wsl, 1], w3v[:, :, cb * 256:(cb + 1) * 256], W=[w13.b[wsl]], q="pool")
                    for c2 in range(2):
                        c = 2 * cb + c2
                        ss = c % 2
                        p1, p3 = g.ps(), g.ps()
                        for (pp, wi) in ((p1, 0), (p3, 1)):
                            for k in range(8):
                                g.mm(pp[:, 0:n], w13[:, wsl, wi, k, c2 * 128:(c2 + 1) * 128], h2[:, hs, k, 0:n], k == 0, k == 7,
                                     [w13.b[wsl], h2.b[hs]], [pp.b[0]])
                        g.act(sil[:, ss, 0:n], p1[:, 0:n], AF.Silu, [p1.b[0]], [sil.b[ss]])
                        g.tt(aT[:, c, 0:n], p3[:, 0:n], sil[:, ss, 0:n], ALU.mult, [p3.b[0], sil.b[ss]], [aT.b[c]], eng="dve")
                for q, t in enumerate(tl):
                    for nh in range(2):
                        p = g.ps()
                        for c in range(22):
                            g.mm(p[:, :], aT[:, c, q * 128:(q + 1) * 128], w2[:, w2s[c // 11], c % 11, nh * 512:(nh + 1) * 512],
                                 c == 0, c == 21, [aT.b[c], w2.b[w2s[c // 11]]], [p.b[0]])
                        yv = Y[:, q, nh * 512:(nh + 1) * 512]
                        if e == 0:
                            g.ts(yv, p[:, :], gates[:, q, e:e + 1], ALU.mult, [p.b[0], gates.b[q]], [Y.b[q]])
                        else:
                            g.stt(yv, p[:, :], gates[:, q, e:e + 1], yv, ALU.mult, ALU.add, [p.b[0], gates.b[q], Y.b[q]], [Y.b[q]])
            for q, t in enumerate(tl):
                sl = t % 2
                r = 1 if t < 2 else 0
                g.dma(xt[:, sl], C.XS[t * 128:(t + 1) * 128, :], R=[C.B_XS[t]], W=[xt.b[sl]])
                g.tt(Y[:, q, :], Y[:, q, :], G2[:, r, :], ALU.mult, [Y.b[q], G2.b[0]], [Y.b[q]], eng="pool")
                g.tt(xt[:, sl], xt[:, sl], Y[:, q, :], ALU.add, [xt.b[sl], Y.b[q]], [xt.b[sl]], eng="pool")
                if not last:
                    g.dma(C.XS[t * 128:(t + 1) * 128, :], xt[:, sl], R=[xt.b[sl]], W=[C.B_XS[t]])
                else:
                    g.act(ft[:], xt[:, sl], AF.Square, [xt.b[sl]], [ft.b[0], fs.b[sl]], accum_out=fs[:, sl:sl + 1])
                    g.act(fs[:, sl:sl + 1], fs[:, sl:sl + 1], AF.Sqrt, [fs.b[sl]], [fs.b[sl]], bias=C.epsc[:, 0:1], scale=1.0 / D)
                    g.recip(fs[:, sl:sl + 1], fs[:, sl:sl + 1], [fs.b[sl]], [fs.b[sl]])
                    g.stt(xt[:, sl], xt[:, sl], fs[:, sl:sl + 1], fg[:], ALU.mult, ALU.mult, [xt.b[sl], fs.b[sl], fg.b[0]],
                          [xt.b[sl]])
                    C.out_tokens.append(g.dma(C.out[(t - 2) * 128:(t - 1) * 128, :], xt[:, sl], R=[xt.b[sl]], W=[C.B_OUT[t]]))


def phase_gdnzero(C, l):
    g = C.g
    with SS() as s1:
        z = g.sb("gz", [128, 256], BF16, stack=s1)
        g.memset(z[:], 0.0, [z.b[0]])
        for t in range(NT):
            g.dma(C.O[t * 128:(t + 1) * 128, 256:512], z[:], R=[z.b[0]], W=[C.B_O[t]])


def build_program(dbg=None, nlayers=DEPTH, phases=("norm1", "ret", "gdn", "na", "wout", "ffn")):
    nc = bass.Bass("TRN2", target_bir_lowering=False)
    C = Ctx()
    I = C.I = {}

    def inp(name, shape, dt=F32):
        I[name] = nc.dram_tensor(name, list(shape), dt, kind="ExternalInput").ap()

    def scratch(name, shape, dt):
        return nc.dram_tensor(name, list(shape), dt, kind="ExternalOutput" if (dbg and name in dbg.split("+")) else "Internal").ap()

    inp("xin", [NTOK, D])
    inp("cvec", [2, D])
    inp("ada_w", [DEPTH, D, 6 * D])
    inp("ada_b", [DEPTH, 6 * D])
    inp("norm1_g", [DEPTH, D])
    inp("norm2_g", [DEPTH, D])
    inp("w_in", [DEPTH, D, D_IN])
    inp("ret_decay", [DEPTH, 2, 4])
    inp("ret_const", [128, 6, 128])
    inp("ret_pcol", [128, 2, 64])
    inp("rope", [SEQ, 2, 256])
    inp("w_out", [DEPTH, D, D])
    inp("ffn_w1", [1, D, D_FF])
    inp("ffn_w3", [1, D, D_FF])
    inp("ffn_w2", [1, D_FF, D])
    inp("moe_router", [1, D, NE])
    inp("moe_w1", [1, NE, D, D_FF])
    inp("moe_w3", [1, NE, D, D_FF])
    inp("moe_w2", [1, NE, D_FF, D])
    inp("final_g", [D])
    inp("gdn_const", [64, 13, 64])
    inp("gdn_bd", [128, 128])
    inp("gdn_a_log", [DEPTH, 2, 4])
    inp("gdn_dt_bias", [DEPTH, 2, 4])
    inp("gdn_norm_g", [DEPTH, 64])
    inp("gdn_convw", [DEPTH, 4, 64, 3, 5])
    inp("na_mask", [128, 5, 640])
    inp("na_bias", [DEPTH, 8, 128, 5, 640])
    C.out = nc.dram_tensor("out", [SEQ, D], F32, kind="ExternalOutput").ap()
    C.out_tokens = []
    C.B_OUT = [Buf() for _ in range(NT)]
    C.MOD = scratch("MOD", [DEPTH, 2, 6 * D], F32)
    C.HT = scratch("HT", [NT, 128, D], BF16)
    C.XS = scratch("XS", [NTOK, D], F32)
    C.O = scratch("O", [NTOK, D], BF16)
    C.H2T = scratch("H2T", [NT, 128, D], BF16)
    C.B_H2T = [Buf() for _ in range(NT)]
    C.DBG = scratch("DBG", [64, 8192], F32) if (dbg and "DBG" in dbg) else None
    C.B_DBG = Buf()
    C.B_MOD = [Buf() for _ in range(DEPTH)]
    C.B_HT = [Buf() for _ in range(NT)]
    C.B_XS = [Buf() for _ in range(NT)]
    C.B_O = [Buf() for _ in range(NT)]

    with ExitStack() as st:
        g = C.g = Gen(nc, st)
        K = g.K
        _CUR["K"] = K
        ident = C.ident = g.sb("ident", [128, 128], BF16)
        g.memset(ident[:], 1.0, [ident.b[0]])
        K.op("pool", lambda h: h.affine_select(out=ident[:], in_=ident[:], pattern=[[-1, 128]],
                                                compare_op=ALU.is_equal, fill=0.0, base=0, channel_multiplier=1),
             reads=[ident.b[0]], writes=[ident.b[0]])
        C.epsc = g.sb("epsc", [128, 1], F32)
        g.memset(C.epsc[:], EPS, [C.epsc.b[0]])
        C.onec = g.sb("onec", [128, 1], F32)
        g.memset(C.onec[:], 1.0, [C.onec.b[0]])

        phase_adaln(C, nlayers)
        for l in range(nlayers):
            need_ctx = l < DEPTH - 1
            if "norm1" in phases:
                phase_norm1(C, l)
            if "ret" in phases:
                phase_ret(C, l, need_ctx)
            if "gdn" in phases:
                phase_gdn(C, l, need_ctx)
            if "na" in phases:
                phase_na(C, l, need_ctx)
            if "gdnzero" in phases:
                phase_gdnzero(C, l)
            if "wout" in phases:
                phase_wout(C, l, need_ctx)
            if "ffn" in phases:
                phase_ffn(C, l, need_ctx, l == nlayers - 1 and nlayers == DEPTH)

        fin = [b.last_w for b in C.B_HT + C.B_O + C.B_XS + C.B_H2T + [C.B_DBG] if b.last_w is not None] + C.out_tokens
        K.finish(fin)
        K.emit()
    return nc


_NC_CACHE = {}
NA_M_REP = [0, 1, 2, 30, 31]


def na_cls(m):
    return {0: 0, 1: 1, 30: 3, 31: 4}.get(m, 2)


def na_kp0(m):
    return min(max(m - 2, 0), 27)


def _na_index_tables():
    kk = np.arange(128)[:, None, None]
    j = np.arange(5)[None, :, None]
    qq = np.arange(128)[None, None, :]
    ki, kc = kk // 64, kk % 64
    qi, qc = qq // 64, qq % 64
    ridx = np.zeros((5, 128, 5, 128), np.int64)
    cidx = np.zeros((5, 128, 5, 128), np.int64)
    valid = np.zeros((5, 128, 5, 128), bool)
    for c, m in enumerate(NA_M_REP):
        kr = 2 * (na_kp0(m) + j) + ki
        r = 2 * m + qi
        r0 = np.clip(r - 4, 0, 56)
        ws = np.clip(qc - 8, 0, 48)
        v = (kr >= r0) & (kr < r0 + 8) & (kc >= ws) & (kc < ws + 16)
        valid[c] = np.broadcast_to(v, (128, 5, 128))
        ridx[c] = np.broadcast_to(np.clip(kr - r + 7, 0, 14), (128, 5, 128))
        cidx[c] = np.broadcast_to(np.clip(kc - qc + 15, 0, 30), (128, 5, 128))
    return ridx, cidx, valid


def _const_inputs():
    if "consts" in _NC_CACHE:
        return _NC_CACHE["consts"]
    ridx, cidx, valid = _na_index_tables()
    ii = np.arange(128)[None, :].astype(np.float64)
    jj = np.arange(128)[:, None].astype(np.float64)
    ret_const = np.stack([np.maximum(ii - jj, 0) + 0 * jj, np.maximum(jj - ii, 0) + 0 * ii, (ii >= jj) * 1.0, (jj >= ii) * 1.0,
                          (ii + 1) + 0 * jj, (128 - ii) + 0 * jj], axis=1).astype(np.float32)
    ret_pcol = np.stack([np.broadcast_to(127 - jj, (128, 64)), np.broadcast_to(jj, (128, 64))], axis=1).astype(np.float32)
    tpos = np.arange(SEQ)
    inv_freq = 10000.0 ** (-np.arange(16, dtype=np.float32) / 16)
    ang = np.concatenate([(tpos // 64).astype(np.float32)[:, None] * inv_freq, (tpos % 64).astype(np.float32)[:, None] * inv_freq],
                         axis=-1).astype(np.float32)
    rope = np.stack([np.tile(np.cos(ang), (1, 8)), np.tile(np.sin(ang), (1, 8))], axis=1).astype(np.float32)
    mm_, i_ = np.arange(64)[:, None], np.arange(64)[None, :]
    Yf, Yb, SUf, SUb = (mm_ <= i_) * 1.0, (mm_ >= i_) * 1.0, (mm_ > i_) * 1.0, (mm_ < i_) * 1.0
    gdn_const = np.stack([Yf, Yb, Yf, Yb, SUf, SUb, SUf, SUb] + [np.eye(64)] * 4 + [np.ones((64, 64))], axis=1).astype(np.float32)
    pp_ = np.arange(128)
    gdn_bd = ((pp_[:, None] // 64) == (pp_[None, :] // 64)).astype(np.float32)
    c = {"gdn_const": np.ascontiguousarray(gdn_const), "gdn_bd": gdn_bd, "ret_const": np.ascontiguousarray(ret_const), "ret_pcol": np.ascontiguousarray(ret_pcol), "rope": rope,
         "na_mask": np.where(valid, 0.0, -1e30).astype(np.float32).transpose(1, 0, 2, 3).reshape(128, 5, 640).copy(),
         "_na_ridx": ridx, "_na_cidx": cidx}
    _NC_CACHE["consts"] = c
    return c


def _prep_core_inputs(b, inputs):
    xin = np.ascontiguousarray(np.concatenate([inputs["ctx"][b], inputs["x"][b]], axis=0))
    cvec = np.ascontiguousarray(np.stack([inputs["c"][b], inputs["c_ctx"]], axis=0))
    m = {"xin": xin, "cvec": cvec}
    for k in ("ada_w", "ada_b", "norm1_g", "norm2_g", "w_in", "w_out", "ffn_w1", "ffn_w3", "ffn_w2", "moe_router", "moe_w1",
              "moe_w3", "moe_w2", "final_g"):
        m[k] = np.ascontiguousarray(inputs[k])
    c = _const_inputs()
    for k in ("gdn_a_log", "gdn_dt_bias", "gdn_norm_g"):
        m[k] = np.ascontiguousarray(inputs[k])
    m["gdn_convw"] = np.ascontiguousarray(inputs["conv_w"].reshape(DEPTH, 5, 3, 4, 64).transpose(0, 3, 4, 2, 1))
    for k in ("na_mask", "ret_const", "ret_pcol", "rope", "gdn_const", "gdn_bd"):
        m[k] = c[k]
    m["ret_decay"] = np.ascontiguousarray(inputs["ret_decay"])
    rp = inputs["na_rpb"]
    gat = rp[:, :, c["_na_ridx"], c["_na_cidx"]]
    m["na_bias"] = np.ascontiguousarray(gat.transpose(0, 1, 3, 2, 4, 5).reshape(DEPTH, 8, 128, 5, 640))
    return m


def kernel(**inputs):
    inputs = {k: np.asarray(v) for k, v in inputs.items()}
    if "nc" not in _NC_CACHE:
        _NC_CACHE["nc"] = build_program()
    nc = _NC_CACHE["nc"]
    in_maps = [_prep_core_inputs(b, inputs) for b in range(8)]
    res = run_bass_kernel_spmd(nc, in_maps, core_ids=list(range(8)))
    return np.stack([r["out"] for r in res.results], axis=0)
```

```python
import os
import numpy as np
import ml_dtypes
import concourse.bass as bass
import concourse.mybir as mybir
from contextlib import ExitStack
from concourse.bass_utils import run_bass_kernel_spmd

F32 = mybir.dt.float32
BF16 = mybir.dt.bfloat16
F32R = mybir.dt.float32r
ALU = mybir.AluOpType
AF = mybir.ActivationFunctionType
AX = mybir.AxisListType

EPOCH = 16000
N_DMA_SEMS = 40

D = 1024
SEQ = 4096
CTX = 256
NTOK = SEQ + CTX
NT = NTOK // 128
DEPTH = 2
D_IN = 3600
D_FF = 2816
NE = 8
EPS = 1e-6


class Buf:
    __slots__ = ("name", "last_w", "reads", "excl")

    def __init__(self, name="", excl=False):
        self.name = name
        self.last_w = None
        self.reads = []
        self.excl = excl


class Eng:
    def __init__(self, name):
        self.name = name
        self.ops = []
        self.n = 0
        self.sems = []
        self.waited = {}


class Ker:
    def __init__(self, nc, stack):
        self.nc = nc
        self.stack = stack
        self.eng = {n: Eng(n) for n in ("pe", "act", "dve", "pool", "sp")}
        self.dma_sems = []
        self.dma_sem_val = []
        self.dma_rr = 0
        for i in range(N_DMA_SEMS):
            s = stack.enter_context(nc.semaphore(f"dq{i}"))
            self.dma_sems.append(s)
            self.dma_sem_val.append(0)

    def _need(self, e, waits, tok):
        if tok is None:
            return
        sem, val, key = tok[0], tok[1], tok[2]
        if e.waited.get(key, 0) >= val:
            return
        e.waited[key] = val
        waits.append((sem, val))

    def _deps(self, e, reads, writes, pe_acc=False):
        waits = []
        for b in reads:
            self._need(e, waits, b.last_w)
            if b.excl:
                for t in b.reads:
                    if t[3] != e.name:
                        self._need(e, waits, t)
        for b in writes:
            if not (pe_acc and b.last_w is not None and b.last_w[3] == "pe"):
                self._need(e, waits, b.last_w)
            for t in b.reads:
                self._need(e, waits, t)
        return waits

    def _commit(self, tok, reads, writes):
        for b in reads:
            b.reads.append(tok)
            if len(b.reads) > 12:
                d = {}
                for t in b.reads:
                    if t[2] not in d or d[t[2]][1] < t[1]:
                        d[t[2]] = t
                b.reads = list(d.values())
        for b in writes:
            b.last_w = tok
            b.reads = []

    def op(self, engname, fn, reads=(), writes=(), pe_acc=False):
        e = self.eng[engname]
        waits = self._deps(e, reads, writes, pe_acc)
        ep = e.n // EPOCH
        while len(e.sems) <= ep:
            e.sems.append(self.stack.enter_context(self.nc.semaphore(f"s_{engname}{len(e.sems)}")))
        sem = e.sems[ep]
        val = e.n % EPOCH + 1
        tok = (sem, val, (engname, ep), engname)
        e.n += 1
        e.ops.append((waits, fn, (sem, 1)))
        self._commit(tok, reads, writes)
        return tok

    def dma(self, qname, out_ap, in_ap, reads=(), writes=(), **kw):
        e = self.eng[qname]
        waits = self._deps(e, reads, writes)
        i = self.dma_rr
        self.dma_rr = (self.dma_rr + 1) % N_DMA_SEMS
        sem = self.dma_sems[i]
        if self.dma_sem_val[i] > 0:
            self._need(e, waits, (sem, self.dma_sem_val[i], ("dq", i), "dma"))
        self.dma_sem_val[i] += 16
        val = self.dma_sem_val[i]
        tok = (sem, val, ("dq", i), "dma")

        def fn(h, out_ap=out_ap, in_ap=in_ap, kw=kw):
            return h.dma_start(out=out_ap, in_=in_ap, **kw)
        e.ops.append((waits, fn, (sem, 16)))
        self._commit(tok, reads, writes)
        return tok

    def barrier(self):
        toks = []
        for n, e in self.eng.items():
            if e.n > 0:
                ep = (e.n - 1) // EPOCH
                toks.append((e.sems[ep], (e.n - 1) % EPOCH + 1, (n, ep), n))
        for i in range(N_DMA_SEMS):
            if self.dma_sem_val[i] > 0:
                toks.append((self.dma_sems[i], self.dma_sem_val[i], ("dq", i), "dma"))
        for n, e in self.eng.items():
            waits = []
            for t in toks:
                self._need(e, waits, t)
            e.ops.append((waits, None, None))

    def finish(self, final_tokens):
        e = self.eng["sp"]
        waits = []
        for t in final_tokens:
            self._need(e, waits, t)
        e.ops.append((waits, None, None))

    def emit(self):
        nc = self.nc
        with nc.Block() as block:
            def run(e):
                def body(h):
                    for waits, fn, inc in e.ops:
                        for (sem, val) in waits:
                            h.wait_ge(sem, val)
                        if fn is not None:
                            ins = fn(h)
                            if inc is not None:
                                ins.then_inc(inc[0], inc[1])
                return body
            block.tensor(run(self.eng["pe"]))
            block.scalar(run(self.eng["act"]))
            block.vector(run(self.eng["dve"]))
            block.gpsimd(run(self.eng["pool"]))
            block.sync(run(self.eng["sp"]))


_CUR = {"K": None}


class SS(ExitStack):
    def __exit__(self, *a):
        if _CUR["K"] is not None and a[0] is None:
            _CUR["K"].barrier()
        return super().__exit__(*a)


class Tile:
    def __init__(self, t, nslots=1):
        self.t = t
        self.b = [Buf() for _ in range(nslots)]

    def __getitem__(self, k):
        return self.t[k]


class Gen:
    def __init__(self, nc, stack):
        self.nc = nc
        self.st = stack
        self.K = Ker(nc, stack)
        self.psum = []
        self.ps_rr = 0
        for i in range(8):
            t = stack.enter_context(nc.psum_tensor(f"ps{i}", [128, 512], F32))
            tl = Tile(t)
            tl.b[0].excl = True
            self.psum.append(tl)

    def ps(self):
        p = self.psum[self.ps_rr]
        self.ps_rr = (self.ps_rr + 1) % 8
        return p

    def sb(self, name, shape, dt, nslots=1, stack=None):
        self.uid = getattr(self, "uid", 0) + 1
        t = (stack or self.st).enter_context(self.nc.sbuf_tensor(f"{name}_{self.uid}", shape, dt))
        return Tile(t, nslots)

    def mm(self, out, lhsT, rhs, start, stop, R, W):
        return self.K.op("pe", lambda h: h.matmul(out, lhsT, rhs, start=start, stop=stop), reads=R, writes=W,
                         pe_acc=not start)

    def tr(self, out, in_, ident, R, W):
        return self.K.op("pe", lambda h: h.transpose(out, in_, ident), reads=R, writes=W, pe_acc=True)

    def act(self, out, in_, func, R, W, bias=None, scale=1.0, accum_out=None, eng="act"):
        kw = {}
        if bias is not None:
            kw["bias"] = bias
        if accum_out is not None:
            kw["accum_out"] = accum_out
        return self.K.op(eng, lambda h: h.activation(out=out, in_=in_, func=func, scale=scale, **kw), reads=R, writes=W)

    def tt(self, out, in0, in1, op, R, W, eng="dve"):
        return self.K.op(eng, lambda h: h.tensor_tensor(out=out, in0=in0, in1=in1, op=op), reads=R, writes=W)

    def ts(self, out, in0, s1, op0, R, W, s2=None, op1=None, eng="dve", accum_out=None):
        kw = {}
        if op1 is not None:
            kw["op1"] = op1
        if accum_out is not None:
            kw["accum_out"] = accum_out
        return self.K.op(eng, lambda h: h.tensor_scalar(out=out, in0=in0, scalar1=s1, scalar2=s2, op0=op0, **kw),
                         reads=R, writes=W)

    def stt(self, out, in0, scalar, in1, op0, op1, R, W, eng="dve"):
        return self.K.op(eng, lambda h: h.scalar_tensor_tensor(out=out, in0=in0, scalar=scalar, in1=in1, op0=op0, op1=op1),
                         reads=R, writes=W)

    def cp(self, out, in_, R, W, eng="dve"):
        if eng == "act":
            return self.K.op("act", lambda h: h.copy(out=out, in_=in_), reads=R, writes=W)
        return self.K.op(eng, lambda h: h.tensor_copy(out=out, in_=in_), reads=R, writes=W)

    def memset(self, ap, val, W, eng="pool"):
        return self.K.op(eng, lambda h: h.memset(ap, val), writes=W)

    def recip(self, out, in_, R, W):
        return self.K.op("dve", lambda h: h.reciprocal(out=out, in_=in_), reads=R, writes=W)

    def dma(self, out, in_, R=(), W=(), q="sp", **kw):
        return self.K.dma(q, out, in_, reads=R, writes=W, **kw)


class Ctx:
    pass


def phase_adaln(C, nlayers):
    g, I = C.g, C.I
    with SS() as s1:
        craw = g.sb("craw", [128, 2, 8], F32, stack=s1)
        for r in range(2):
            g.dma(craw[:, r, :], I["cvec"][r].rearrange("(p k) -> p k", k=8), W=[craw.b[0]])
        scT = g.sb("scT", [128, 8, 2], F32, stack=s1)
        g.act(scT[:].rearrange("p k r -> p r k"), craw[:], AF.Silu, [craw.b[0]], [scT.b[0]])
        adab = g.sb("adab", [2, 6 * D], F32, stack=s1)
        modsb = g.sb("modsb", [2, 6 * D], F32, stack=s1)
        wch = g.sb("wch", [128, 2, 8, 512], F32, nslots=2, stack=s1)
        it = 0
        for l in range(nlayers):
            g.dma(adab[:], I["ada_b"][l:l + 1, :].partition_broadcast(2), W=[adab.b[0]])
            for n in range(12):
                sl = it % 2
                it += 1
                g.dma(wch[:, sl], I["ada_w"][l].rearrange("(p k) n -> p k n", k=8)[:, :, n * 512:(n + 1) * 512],
                      W=[wch.b[sl]])
                p = g.ps()
                for k in range(8):
                    g.mm(p[0:2, :], scT[:, k, :], wch[:, sl, k, :], k == 0, k == 7, [scT.b[0], wch.b[sl]], [p.b[0]])
                g.tt(modsb[:, n * 512:(n + 1) * 512], p[0:2, :], adab[:, n * 512:(n + 1) * 512], ALU.add,
                     [p.b[0], adab.b[0]], [modsb.b[0]])
            g.dma(C.MOD[l], modsb[:], R=[modsb.b[0]], W=[C.B_MOD[l]])


def norm_mod_tiles(C, l, which, s1):
    g, I = C.g, C.I
    so, co = (0, D) if which == 1 else (3 * D, 4 * D)
    gname = "norm1_g" if which == 1 else "norm2_g"
    Abc = g.sb("Abc", [128, 2, D], F32, stack=s1)
    Sbc = g.sb("Sbc", [128, 2, D], F32, stack=s1)
    gbc = g.sb("gbc", [128, D], F32, stack=s1)
    g.dma(gbc[:], I[gname][l:l + 1, :].partition_broadcast(128), W=[gbc.b[0]])
    for r in range(2):
        g.dma(Abc[:, r, :], C.MOD[l, r:r + 1, co:co + D].partition_broadcast(128), R=[C.B_MOD[l]], W=[Abc.b[0]])
        g.dma(Sbc[:, r, :], C.MOD[l, r:r + 1, so:so + D].partition_broadcast(128), R=[C.B_MOD[l]], W=[Sbc.b[0]])
        g.stt(Abc[:, r, :], Abc[:, r, :], 1.0, gbc[:], ALU.add, ALU.mult, [Abc.b[0], gbc.b[0]], [Abc.b[0]])
    return Abc, Sbc


def norm_mod_transpose(C, xt_ap, xt_b, r, Abc, Sbc, T, sl, dstHT, dstB):
    g = C.g
    sq, ssq, hf, hb, hT = T
    g.act(sq[:], xt_ap, AF.Square, [xt_b], [sq.b[0], ssq.b[sl]], accum_out=ssq[:, sl:sl + 1])
    g.act(ssq[:, sl:sl + 1], ssq[:, sl:sl + 1], AF.Sqrt, [ssq.b[sl]], [ssq.b[sl]], bias=C.epsc[:, 0:1], scale=1.0 / D)
    g.recip(ssq[:, sl:sl + 1], ssq[:, sl:sl + 1], [ssq.b[sl]], [ssq.b[sl]])
    g.stt(hf[:, sl], xt_ap, ssq[:, sl:sl + 1], Abc[:, r, :], ALU.mult, ALU.mult,
          [xt_b, ssq.b[sl], Abc.b[0]], [hf.b[sl]])
    g.tt(hb[:, sl], hf[:, sl], Sbc[:, r, :], ALU.add, [hf.b[sl], Sbc.b[0]], [hb.b[sl]], eng="pool")
    p = g.ps()
    pb = p[:].bitcast(BF16)
    for k in range(8):
        g.tr(pb[:, k * 128:(k + 1) * 128], hb[:, sl, k * 128:(k + 1) * 128], C.ident[:],
             [hb.b[sl], C.ident.b[0]], [p.b[0]])
    g.cp(hT[:, sl], pb, [p.b[0]], [hT.b[sl]], eng="act")
    g.dma(dstHT, hT[:, sl], R=[hT.b[sl]], W=[dstB])


def nm_scratch(C, s1):
    g = C.g
    sq = g.sb("sq", [128, D], F32, stack=s1)
    ssq = g.sb("ssq", [128, 2], F32, nslots=2, stack=s1)
    hf = g.sb("hf", [128, 2, D], F32, nslots=2, stack=s1)
    hb = g.sb("hb", [128, 2, D], BF16, nslots=2, stack=s1)
    hT = g.sb("hT", [128, 2, D], BF16, nslots=2, stack=s1)
    return (sq, ssq, hf, hb, hT)


def phase_norm1(C, l):
    g, I = C.g, C.I
    xsrc = I["xin"] if l == 0 else C.XS
    with SS() as s1:
        Abc, Sbc = norm_mod_tiles(C, l, 1, s1)
        xt = g.sb("xt", [128, 2, D], F32, nslots=2, stack=s1)
        T = nm_scratch(C, s1)
        for t in range(NT):
            sl = t % 2
            r = 1 if t < 2 else 0
            g.dma(xt[:, sl], xsrc[t * 128:(t + 1) * 128, :], R=[C.B_XS[t]] if l > 0 else [], W=[xt.b[sl]])
            norm_mod_transpose(C, xt[:, sl], xt.b[sl], r, Abc, Sbc, T, sl, C.HT[t], C.B_HT[t])


def load_ht_all(C, s1):
    g = C.g
    hts = g.sb("hts", [128, 8, NTOK], BF16, nslots=NT, stack=s1)
    for t in range(NT):
        g.dma(hts[:, :, t * 128:(t + 1) * 128], C.HT[t].rearrange("p (k c) -> p k c", k=8), R=[C.B_HT[t]],
              W=[hts.b[t]])
    return hts


def load_w_bf16(C, name, l, c0, c1, tname, s1, krows=8):
    g = C.g
    w = g.sb(tname, [128, krows, c1 - c0], BF16, stack=s1)
    src = C.I[name][l].rearrange("(k p) n -> p k n", p=128)
    for k0 in range(0, krows, 4):
        k1 = min(krows, k0 + 4)
        g.dma(w[:, k0:k1, :], src[:, k0:k1, c0:c1], W=[w.b[0]], q="pool")
    return w


def phase_na(C, l, need_ctx):
    g, I = C.g, C.I
    QC, KC, VC = 2064, 2576, 3088
    with SS() as s1:
        hts = load_ht_all(C, s1)
        wna = load_w_bf16(C, "w_in", l, QC, D_IN, "wna", s1)
        mask = g.sb("namask", [128, 5, 640], F32, stack=s1)
        g.dma(mask[:], I["na_mask"], W=[mask.b[0]])
        braw = g.sb("nabraw", [128, 5, 640], F32, stack=s1)
        bias = g.sb("nabias", [128, 2, 5, 640], BF16, nslots=2, stack=s1)
        qT = g.sb("naqT", [128, NTOK], BF16, nslots=9, stack=s1)
        kT = g.sb("nakT", [128, NTOK], BF16, nslots=9, stack=s1)
        vx = g.sb("navx", [128, NT, 2, 65], BF16, nslots=NT, stack=s1)
        pT = g.sb("napT", [128, 2, 7, 128], BF16, nslots=2, stack=s1)
        rden = g.sb("narden", [128, 2, 2], F32, nslots=2, stack=s1)
        ona = g.sb("naout", [128, 2, 128], BF16, nslots=2, stack=s1)
        g.memset(vx[:], 1.0, vx.b)
        groups = [(0, 256)] + [(256 + 512 * i, 512) for i in range(8)]
        unit = 0
        for hp in range(4):
            for h2 in range(2):
                h = 2 * hp + h2
                g.dma(braw[:], I["na_bias"][l, h], W=[braw.b[0]])
                g.tt(braw[:], braw[:], mask[:], ALU.add, [braw.b[0], mask.b[0]], [braw.b[0]], eng="pool")
                g.act(bias[:, h2], braw[:], AF.Copy, [braw.b[0]], [bias.b[h2]], scale=8.0)
            for gi, (t0, n) in enumerate(groups):
                tl = list(range(t0 // 128, (t0 + n) // 128))
                for (dst, c0) in ((qT, hp * 128), (kT, (KC - QC) + hp * 128)):
                    p = g.ps()
                    for k in range(8):
                        g.mm(p[:, 0:n], wna[:, k, c0:c0 + 128], hts[:, k, t0:t0 + n], k == 0, k == 7,
                             [wna.b[0]] + [hts.b[t] for t in tl], [p.b[0]])
                    g.cp(dst[:, t0:t0 + n], p[:, 0:n], [p.b[0]], [dst.b[gi]], eng="act" if dst is qT else "dve")
                p = g.ps()
                c0 = (VC - QC) + hp * 128
                for j, t in enumerate(tl):
                    for k in range(8):
                        g.mm(p[:, j * 128:(j + 1) * 128], hts[:, k, t * 128:(t + 1) * 128], wna[:, k, c0:c0 + 128],
                             k == 0, k == 7, [wna.b[0], hts.b[t]], [p.b[0]])
                g.cp(vx[:, tl[0]:tl[-1] + 1, :, 0:64],
                     p[:, 0:n // 128 * 128].rearrange("p (a b c) -> p a b c", b=2, c=64),
                     [p.b[0]], [vx.b[t] for t in tl], eng="dve")

            def tok_group(t):
                return 0 if t < 2 else 1 + (t - 2) // 4

            def attend(qt, local_tiles, cls, h2list=(0, 1)):
                nonlocal unit
                sl = unit % 2
                unit += 1
                po = g.ps()
                for h2 in h2list:
                    base = h2 * 64
                    keyt = list(local_tiles) + [0, 1]
                    nk = len(keyt)
                    banks = [g.ps(), g.ps()] if nk > 4 else [g.ps()]
                    for ci, kt in enumerate(keyt):
                        pb = banks[ci // 4]
                        reg = pb[:, (ci % 4) * 128:(ci % 4 + 1) * 128]
                        has_b = ci < len(local_tiles)
                        g.mm(reg, kT[base:base + 64, kt * 128:(kt + 1) * 128], qT[base:base + 64, qt * 128:(qt + 1) * 128],
                             True, not has_b, [kT.b[tok_group(kt)], qT.b[tok_group(qt)]], [pb.b[0]])
                        if has_b:
                            g.mm(reg, C.ident[:], bias[:, h2, cls, ci * 128:(ci + 1) * 128], False, True,
                                 [C.ident.b[0], bias.b[h2]], [pb.b[0]])
                    for bi, pb in enumerate(banks):
                        n = min(4, nk - 4 * bi) * 128
                        g.act(pT[:, sl, 4 * bi:4 * bi + n // 128, :].rearrange("p a b -> p (a b)") if False else
                              pT[:, sl].rearrange("p a b -> p (a b)")[:, bi * 512:bi * 512 + n],
                              pb[:, 0:n], AF.Exp, [pb.b[0]], [pT.b[sl]], scale=0.125)
                    for ci, kt in enumerate(keyt):
                        g.mm(po[:, h2 * 65:(h2 + 1) * 65], pT[:, sl, ci, :], vx[:, kt, h2, :], ci == 0, ci == nk - 1,
                             [pT.b[sl], vx.b[kt]], [po.b[0]])
                    g.recip(rden[:, sl, h2:h2 + 1], po[:, h2 * 65 + 64:h2 * 65 + 65], [po.b[0]], [rden.b[sl]])
                    g.ts(ona[:, sl, h2 * 64:(h2 + 1) * 64], po[:, h2 * 65:h2 * 65 + 64], rden[:, sl, h2:h2 + 1], ALU.mult,
                         [po.b[0], rden.b[sl]], [ona.b[sl]])
                g.dma(C.O[qt * 128:(qt + 1) * 128, 512 + hp * 128:512 + (hp + 1) * 128], ona[:, sl, :],
                      R=[ona.b[sl]], W=[C.B_O[qt]])

            for m in range(32):
                kp0 = na_kp0(m)
                attend(2 + m, [2 + kp0 + j for j in range(5)], na_cls(m))
            if need_ctx:
                for qt in range(2):
                    attend(qt, [], 0)


def silu_from(g, out, outW, x, xR, tmp, tmpB):
    g.act(tmp, x, AF.Exp, xR, [tmpB], scale=-1.0)
    g.ts(tmp, tmp, 1.0, ALU.add, [tmpB], [tmpB])
    g.recip(tmp, tmp, [tmpB], [tmpB])
    g.tt(out, x, tmp, ALU.mult, list(xR) + [tmpB], outW)


def phase_ret(C, l, need_ctx):
    g, I = C.g, C.I
    with SS() as s1:
        lgb = g.sb("lgb", [128, 8], F32, stack=s1)
        g.dma(lgb[:], I["ret_decay"][l:l + 1].rearrange("o a b -> o (a b)").partition_broadcast(128), W=[lgb.b[0]])
        g.act(lgb[:], lgb[:], AF.Exp, [lgb.b[0]], [lgb.b[0]], scale=-float(np.log(2.0)))
        g.act(lgb[:], lgb[:], AF.Ln, [lgb.b[0]], [lgb.b[0]], scale=-1.0, bias=C.onec[:, 0:1])
        cst = g.sb("retc", [128, 6, 128], F32, stack=s1)
        g.dma(cst[:], I["ret_const"], W=[cst.b[0]])
        pcol = g.sb("retpc", [128, 2, 64], F32, stack=s1)
        g.dma(pcol[:], I["ret_pcol"], W=[pcol.b[0]])
        c128 = g.sb("retc128", [128, 128], F32, stack=s1)
        g.memset(c128[:], 128.0, [c128.b[0]])
        BD = g.sb("retBD", [128, 2, 128], F32, stack=s1)
        g.memset(BD[:], 0.0, [BD.b[0]])
        g.memset(BD[0:64, :, 0:64], 1.0, [BD.b[0]])
        g.memset(BD[64:128, :, 64:128], 1.0, [BD.b[0]])
        MT = g.sb("retMT", [128, 2, 2, 128], BF16, stack=s1)
        g.memset(MT[:], 0.0, [MT.b[0]])
        g.memset(MT[0:64, 0], 1.0, [MT.b[0]])
        g.memset(MT[64:128, 1], 1.0, [MT.b[0]])
        D2 = g.sb("retD2", [128, 4, 128], F32, stack=s1)
        tmpd = g.sb("rettmp", [128, 128], F32, stack=s1)
        QF = g.sb("retQF", [128, 2, 128], F32, stack=s1)
        QB = g.sb("retQB", [128, 2, 128], F32, stack=s1)
        KF = g.sb("retKF", [128, 256], F32, stack=s1)
        KB = g.sb("retKB", [128, 256], F32, stack=s1)
        CDF = g.sb("retCDF", [128, 2, 128], F32, stack=s1)
        CDB = g.sb("retCDB", [128, 2, 128], F32, stack=s1)
        R0 = [lgb.b[0], cst.b[0]]
        for h in range(4):
            f, b = lgb[:, h:h + 1], lgb[:, 4 + h:5 + h]
            g.act(D2[:, h, :], cst[:, 0, :], AF.Exp, R0, [D2.b[0]], scale=f)
            g.tt(D2[:, h, :], D2[:, h, :], cst[:, 2, :], ALU.mult, [D2.b[0], cst.b[0]], [D2.b[0]])
            g.act(tmpd[:], cst[:, 1, :], AF.Exp, R0, [tmpd.b[0]], scale=b)
            g.tt(tmpd[:], tmpd[:], cst[:, 3, :], ALU.mult, [tmpd.b[0], cst.b[0]], [tmpd.b[0]])
            g.tt(D2[:, h, :], D2[:, h, :], tmpd[:], ALU.add, [D2.b[0], tmpd.b[0]], [D2.b[0]])
            g.ts(D2[:, h, :], D2[:, h, :], 0.125, ALU.mult, [D2.b[0]], [D2.b[0]])
            pr, bs = h // 2, (h % 2) * 64
            g.act(QF[bs:bs + 64, pr, :], cst[bs:bs + 64, 4, :], AF.Exp, R0, [QF.b[0]], scale=lgb[bs:bs + 64, h:h + 1])
            g.act(QB[bs:bs + 64, pr, :], cst[bs:bs + 64, 5, :], AF.Exp, R0, [QB.b[0]], scale=lgb[bs:bs + 64, 4 + h:5 + h])
            g.act(KF[:, h * 64:(h + 1) * 64], pcol[:, 0, :], AF.Exp, [lgb.b[0], pcol.b[0]], [KF.b[0]], scale=f)
            g.act(KB[:, h * 64:(h + 1) * 64], pcol[:, 1, :], AF.Exp, [lgb.b[0], pcol.b[0]], [KB.b[0]], scale=b)
            g.act(CDF[bs:bs + 64, pr, :], c128[bs:bs + 64, :], AF.Exp, [lgb.b[0], c128.b[0]], [CDF.b[0]],
                  scale=lgb[bs:bs + 64, h:h + 1])
            g.act(CDB[bs:bs + 64, pr, :], c128[bs:bs + 64, :], AF.Exp, [lgb.b[0], c128.b[0]], [CDB.b[0]],
                  scale=lgb[bs:bs + 64, 4 + h:5 + h])
        g.ts(KF[:], KF[:], 0.125, ALU.mult, [KF.b[0]], [KF.b[0]])
        g.ts(KB[:], KB[:], 0.125, ALU.mult, [KB.b[0]], [KB.b[0]])

        import os
        RS = int(os.environ.get("RET_STOP", "9"))
        if RS <= 1:
            return
        qkT = g.sb("retqkT", [128, NT, 4, 128], BF16, nslots=NT, stack=s1)
        vall = g.sb("retv", [128, NT, 256], BF16, nslots=NT, stack=s1)
        sg = g.sb("retsg", [128, NT, 256], BF16, nslots=NT, stack=s1)
        SinF = g.sb("retSinF", [128, NT, 2, 128], BF16, nslots=NT, stack=s1)
        SinB = g.sb("retSinB", [128, NT, 2, 128], BF16, nslots=NT, stack=s1)
        with SS() as s2:
            wret = load_w_bf16(C, "w_in", l, 0, 1024, "wret", s2)
            kdf = g.sb("retkdf", [128, NT, 256], BF16, nslots=NT, stack=s2)
            kdb = g.sb("retkdb", [128, NT, 256], BF16, nslots=NT, stack=s2)
            ht = g.sb("retht", [128, 2, 8, 128], BF16, nslots=2, stack=s2)
            rope = g.sb("retrope", [128, 2, 2, 256], F32, nslots=2, stack=s2)
            qk32 = g.sb("retqk32", [128, 512], F32, stack=s2)
            ra = g.sb("retra", [128, 4, 256], F32, nslots=2, stack=s2)
            qkr = g.sb("retqkr", [128, 2, 512], BF16, nslots=2, stack=s2)
            for t in range(NT):
                sl = t % 2
                g.dma(ht[:, sl], C.HT[t].rearrange("p (k c) -> p k c", k=8), R=[C.B_HT[t]], W=[ht.b[sl]])
                p0, p1 = g.ps(), g.ps()
                for (p, n0) in ((p0, 0), (p1, 512)):
                    for k in range(8):
                        g.mm(p[:, :], ht[:, sl, k, :], wret[:, k, n0:n0 + 512], k == 0, k == 7,
                             [ht.b[sl], wret.b[0]], [p.b[0]])
                SUB = int(os.environ.get("RET_SUB", "9"))
                if SUB <= 0:
                    continue
                if os.environ.get("RET_V", "1") == "1":
                    g.cp(vall[:, t, :], p1[:, 0:256], [p1.b[0]], [vall.b[t]], eng="dve")
                if os.environ.get("RET_G", "1") == "1":
                    silu_from(g, sg[:, t, :], [sg.b[t]], p1[:, 256:512], [p1.b[0]], qk32[:, 0:256], qk32.b[0])
                if SUB <= 1:
                    continue
                if t >= 2:
                    g.dma(rope[:, sl], I["rope"][(t - 2) * 128:(t - 1) * 128], W=[rope.b[sl]])
                    g.cp(qk32[:], p0[:, :], [p0.b[0]], [qk32.b[0]], eng="act")
                    v4 = qk32[:].rearrange("p (h a c) -> p h a c", a=2, c=32)
                    t1, t2 = v4[:, :, 0, :], v4[:, :, 1, :]
                    cs = rope[:, sl, 0, :].rearrange("p (h c) -> p h c", c=32)
                    sn = rope[:, sl, 1, :].rearrange("p (h c) -> p h c", c=32)
                    rv = [ra[:, i, :].rearrange("p (h c) -> p h c", c=32) for i in range(4)]
                    RR = [qk32.b[0], rope.b[sl]]
                    g.tt(rv[0], t1, cs, ALU.mult, RR, [ra.b[0]], eng="dve")
                    g.tt(rv[1], t2, sn, ALU.mult, RR, [ra.b[0]], eng="pool")
                    g.tt(rv[2], t1, sn, ALU.mult, RR, [ra.b[1]], eng="dve")
                    g.tt(rv[3], t2, cs, ALU.mult, RR, [ra.b[1]], eng="pool")
                    o4 = qkr[:, sl, :].rearrange("p (h a c) -> p h a c", a=2, c=32)
                    g.tt(o4[:, :, 0, :], rv[0], rv[1], ALU.subtract, [ra.b[0]], [qkr.b[sl]], eng="dve")
                    g.tt(o4[:, :, 1, :], rv[2], rv[3], ALU.add, [ra.b[1]], [qkr.b[sl]], eng="pool")
                else:
                    g.cp(qkr[:, sl, :], p0[:, :], [p0.b[0]], [qkr.b[sl]], eng="act")
                if SUB <= 2:
                    continue
                g.tt(kdf[:, t, :], qkr[:, sl, 256:512], KF[:], ALU.mult, [qkr.b[sl], KF.b[0]], [kdf.b[t]], eng="dve")
                g.tt(kdb[:, t, :], qkr[:, sl, 256:512], KB[:], ALU.mult, [qkr.b[sl], KB.b[0]], [kdb.b[t]], eng="pool")
                if SUB <= 3:
                    continue
                pt = g.ps()
                ptb = pt[:].bitcast(BF16)
                for c4 in range(4):
                    g.tr(ptb[:, c4 * 128:(c4 + 1) * 128], qkr[:, sl, c4 * 128:(c4 + 1) * 128], C.ident[:],
                         [qkr.b[sl], C.ident.b[0]], [pt.b[0]])
                g.cp(qkT[:, t].rearrange("p a b -> p (a b)"), ptb[:, 0:512], [pt.b[0]], [qkT.b[t]], eng="act")

            if RS <= 2:
                return
            S = g.sb("retS", [128, 2, 2, 128], F32, nslots=2, stack=s2)
            tS = g.sb("rettS", [128, 2, 2, 128], F32, nslots=2, stack=s2)
            g.memset(S[:], 0.0, S.b)
            order_f = list(range(NT))
            order_b = [1, 0] + list(range(NT - 1, 1, -1))
            for step in range(NT):
                for d, (order, kd, Sin, CDt) in enumerate(((order_f, kdf, SinF, CDF), (order_b, kdb, SinB, CDB))):
                    t = order[step]
                    p = g.ps()
                    for pr in range(2):
                        g.mm(p[:, pr * 128:(pr + 1) * 128], kd[:, t, pr * 128:(pr + 1) * 128], vall[:, t, pr * 128:(pr + 1) * 128],
                             True, True, [kd.b[t], vall.b[t]], [p.b[0]])
                    g.tt(tS[:, d].rearrange("p a b -> p (a b)"), p[:, 0:256], BD[:].rearrange("p a b -> p (a b)"), ALU.mult,
                         [p.b[0], BD.b[0]], [tS.b[d]], eng="dve")
                    g.cp(Sin[:, t], S[:, d], [S.b[d]], [Sin.b[t]], eng="act")
                    g.tt(S[:, d], S[:, d], CDt[:], ALU.mult, [S.b[d], CDt.b[0]], [S.b[d]], eng="pool")
                    g.tt(S[:, d], S[:, d], tS[:, d], ALU.add, [S.b[d], tS.b[d]], [S.b[d]], eng="pool")

        if RS <= 3:
            return
        with SS() as s2:
            AT = g.sb("retAT", [128, 2, 4, 128], BF16, nslots=2, stack=s2)
            qm = g.sb("retqm", [128, 2, 4, 128], BF16, nslots=2, stack=s2)
            qsf = g.sb("retqsf", [128, 2, 2, 128], BF16, nslots=2, stack=s2)
            qsb = g.sb("retqsb", [128, 2, 2, 128], BF16, nslots=2, stack=s2)
            o32 = g.sb("reto32", [128, 2, 256], F32, nslots=2, stack=s2)
            osq = g.sb("retosq", [128, 256], F32, stack=s2)
            rs = g.sb("retrs", [128, 2, 4], F32, nslots=2, stack=s2)
            ob = g.sb("retob", [128, 2, 256], BF16, nslots=2, stack=s2)
            for t in range(NT):
                if t < 2 and not need_ctx:
                    continue
                sl = t % 2
                for par in range(2):
                    g.tt(qm[:, sl, 2 * par:2 * par + 2, :], qkT[:, t, 0:2, :], MT[:, par], ALU.mult, [qkT.b[t], MT.b[0]],
                         [qm.b[sl]], eng="pool")
                ps_ = g.ps()
                for h in range(4):
                    pr, par = h // 2, h % 2
                    g.mm(ps_[:, h * 128:(h + 1) * 128], qkT[:, t, 2 + pr, :], qm[:, sl, 2 * par + pr, :], True, True,
                         [qkT.b[t], qm.b[sl]], [ps_.b[0]])
                g.tt(AT[:, sl].rearrange("p a b -> p (a b)"), ps_[:, :], D2[:].rearrange("p a b -> p (a b)"), ALU.mult,
                     [ps_.b[0], D2.b[0]], [AT.b[sl]], eng="dve")
                g.tt(qsf[:, sl], qkT[:, t, 0:2, :], QF[:], ALU.mult, [qkT.b[t], QF.b[0]], [qsf.b[sl]], eng="pool")
                g.tt(qsb[:, sl], qkT[:, t, 0:2, :], QB[:], ALU.mult, [qkT.b[t], QB.b[0]], [qsb.b[sl]], eng="pool")
                po = g.ps()
                for pr in range(2):
                    reg = po[:, pr * 128:(pr + 1) * 128]
                    g.mm(reg, qsf[:, sl, pr, :], SinF[:, t, pr, :], True, False, [qsf.b[sl], SinF.b[t]], [po.b[0]])
                    g.mm(reg, qsb[:, sl, pr, :], SinB[:, t, pr, :], False, False, [qsb.b[sl], SinB.b[t]], [po.b[0]])
                    for par in range(2):
                        h = 2 * pr + par
                        g.mm(po[:, h * 64:(h + 1) * 64], AT[:, sl, h, :], vall[:, t, h * 64:(h + 1) * 64], False, par == 1,
                             [AT.b[sl], vall.b[t]], [po.b[0]])
                g.cp(o32[:, sl, :], po[:, 0:256], [po.b[0]], [o32.b[sl]], eng="act")
                g.tt(osq[:], o32[:, sl, :], o32[:, sl, :], ALU.mult, [o32.b[sl]], [osq.b[0]], eng="pool")
                g.K.op("dve", lambda h_, o_=rs[:, sl, :], i_=osq[:].rearrange("p (h c) -> p h c", c=64):
                       h_.tensor_reduce(out=o_, in_=i_, axis=AX.X, op=ALU.add), reads=[osq.b[0]], writes=[rs.b[sl]])
                g.act(rs[:, sl, :], rs[:, sl, :], AF.Sqrt, [rs.b[sl]], [rs.b[sl]], bias=C.epsc[:, 0:1], scale=1.0 / 64)
                g.recip(rs[:, sl, :], rs[:, sl, :], [rs.b[sl]], [rs.b[sl]])
                for h in range(4):
                    g.stt(ob[:, sl, h * 64:(h + 1) * 64], o32[:, sl, h * 64:(h + 1) * 64], rs[:, sl, h:h + 1],
                          sg[:, t, h * 64:(h + 1) * 64], ALU.mult, ALU.mult, [o32.b[sl], rs.b[sl], sg.b[t]], [ob.b[sl]])
                g.dma(C.O[t * 128:(t + 1) * 128, 0:256], ob[:, sl, :], R=[ob.b[sl]], W=[C.B_O[t]])


def phase_gdn(C, l, need_ctx):
    g, I = C.g, C.I
    NCH = NTOK // 64
    order = [list(range(NCH)), [3, 2, 1, 0] + list(range(NCH - 1, 3, -1))]
    id64 = C.ident[0:64, 0:64]
    with SS() as s1:
        cst = g.sb("gdnc", [64, 13, 64], F32, stack=s1)
        g.dma(cst[:], I["gdn_const"], W=[cst.b[0]])
        CB = cst.b[0]
        Y4, SU4, I4 = cst[:, 0:4, :], cst[:, 4:8, :], cst[:, 8:12, :]
        idf, ones = cst[:, 8, :], cst[:, 12, :]
        one64 = C.onec[0:64, 0:1]
        eps64 = C.epsc[0:64, 0:1]
        ab = g.sb("gab", [64, 16, NCH], F32, stack=s1)
        with SS() as s2:
            ht = g.sb("ght", [128, 2, 8, 128], BF16, nslots=2, stack=s2)
            wab = load_w_bf16(C, "w_in", l, 2048, 2064, "wab", s2)
            p = None
            for t in range(NT):
                sl = t % 2
                g.dma(ht[:, sl], C.HT[t].rearrange("p (k c) -> p k c", k=8), R=[C.B_HT[t]], W=[ht.b[sl]])
                for half in range(2):
                    c = 2 * t + half
                    if c % 32 == 0:
                        p = g.ps()
                        c0 = c
                    reg = p[0:64, (c - c0) * 16:(c - c0 + 1) * 16]
                    for k in range(8):
                        g.mm(reg, ht[:, sl, k, half * 64:(half + 1) * 64], wab[:, k, 0:16], k == 0, k == 7,
                             [ht.b[sl], wab.b[0]], [p.b[0]])
                    if c % 32 == 31 or c == NCH - 1:
                        n = c - c0 + 1
                        g.cp(ab[:, :, c0:c0 + n].rearrange("p k c -> p c k"),
                             p[0:64, 0:n * 16].rearrange("p (c k) -> p c k", k=16), [p.b[0]], [ab.b[0]], eng="dve")
        prm = g.sb("gprm", [64, 2, 8], F32, stack=s1)
        g.dma(prm[:, 0, :], I["gdn_a_log"][l:l + 1].rearrange("o a b -> o (a b)").partition_broadcast(64), W=[prm.b[0]])
        g.dma(prm[:, 1, :], I["gdn_dt_bias"][l:l + 1].rearrange("o a b -> o (a b)").partition_broadcast(64), W=[prm.b[0]])
        sc = {}
        for nm in ("G", "beta", "nbeta", "egi", "erem", "egl", "begi"):
            sc[nm] = g.sb("g" + nm, [64, 8, NCH], F32, stack=s1)
        G, beta, nbeta, egi, erem, egl, begi = [sc[k] for k in ("G", "beta", "nbeta", "egi", "erem", "egl", "begi")]
        bc8 = lambda ap: ap.unsqueeze(2).to_broadcast([64, 8, NCH])
        g.tt(G[:], ab[:, 0:8, :], bc8(prm[:, 1, :]), ALU.add, [ab.b[0], prm.b[0]], [G.b[0]])
        g.act(G[:], G[:], AF.Exp, [G.b[0]], [G.b[0]])
        g.act(G[:], G[:], AF.Ln, [G.b[0]], [G.b[0]], bias=one64)
        g.act(prm[:, 0, :], prm[:, 0, :], AF.Exp, [prm.b[0]], [prm.b[0]])
        g.ts(prm[:, 0, :], prm[:, 0, :], -1.0, ALU.mult, [prm.b[0]], [prm.b[0]])
        g.tt(G[:], G[:], bc8(prm[:, 0, :]), ALU.mult, [G.b[0], prm.b[0]], [G.b[0]])
        g.act(beta[:], ab[:, 8:16, :], AF.Exp, [ab.b[0]], [beta.b[0]], scale=-1.0)
        g.ts(beta[:], beta[:], 1.0, ALU.add, [beta.b[0]], [beta.b[0]])
        g.recip(beta[:], beta[:], [beta.b[0]], [beta.b[0]])
        g.ts(nbeta[:], beta[:], -1.0, ALU.mult, [beta.b[0]], [nbeta.b[0]])
        for d in range(2):
            rhs = G[:, d * 4:(d + 1) * 4, :].rearrange("p a c -> p (a c)")
            for (dst, lhs) in ((egi, cst[:, d, :]), (erem, cst[:, 4 + d, :]), (egl, ones)):
                p = g.ps()
                g.mm(p[0:64, 0:4 * NCH], lhs, rhs, True, True, [CB, G.b[0]], [p.b[0]])
                g.act(dst[:, d * 4:(d + 1) * 4, :].rearrange("p a c -> p (a c)"), p[0:64, 0:4 * NCH], AF.Exp, [p.b[0]],
                      [dst.b[0]])
        g.tt(begi[:], beta[:], egi[:], ALU.mult, [beta.b[0], egi.b[0]], [begi.b[0]])
        ngb = g.sb("gngb", [64, 64], F32, stack=s1)
        g.dma(ngb[:], I["gdn_norm_g"][l:l + 1, :].partition_broadcast(64), W=[ngb.b[0]])

        for hp in range(2):
            with SS() as s2:
                qT = g.sb("gqT", [64, 2, NTOK], BF16, nslots=2, stack=s2)
                kT = g.sb("gkT", [64, 2, NTOK], BF16, nslots=2, stack=s2)
                ktm = g.sb("gktm", [64, 2, NCH, 64], BF16, nslots=2, stack=s2)
                vtm = g.sb("gvtm", [64, 2, NCH, 64], BF16, nslots=2, stack=s2)
                sgt = g.sb("gsgt", [64, 2, NCH, 64], BF16, stack=s2)
                with SS() as s3:
                    wg = load_w_bf16(C, "w_in", l, 1024, 2048, "wg", s3)
                    raw = g.sb("graw", [128, NTOK + 8], F32, stack=s3)
                    acc = g.sb("gacc", [128, NTOK], F32, stack=s3)
                    o128 = g.sb("go128", [128, NTOK], BF16, stack=s3)
                    hg = g.sb("ghg", [128, 2, 8, 512], BF16, nslots=2, stack=s3)
                    cw = g.sb("gcw", [128, 3, 5], F32, stack=s3)
                    sq = g.sb("gsq", [128, 2, 512], F32, nslots=2, stack=s3)
                    bd1 = g.sb("gbd1", [128, 128], F32, stack=s3)
                    g.dma(bd1[:], I["gdn_bd"], W=[bd1.b[0]])
                    g.memset(raw[:], 0.0, [raw.b[0]])
                    g.dma(cw[:], I["gdn_convw"][l, 2 * hp:2 * hp + 2].rearrange("h d t k -> (h d) t k"), W=[cw.b[0]])
                    groups = [(0, 256)] + [(256 + 512 * i, 512) for i in range(8)]
                    git = 0
                    for typ in range(3):
                        wc0 = typ * 256 + hp * 128
                        for (t0, n) in groups:
                            sl = git % 2
                            git += 1
                            for q in range(n // 128):
                                t = t0 // 128 + q
                                g.dma(hg[:, sl, :, q * 128:(q + 1) * 128], C.HT[t].rearrange("p (k c) -> p k c", k=8),
                                      R=[C.B_HT[t]], W=[hg.b[sl]])
                            p = g.ps()
                            for k in range(8):
                                g.mm(p[:, 0:n], wg[:, k, wc0:wc0 + 128], hg[:, sl, k, 0:n], k == 0, k == 7,
                                     [wg.b[0], hg.b[sl]], [p.b[0]])
                            off = t0 + 2 if t0 < 256 else t0 + 6
                            g.cp(raw[:, off:off + n], p[:, 0:n], [p.b[0]], [raw.b[0]], eng="act")
                        for (a0, n, r0) in ((0, 256, 0), (256, 2048, 260), (2304, 2048, 2308)):
                            for k in range(5):
                                src = raw[:, r0 + k:r0 + k + n]
                                if k == 0:
                                    g.ts(acc[:, a0:a0 + n], src, cw[:, typ, 0:1], ALU.mult, [raw.b[0], cw.b[0]], [acc.b[0]])
                                else:
                                    g.stt(acc[:, a0:a0 + n], src, cw[:, typ, k:k + 1], acc[:, a0:a0 + n], ALU.mult, ALU.add,
                                          [raw.b[0], cw.b[0], acc.b[0]], [acc.b[0]])
                        g.act(acc[:], acc[:], AF.Silu, [acc.b[0]], [acc.b[0]])
                        if typ == 2:
                            g.cp(o128[:], acc[:], [acc.b[0]], [o128.b[0]], eng="pool")
                        else:
                            for gi2, (t0, n) in enumerate(groups):
                                ss = gi2 % 2
                                g.tt(sq[:, ss, 0:n], acc[:, t0:t0 + n], acc[:, t0:t0 + n], ALU.mult, [acc.b[0]], [sq.b[ss]],
                                     eng="pool")
                                p = g.ps()
                                g.mm(p[:, 0:n], bd1[:], sq[:, ss, 0:n], True, True, [bd1.b[0], sq.b[ss]], [p.b[0]])
                                g.act(sq[:, ss, 0:n], p[:, 0:n], AF.Sqrt, [p.b[0]], [sq.b[ss]], bias=C.epsc[:, 0:1])
                                g.recip(sq[:, ss, 0:n], sq[:, ss, 0:n], [sq.b[ss]], [sq.b[ss]])
                                g.stt(o128[:, t0:t0 + n], acc[:, t0:t0 + n], 0.125 if typ == 0 else 1.0, sq[:, ss, 0:n],
                                      ALU.mult, ALU.mult, [acc.b[0], sq.b[ss]], [o128.b[0]])
                            dst = qT if typ == 0 else kT
                            for hh in range(2):
                                g.dma(dst[:, hh, :], o128[hh * 64:(hh + 1) * 64, :], R=[o128.b[0]], W=[dst.b[hh]])
                        if typ >= 1:
                            dst_tm = ktm if typ == 1 else vtm
                            for c0 in range(0, NCH, 8):
                                nn = min(8, NCH - c0)
                                p = g.ps()
                                pb = p[:].bitcast(BF16)
                                for ci in range(nn):
                                    c = c0 + ci
                                    g.tr(pb[0:64, ci * 128:(ci + 1) * 128], o128[:, c * 64:(c + 1) * 64], C.ident[:],
                                         [o128.b[0], C.ident.b[0]], [p.b[0]])
                                g.cp(dst_tm[:, :, c0:c0 + nn, :], pb[0:64, 0:nn * 128].rearrange("p (c h d) -> p h c d", h=2, d=64),
                                     [p.b[0]], [dst_tm.b[0], dst_tm.b[1]], eng="act")
                    ht2 = g.sb("ght2", [128, 2, 8, 128], BF16, nslots=2, stack=s3)
                    sg32 = g.sb("gsg32", [64, 512], F32, stack=s3)
                    p = None
                    for t in range(NT):
                        sl = t % 2
                        g.dma(ht2[:, sl], C.HT[t].rearrange("p (k c) -> p k c", k=8), R=[C.B_HT[t]], W=[ht2.b[sl]])
                        for half in range(2):
                            c = 2 * t + half
                            if c % 4 == 0:
                                p = g.ps()
                                c0 = c
                            reg = p[0:64, (c - c0) * 128:(c - c0 + 1) * 128]
                            for k in range(8):
                                g.mm(reg, ht2[:, sl, k, half * 64:(half + 1) * 64], wg[:, k, 768 + hp * 128:768 + (hp + 1) * 128],
                                     k == 0, k == 7, [ht2.b[sl], wg.b[0]], [p.b[0]])
                            if c % 4 == 3:
                                g.cp(sg32[:], p[0:64, :], [p.b[0]], [sg32.b[0]], eng="act")
                                g.act(sgt[:, :, c0:c0 + 4, :], sg32[:].rearrange("p (c h d) -> p h c d", h=2, d=64), AF.Silu,
                                      [sg32.b[0]], [sgt.b[0]])

                Oacc = g.sb("gOacc", [64, 2, NCH, 64], F32, nslots=2, stack=s2)
                with SS() as s3:
                    NSET, RS_ = 3, 4

                    def tn(nm, dt, ns, w=64, extra=()):
                        return g.sb(nm, [64, ns] + list(extra) + [4, w] if not extra else [64, ns] + list(extra), dt, nslots=ns, stack=s3)
                    X4 = tn("gX4", F32, NSET)
                    E8 = g.sb("gE8", [64, NSET, 4, 2, 64], F32, nslots=NSET, stack=s3)
                    DT4, DS4, P0, P0T, TT = [tn(n_, F32, NSET) for n_ in ("gDT4", "gDS4", "gP0", "gP0T", "gTT")]
                    PP = g.sb("gPP", [64, NSET * 2, 4, 2, 64], F32, nslots=NSET * 2, stack=s3)
                    TTb, vb4, kbeg4 = [tn(n_, BF16, NSET) for n_ in ("gTTb", "gvb4", "gkbeg4")]
                    attn4, wTb, kdec4 = [tn(n_, BF16, RS_) for n_ in ("gattn4", "gwTb", "gkdec4")]
                    U4 = tn("gU4", F32, RS_)
                    S = g.sb("gS", [64, 4, 64], F32, stack=s3)
                    Sb = g.sb("gSb", [64, 4, 64], BF16, stack=s3)
                    vn4 = g.sb("gvn4", [64, 4, 64], BF16, stack=s3)
                    qs4 = g.sb("gqs4", [64, 4, 64], F32, stack=s3)
                    g.memset(S[:], 0.0, [S.b[0]])
                    g.memset(Sb[:], 0.0, [Sb.b[0]])
                    idr = g.sb("gidr", [64, 64], F32, stack=s3)
                    g.cp(idr[:].bitcast(F32R), idf, [CB], [idr.b[0]])
                    RR_ = lambda ap: ap.bitcast(F32R)
                    visited = set()
                    units = [(hh, d) for hh in range(2) for d in range(2)]

                    def col(tile_, u, step):
                        hh, d = units[u]
                        c = order[d][step]
                        dh = d * 4 + 2 * hp + hh
                        return tile_[:, dh, c:c + 1]

                    def indep(step):
                        ts_ = step % NSET
                        sl = step % RS_
                        cs = [order[d][step] for (hh, d) in units]
                        for u in range(4):
                            g.ts(X4[:, ts_, u, :], SU4[:, u, :], col(G, u, step), ALU.mult, [CB, G.b[0]], [X4.b[ts_]])
                        yield
                        pa, pbk = g.ps(), g.ps()
                        pav = pa[0:64, :].rearrange("p (u t i) -> p u t i", u=4, t=2)
                        pbv = pbk[0:64, :].rearrange("p (u t i) -> p u t i", u=4, t=2)
                        for u, (hh, d) in enumerate(units):
                            g.mm(pav[:, u, 0, :], X4[:, ts_, u, :], cst[:, d, :], True, True, [X4.b[ts_], CB], [pa.b[0]])
                            g.mm(pav[:, u, 1, :], cst[:, d, :], X4[:, ts_, u, :], True, True, [X4.b[ts_], CB], [pa.b[0]])
                        for u, (hh, d) in enumerate(units):
                            c = cs[u]
                            kc, qc = kT[:, hh, c * 64:(c + 1) * 64], qT[:, hh, c * 64:(c + 1) * 64]
                            g.mm(pbv[:, u, 0, :], kc, qc, True, True, [kT.b[hh], qT.b[hh]], [pbk.b[0]])
                            g.mm(pbv[:, u, 1, :], kc, kc, True, True, [kT.b[hh]], [pbk.b[0]])
                        yield
                        g.act(E8[:, ts_].rearrange("p u t i -> p (u t i)"), pa[0:64, :], AF.Exp, [pa.b[0]], [E8.b[ts_]])
                        yield
                        g.tt(DT4[:, ts_], E8[:, ts_, :, 0, :], Y4, ALU.mult, [E8.b[ts_], CB], [DT4.b[ts_]], eng="pool")
                        g.tt(DS4[:, ts_], E8[:, ts_, :, 1, :], SU4, ALU.mult, [E8.b[ts_], CB], [DS4.b[ts_]], eng="pool")
                        yield
                        for u in range(4):
                            g.stt(RR_(P0[:, ts_, u, :]), pbv[:, u, 1, :], col(nbeta, u, step), DS4[:, ts_, u, :], ALU.mult, ALU.mult,
                                  [pbk.b[0], nbeta.b[0], DS4.b[ts_]], [P0.b[ts_]])
                        g.tt(attn4[:, sl], pbv[:, :, 0, :], DT4[:, ts_], ALU.mult, [pbk.b[0], DT4.b[ts_]], [attn4.b[sl]])
                        yield
                        pc = g.ps()
                        pcv = pc[0:64, 0:256].rearrange("p (u i) -> p u i", u=4)
                        for u in range(4):
                            g.mm(pcv[:, u, :], RR_(P0[:, ts_, u, :]), RR_(idr[:]), True, True, [P0.b[ts_], idr.b[0]], [pc.b[0]])
                        yield
                        g.cp(RR_(P0T[:, ts_].rearrange("p u i -> p (u i)")), pc[0:64, 0:256], [pc.b[0]], [P0T.b[ts_]], eng="act")
                        yield
                        g.tt(RR_(TT[:, ts_]), P0T[:, ts_], I4, ALU.add, [P0T.b[ts_], CB], [TT.b[ts_]], eng="pool")
                        Pk, PkT, PkB = P0[:, ts_], P0T[:, ts_], [P0.b[ts_], P0T.b[ts_]]

                        def sq_mm(k, Pk, PkT, PkB):
                            pd = g.ps()
                            pdv = pd[0:64, :].rearrange("p (u t i) -> p u t i", u=4, t=2)
                            for u in range(4):
                                g.mm(pdv[:, u, 0, :], RR_(PkT[:, u, :]), RR_(Pk[:, u, :]), True, True, PkB, [pd.b[0]])
                                if k < 4:
                                    g.mm(pdv[:, u, 1, :], RR_(Pk[:, u, :]), RR_(PkT[:, u, :]), True, True, PkB, [pd.b[0]])
                            return pd

                        pd = sq_mm(0, Pk, PkT, PkB)
                        yield
                        for k in range(5):
                            ps_ = ts_ * 2 + k % 2
                            g.cp(RR_(PP[:, ps_].rearrange("p u t i -> p (u t i)")), pd[0:64, :], [pd.b[0]], [PP.b[ps_]], eng="act")
                            yield
                            Pk, PkT, PkB = PP[:, ps_, :, 0, :], PP[:, ps_, :, 1, :], [PP.b[ps_]]
                            if k < 4:
                                pd = sq_mm(k + 1, Pk, PkT, PkB)
                            pe_ = g.ps()
                            pev = pe_[0:64, 0:256].rearrange("p (u i) -> p u i", u=4)
                            for u in range(4):
                                g.mm(pev[:, u, :], RR_(Pk[:, u, :]), RR_(TT[:, ts_, u, :]), True, True, PkB + [TT.b[ts_]], [pe_.b[0]])
                            yield
                            g.tt(RR_(TT[:, ts_]), TT[:, ts_], pev, ALU.add, [TT.b[ts_], pe_.b[0]], [TT.b[ts_]])
                        g.cp(TTb[:, ts_], TT[:, ts_], [TT.b[ts_]], [TTb.b[ts_]], eng="act")
                        for u, (hh, d) in enumerate(units):
                            c = cs[u]
                            g.ts(vb4[:, ts_, u, :], vtm[:, hh, c, :], col(beta, u, step), ALU.mult, [vtm.b[hh], beta.b[0]], [vb4.b[ts_]])
                            g.ts(kbeg4[:, ts_, u, :], ktm[:, hh, c, :], col(begi, u, step), ALU.mult, [ktm.b[hh], begi.b[0]],
                                 [kbeg4.b[ts_]])
                            g.ts(kdec4[:, sl, u, :], ktm[:, hh, c, :], col(erem, u, step), ALU.mult, [ktm.b[hh], erem.b[0]],
                                 [kdec4.b[sl]])
                        yield
                        pf = g.ps()
                        pfv = pf[0:64, :].rearrange("p (u t i) -> p u t i", u=4, t=2)
                        for u in range(4):
                            g.mm(pfv[:, u, 0, :], TTb[:, ts_, u, :], vb4[:, ts_, u, :], True, True, [TTb.b[ts_], vb4.b[ts_]], [pf.b[0]])
                            g.mm(pfv[:, u, 1, :], kbeg4[:, ts_, u, :], TTb[:, ts_, u, :], True, True, [TTb.b[ts_], kbeg4.b[ts_]],
                                 [pf.b[0]])
                        yield
                        g.cp(U4[:, sl], pfv[:, :, 0, :], [pf.b[0]], [U4.b[sl]], eng="act")
                        g.cp(wTb[:, sl], pfv[:, :, 1, :], [pf.b[0]], [wTb.b[sl]], eng="act")

                    def dep(step):
                        sl = step % RS_
                        cs = [order[d][step] for (hh, d) in units]
                        pg = g.ps()
                        pgv = pg[0:64, :].rearrange("p (u t i) -> p u t i", u=4, t=2)
                        for u, (hh, d) in enumerate(units):
                            c = cs[u]
                            g.mm(pgv[:, u, 0, :], wTb[:, sl, u, :], Sb[:, u, :], True, True, [wTb.b[sl], Sb.b[0]], [pg.b[0]])
                            g.mm(pgv[:, u, 1, :], qT[:, hh, c * 64:(c + 1) * 64], Sb[:, u, :], True, True, [qT.b[hh], Sb.b[0]],
                                 [pg.b[0]])
                        g.tt(vn4[:], U4[:, sl], pgv[:, :, 0, :], ALU.subtract, [U4.b[sl], pg.b[0]], [vn4.b[0]])
                        for u in range(4):
                            g.act(qs4[:, u, :], pgv[:, u, 1, :], AF.Copy, [pg.b[0], egi.b[0]], [qs4.b[0]], scale=col(egi, u, step))
                        ph = g.ps()
                        phv = ph[0:64, :].rearrange("p (u t i) -> p u t i", u=4, t=2)
                        for u in range(4):
                            g.mm(phv[:, u, 0, :], kdec4[:, sl, u, :], vn4[:, u, :], True, True, [kdec4.b[sl], vn4.b[0]], [ph.b[0]])
                            g.mm(phv[:, u, 1, :], attn4[:, sl, u, :], vn4[:, u, :], True, True, [attn4.b[sl], vn4.b[0]], [ph.b[0]])
                        for u in range(4):
                            g.stt(S[:, u, :], S[:, u, :], col(egl, u, step), phv[:, u, 0, :], ALU.mult, ALU.add,
                                  [S.b[0], egl.b[0], ph.b[0]], [S.b[0]])
                        g.cp(Sb[:], S[:], [S.b[0]], [Sb.b[0]], eng="act")
                        for u, (hh, d) in enumerate(units):
                            c = cs[u]
                            if (hh, c) not in visited:
                                visited.add((hh, c))
                                g.tt(Oacc[:, hh, c, :], qs4[:, u, :], phv[:, u, 1, :], ALU.add, [qs4.b[0], ph.b[0]], [Oacc.b[hh]])
                            else:
                                g.tt(qs4[:, u, :], qs4[:, u, :], phv[:, u, 1, :], ALU.add, [qs4.b[0], ph.b[0]], [qs4.b[0]])
                                g.tt(Oacc[:, hh, c, :], Oacc[:, hh, c, :], qs4[:, u, :], ALU.add, [qs4.b[0], Oacc.b[hh]],
                                     [Oacc.b[hh]], eng="pool")

                    NS = int(os.environ.get("GDN_STEPS", str(NCH)))
                    gens = []
                    done = set()
                    nxt_i, nxt_d = 0, 0
                    while nxt_d < NS:
                        while len(gens) < NSET and nxt_i < NS and nxt_i < nxt_d + RS_:
                            gens.append((nxt_i, indep(nxt_i)))
                            nxt_i += 1
                        still = []
                        for (st_, ge_) in gens:
                            try:
                                next(ge_)
                                still.append((st_, ge_))
                            except StopIteration:
                                done.add(st_)
                        gens = still
                        while nxt_d in done:
                            dep(nxt_d)
                            nxt_d += 1
                    if C.DBG is not None and hp == 0 and NS < NCH:
                        o_ = 0
                        for nm_, tl_, ap_ in (("S", S, S[:]), ("qs4", qs4, qs4[:])):
                            g.dma(C.DBG[:, o_:o_ + 256].rearrange("p (u i) -> p u i", u=4), ap_, R=tl_.b, W=[C.B_DBG], q="pool")
                            o_ += 256

                if C.DBG is not None and hp == 0 and "GDN_STEPS" not in os.environ:
                    o_ = 0
                    for tl_ in (G, beta, egi, erem, egl):
                        g.dma(C.DBG[:, o_:o_ + 8 * NCH], tl_[:].rearrange("p a c -> p (a c)"), R=[tl_.b[0]], W=[C.B_DBG], q="pool")
                        o_ += 8 * NCH
                    g.dma(C.DBG[:, 3000:3512], qT[:, 0, 0:512], R=[qT.b[0]], W=[C.B_DBG], q="pool")
                    g.dma(C.DBG[:, 3512:4024], kT[:, 0, 0:512], R=[kT.b[0]], W=[C.B_DBG], q="pool")
                    g.dma(C.DBG[:, 4024:4536], ktm[:, 0, 0:8, :].rearrange("p c d -> p (c d)"), R=[ktm.b[0]], W=[C.B_DBG], q="pool")
                    g.dma(C.DBG[:, 4536:5048], vtm[:, 0, 0:8, :].rearrange("p c d -> p (c d)"), R=[vtm.b[0]], W=[C.B_DBG], q="pool")
                    g.dma(C.DBG[:, 5048:5560], Oacc[:, 0, 0:8, :].rearrange("p c d -> p (c d)"), R=[Oacc.b[0]], W=[C.B_DBG], q="pool")
                    g.dma(C.DBG[:, 5560:6072], Oacc[:, 0, 60:68, :].rearrange("p c d -> p (c d)"), R=[Oacc.b[0]], W=[C.B_DBG], q="pool")
                    g.dma(C.DBG[:, 6072:6584], sgt[:, 0, 0:8, :].rearrange("p c d -> p (c d)"), R=[sgt.b[0]], W=[C.B_DBG], q="pool")
                with SS() as s3:
                    osq = g.sb("gosq", [64, 2 * NCH, 64], F32, stack=s3)
                    rs = g.sb("grs", [64, 2 * NCH], F32, stack=s3)
                    ob = vtm
                    OB = [Oacc.b[0], Oacc.b[1]]
                    Ov = Oacc[:].rearrange("p h c d -> p (h c) d")
                    g.tt(osq[:], Ov, Ov, ALU.mult, OB, [osq.b[0]], eng="pool")
                    g.K.op("dve", lambda h_: h_.tensor_reduce(out=rs[:], in_=osq[:], axis=AX.X, op=ALU.add), reads=[osq.b[0]],
                           writes=[rs.b[0]])
                    g.act(rs[:], rs[:], AF.Sqrt, [rs.b[0]], [rs.b[0]], bias=eps64, scale=1.0 / 64)
                    g.recip(rs[:], rs[:], [rs.b[0]], [rs.b[0]])
                    g.tt(osq[:], Ov, rs[:].unsqueeze(2).to_broadcast([64, 2 * NCH, 64]), ALU.mult, OB + [rs.b[0]], [osq.b[0]])
                    g.tt(osq[:], osq[:], ngb[:].unsqueeze(1).to_broadcast([64, 2 * NCH, 64]), ALU.mult, [osq.b[0], ngb.b[0]],
                         [osq.b[0]])
                    g.tt(ob[:].rearrange("p h c d -> p (h c) d"), osq[:], sgt[:].rearrange("p h c d -> p (h c) d"), ALU.mult,
                         [osq.b[0], sgt.b[0]], [ob.b[0], ob.b[1]])
                    for hh in range(2):
                        h = 2 * hp + hh
                        g.dma(C.O.rearrange("(c p) n -> p c n", p=64)[:, :, 256 + h * 64:256 + (h + 1) * 64], ob[:, hh],
                              R=[ob.b[0], ob.b[1]], W=C.B_O)


def phase_wout(C, l, need_ctx):
    g, I = C.g, C.I
    xsrc = I["xin"] if l == 0 else C.XS
    with SS() as s1:
        wout = load_w_bf16(C, "w_out", l, 0, D, "wout", s1)
        Abc, Sbc = norm_mod_tiles(C, l, 2, s1)
        G1 = g.sb("G1bc", [128, 2, D], F32, stack=s1)
        for r in range(2):
            g.dma(G1[:, r, :], C.MOD[l, r:r + 1, 2 * D:3 * D].partition_broadcast(128), R=[C.B_MOD[l]], W=[G1.b[0]])
        T = nm_scratch(C, s1)
        xt = g.sb("xt", [128, 2, D], F32, nslots=2, stack=s1)
        ot = g.sb("ot", [128, 2, D], BF16, nslots=2, stack=s1)
        oT = g.sb("oT", [128, 2, D], BF16, nslots=2, stack=s1)
        tmp = g.sb("wtmp", [128, D], F32, stack=s1)
        for t in range(NT):
            if t < 2 and not need_ctx:
                continue
            sl = t % 2
            r = 1 if t < 2 else 0
            g.dma(xt[:, sl], xsrc[t * 128:(t + 1) * 128, :], R=[C.B_XS[t]] if l > 0 else [], W=[xt.b[sl]])
            g.dma(ot[:, sl], C.O[t * 128:(t + 1) * 128, :], R=[C.B_O[t]], W=[ot.b[sl]])
            p = g.ps()
            pb = p[:].bitcast(BF16)
            for k in range(8):
                g.tr(pb[:, k * 128:(k + 1) * 128], ot[:, sl, k * 128:(k + 1) * 128], C.ident[:],
                     [ot.b[sl], C.ident.b[0]], [p.b[0]])
            g.cp(oT[:, sl], pb, [p.b[0]], [oT.b[sl]], eng="act")
            for nh in range(2):
                p = g.ps()
                for k in range(8):
                    g.mm(p[:, :], oT[:, sl, k * 128:(k + 1) * 128], wout[:, k, nh * 512:(nh + 1) * 512], k == 0, k == 7,
                         [oT.b[sl], wout.b[0]], [p.b[0]])
                g.tt(tmp[:, nh * 512:(nh + 1) * 512], p[:, :], G1[:, r, nh * 512:(nh + 1) * 512], ALU.mult,
                     [p.b[0], G1.b[0]], [tmp.b[0]], eng="dve")
            g.tt(xt[:, sl], xt[:, sl], tmp[:], ALU.add, [xt.b[sl], tmp.b[0]], [xt.b[sl]], eng="pool")
            g.dma(C.XS[t * 128:(t + 1) * 128, :], xt[:, sl], R=[xt.b[sl]], W=[C.B_XS[t]])
            norm_mod_transpose(C, xt[:, sl], xt.b[sl], r, Abc, Sbc, T, sl, C.H2T[t], C.B_H2T[t])


def phase_ffn(C, l, need_ctx, last):
    g, I = C.g, C.I
    moe = (l % 2 == 1)
    j = l // 2
    E = NE if moe else 1
    if moe:
        W1 = lambda e: I["moe_w1"][j, e]
        W3 = lambda e: I["moe_w3"][j, e]
        W2 = lambda e: I["moe_w2"][j, e]
    else:
        W1 = lambda e: I["ffn_w1"][j]
        W3 = lambda e: I["ffn_w3"][j]
        W2 = lambda e: I["ffn_w2"][j]
    tiles = [t for t in range(NT) if t >= 2 or need_ctx]
    groups = []
    if need_ctx:
        groups.append([0, 1])
    for i in range(4):
        groups.append([2 + 8 * i + q for q in range(8)])
    with SS() as s1:
        G2 = g.sb("G2bc", [128, 2, D], F32, stack=s1)
        for r in range(2):
            g.dma(G2[:, r, :], C.MOD[l, r:r + 1, 5 * D:6 * D].partition_broadcast(128), R=[C.B_MOD[l]], W=[G2.b[0]])
        if last:
            fg = g.sb("fgbc", [128, D], F32, stack=s1)
            g.dma(fg[:], I["final_g"].rearrange("(o d) -> o d", o=1).partition_broadcast(128), W=[fg.b[0]])
        if moe:
            wr = g.sb("wrouter", [128, 8, NE], BF16, stack=s1)
            g.dma(wr[:], I["moe_router"][j].rearrange("(k p) e -> p k e", p=128), W=[wr.b[0]], q="pool")
        h2 = g.sb("h2", [128, 1, 8, 1024], BF16, nslots=1, stack=s1)
        aT = g.sb("aT", [128, 22, 1024], BF16, nslots=22, stack=s1)
        Y = g.sb("Yacc", [128, 8, D], F32, nslots=8, stack=s1)
        w13 = g.sb("w13", [128, 3, 2, 8, 256], BF16, nslots=3, stack=s1)
        w2 = g.sb("w2", [128, 2, 11, D], BF16, nslots=2, stack=s1)
        sil = g.sb("sil", [128, 2, 512], F32, nslots=2, stack=s1)
        gates = g.sb("gates", [128, 8, NE], F32, nslots=8, stack=s1)
        rt = g.sb("rt", [128, 8, NE], F32, stack=s1)
        rc = g.sb("rc", [128, 8], F32, stack=s1)
        xt = g.sb("xt", [128, 2, D], F32, nslots=2, stack=s1)
        ft = g.sb("ftmp", [128, D], F32, stack=s1)
        fs = g.sb("fssq", [128, 2], F32, nslots=2, stack=s1)
        w13_it = 0
        w2_it = 0
        for gi_, tl in enumerate(groups):
            hs = 0
            n = 128 * len(tl)
            for q, t in enumerate(tl):
                g.dma(h2[:, hs, :, q * 128:(q + 1) * 128], C.H2T[t].rearrange("p (k c) -> p k c", k=8), R=[C.B_H2T[t]],
                      W=[h2.b[hs]])
            for q, t in enumerate(tl):
                if not moe:
                    g.memset(gates[:, q, :], 1.0, [gates.b[q]])
                    continue
                p = g.ps()
                for k in range(8):
                    g.mm(p[:, 0:NE], h2[:, hs, k, q * 128:(q + 1) * 128], wr[:, k, :], k == 0, k == 7, [h2.b[hs], wr.b[0]],
                         [p.b[0]])
                RT, RC = [rt.b[0]], [rc.b[0]]
                lg, eq1, msk, eq2 = rt[:, 0, :], rt[:, 1, :], rt[:, 2, :], rt[:, 3, :]
                g.cp(lg, p[:, 0:NE], [p.b[0]], RT, eng="dve")
                red = lambda o_, i_: g.K.op("dve", lambda h_: h_.tensor_reduce(out=o_, in_=i_, axis=AX.X, op=ALU.max),
                                            reads=RT + RC, writes=RC)
                red(rc[:, 0:1], lg)
                g.ts(eq1, lg, rc[:, 0:1], ALU.is_equal, RT + RC, RT)
                g.stt(msk, eq1, -1e30, lg, ALU.mult, ALU.add, RT, RT)
                red(rc[:, 1:2], msk)
                g.ts(eq2, msk, rc[:, 1:2], ALU.is_equal, RT + RC, RT)
                g.tt(rc[:, 2:3], rc[:, 1:2], rc[:, 0:1], ALU.subtract, RC, RC)
                g.act(rc[:, 3:4], rc[:, 2:3], AF.Exp, RC, RC)
                g.ts(rc[:, 4:5], rc[:, 3:4], 1.0, ALU.add, RC, RC)
                g.recip(rc[:, 4:5], rc[:, 4:5], RC, RC)
                g.tt(rc[:, 5:6], rc[:, 3:4], rc[:, 4:5], ALU.mult, RC, RC)
                g.ts(eq1, eq1, rc[:, 4:5], ALU.mult, RT + RC, RT)
                g.stt(gates[:, q, :], eq2, rc[:, 5:6], eq1, ALU.mult, ALU.add, RT + RC, [gates.b[q]])
            for e in range(E):
                w1v = W1(e).rearrange("(k p) n -> p k n", p=128)
                w3v = W3(e).rearrange("(k p) n -> p k n", p=128)
                w2v = W2(e).rearrange("(c p) n -> p c n", p=128)
                w2s = [0, 1]
                sit = 0
                for cb in range(11):
                    wsl = w13_it % 3
                    w13_it += 1
                    g.dma(w13[:, wsl, 0], w1v[:, :, cb * 256:(cb + 1) * 256], W=[w13.b[wsl]], q="pool")
                    g.dma(w13[:, wsl, 1], w3v[:, :, cb * 256:(cb + 1) * 256], W=[w13.b[wsl]], q="pool")
                    if cb == 2:
                        for hf_ in range(2):
                            g.dma(w2[:, hf_], w2v[:, hf_ * 11:(hf_ + 1) * 11, :], W=[w2.b[hf_]], q="pool")
                    for c2 in range(2):
                        c = 2 * cb + c2
                        for n0 in range(0, n, 512):
                            nn = min(512, n - n0)
                            ss = sit % 2
                            sit += 1
                            p1, p3 = g.ps(), g.ps()
                            for (pp, wi) in ((p1, 0), (p3, 1)):
                                for k in range(8):
                                    g.mm(pp[:, 0:nn], w13[:, wsl, wi, k, c2 * 128:(c2 + 1) * 128], h2[:, hs, k, n0:n0 + nn], k == 0,
                                         k == 7, [w13.b[wsl], h2.b[hs]], [pp.b[0]])
                            g.act(sil[:, ss, 0:nn], p1[:, 0:nn], AF.Silu, [p1.b[0]], [sil.b[ss]])
                            g.tt(aT[:, c, n0:n0 + nn], p3[:, 0:nn], sil[:, ss, 0:nn], ALU.mult, [p3.b[0], sil.b[ss]], [aT.b[c]],
                                 eng="dve")
                for q, t in enumerate(tl):
                    for nh in range(2):
                        p = g.ps()
                        for c in range(22):
                            g.mm(p[:, :], aT[:, c, q * 128:(q + 1) * 128], w2[:, w2s[c // 11], c % 11, nh * 512:(nh + 1) * 512],
                                 c == 0, c == 21, [aT.b[c], w2.b[w2s[c // 11]]], [p.b[0]])
                        yv = Y[:, q, nh * 512:(nh + 1) * 512]
                        if e == 0:
                            g.ts(yv, p[:, :], gates[:, q, e:e + 1], ALU.mult, [p.b[0], gates.b[q]], [Y.b[q]])
                        else:
                            g.stt(yv, p[:, :], gates[:, q, e:e + 1], yv, ALU.mult, ALU.add, [p.b[0], gates.b[q], Y.b[q]], [Y.b[q]])
            for q, t in enumerate(tl):
                sl = t % 2
                r = 1 if t < 2 else 0
                g.dma(xt[:, sl], C.XS[t * 128:(t + 1) * 128, :], R=[C.B_XS[t]], W=[xt.b[sl]])
                g.tt(Y[:, q, :], Y[:, q, :], G2[:, r, :], ALU.mult, [Y.b[q], G2.b[0]], [Y.b[q]], eng="pool")
                g.tt(xt[:, sl], xt[:, sl], Y[:, q, :], ALU.add, [xt.b[sl], Y.b[q]], [xt.b[sl]], eng="pool")
                if not last:
                    g.dma(C.XS[t * 128:(t + 1) * 128, :], xt[:, sl], R=[xt.b[sl]], W=[C.B_XS[t]])
                else:
                    g.act(ft[:], xt[:, sl], AF.Square, [xt.b[sl]], [ft.b[0], fs.b[sl]], accum_out=fs[:, sl:sl + 1])
                    g.act(fs[:, sl:sl + 1], fs[:, sl:sl + 1], AF.Sqrt, [fs.b[sl]], [fs.b[sl]], bias=C.epsc[:, 0:1], scale=1.0 / D)
                    g.recip(fs[:, sl:sl + 1], fs[:, sl:sl + 1], [fs.b[sl]], [fs.b[sl]])
                    g.stt(xt[:, sl], xt[:, sl], fs[:, sl:sl + 1], fg[:], ALU.mult, ALU.mult, [xt.b[sl], fs.b[sl], fg.b[0]],
                          [xt.b[sl]])
                    C.out_tokens.append(g.dma(C.out[(t - 2) * 128:(t - 1) * 128, :], xt[:, sl], R=[xt.b[sl]], W=[C.B_OUT[t]]))


def phase_gdnzero(C, l):
    g = C.g
    with SS() as s1:
        z = g.sb("gz", [128, 256], BF16, stack=s1)
        g.memset(z[:], 0.0, [z.b[0]])
        for t in range(NT):
            g.dma(C.O[t * 128:(t + 1) * 128, 256:512], z[:], R=[z.b[0]], W=[C.B_O[t]])


def build_program(dbg=None, nlayers=DEPTH, phases=("norm1", "ret", "gdn", "na", "wout", "ffn")):
    nc = bass.Bass("TRN2", target_bir_lowering=False)
    C = Ctx()
    I = C.I = {}

    def inp(name, shape, dt=F32):
        I[name] = nc.dram_tensor(name, list(shape), dt, kind="ExternalInput").ap()

    def scratch(name, shape, dt):
        return nc.dram_tensor(name, list(shape), dt, kind="ExternalOutput" if (dbg and name in dbg.split("+")) else "Internal").ap()

    inp("xin", [NTOK, D])
    inp("cvec", [2, D])
    inp("ada_w", [DEPTH, D, 6 * D])
    inp("ada_b", [DEPTH, 6 * D])
    inp("norm1_g", [DEPTH, D])
    inp("norm2_g", [DEPTH, D])
    inp("w_in", [DEPTH, D, D_IN])
    inp("ret_decay", [DEPTH, 2, 4])
    inp("ret_const", [128, 6, 128])
    inp("ret_pcol", [128, 2, 64])
    inp("rope", [SEQ, 2, 256])
    inp("w_out", [DEPTH, D, D])
    inp("ffn_w1", [1, D, D_FF])
    inp("ffn_w3", [1, D, D_FF])
    inp("ffn_w2", [1, D_FF, D])
    inp("moe_router", [1, D, NE])
    inp("moe_w1", [1, NE, D, D_FF])
    inp("moe_w3", [1, NE, D, D_FF])
    inp("moe_w2", [1, NE, D_FF, D])
    inp("final_g", [D])
    inp("gdn_const", [64, 13, 64])
    inp("gdn_bd", [128, 128])
    inp("gdn_a_log", [DEPTH, 2, 4])
    inp("gdn_dt_bias", [DEPTH, 2, 4])
    inp("gdn_norm_g", [DEPTH, 64])
    inp("gdn_convw", [DEPTH, 4, 64, 3, 5])
    inp("na_mask", [128, 5, 640])
    inp("na_bias", [DEPTH, 8, 128, 5, 640])
    C.out = nc.dram_tensor("out", [SEQ, D], F32, kind="ExternalOutput").ap()
    C.out_tokens = []
    C.B_OUT = [Buf() for _ in range(NT)]
    C.MOD = scratch("MOD", [DEPTH, 2, 6 * D], F32)
    C.HT = scratch("HT", [NT, 128, D], BF16)
    C.XS = scratch("XS", [NTOK, D], F32)
    C.O = scratch("O", [NTOK, D], BF16)
    C.H2T = scratch("H2T", [NT, 128, D], BF16)
    C.B_H2T = [Buf() for _ in range(NT)]
    C.DBG = scratch("DBG", [64, 8192], F32) if (dbg and "DBG" in dbg) else None
    C.B_DBG = Buf()
    C.B_MOD = [Buf() for _ in range(DEPTH)]
    C.B_HT = [Buf() for _ in range(NT)]
    C.B_XS = [Buf() for _ in range(NT)]
    C.B_O = [Buf() for _ in range(NT)]

    with ExitStack() as st:
        g = C.g = Gen(nc, st)
        K = g.K
        _CUR["K"] = K
        ident = C.ident = g.sb("ident", [128, 128], BF16)
        g.memset(ident[:], 1.0, [ident.b[0]])
        K.op("pool", lambda h: h.affine_select(out=ident[:], in_=ident[:], pattern=[[-1, 128]],
                                                compare_op=ALU.is_equal, fill=0.0, base=0, channel_multiplier=1),
             reads=[ident.b[0]], writes=[ident.b[0]])
        C.epsc = g.sb("epsc", [128, 1], F32)
        g.memset(C.epsc[:], EPS, [C.epsc.b[0]])
        C.onec = g.sb("onec", [128, 1], F32)
        g.memset(C.onec[:], 1.0, [C.onec.b[0]])

        phase_adaln(C, nlayers)
        for l in range(nlayers):
            need_ctx = l < DEPTH - 1
            if "norm1" in phases:
                phase_norm1(C, l)
            if "ret" in phases:
                phase_ret(C, l, need_ctx)
            if "gdn" in phases:
                phase_gdn(C, l, need_ctx)
            if "na" in phases:
                phase_na(C, l, need_ctx)
            if "gdnzero" in phases:
                phase_gdnzero(C, l)
            if "wout" in phases:
                phase_wout(C, l, need_ctx)
            if "ffn" in phases:
                phase_ffn(C, l, need_ctx, l == nlayers - 1 and nlayers == DEPTH)

        fin = [b.last_w for b in C.B_HT + C.B_O + C.B_XS + C.B_H2T + [C.B_DBG] if b.last_w is not None] + C.out_tokens
        K.finish(fin)
        K.emit()
    return nc


_NC_CACHE = {}
NA_M_REP = [0, 1, 2, 30, 31]


def na_cls(m):
    return {0: 0, 1: 1, 30: 3, 31: 4}.get(m, 2)


def na_kp0(m):
    return min(max(m - 2, 0), 27)


def _na_index_tables():
    kk = np.arange(128)[:, None, None]
    j = np.arange(5)[None, :, None]
    qq = np.arange(128)[None, None, :]
    ki, kc = kk // 64, kk % 64
    qi, qc = qq // 64, qq % 64
    ridx = np.zeros((5, 128, 5, 128), np.int64)
    cidx = np.zeros((5, 128, 5, 128), np.int64)
    valid = np.zeros((5, 128, 5, 128), bool)
    for c, m in enumerate(NA_M_REP):
        kr = 2 * (na_kp0(m) + j) + ki
        r = 2 * m + qi
        r0 = np.clip(r - 4, 0, 56)
        ws = np.clip(qc - 8, 0, 48)
        v = (kr >= r0) & (kr < r0 + 8) & (kc >= ws) & (kc < ws + 16)
        valid[c] = np.broadcast_to(v, (128, 5, 128))
        ridx[c] = np.broadcast_to(np.clip(kr - r + 7, 0, 14), (128, 5, 128))
        cidx[c] = np.broadcast_to(np.clip(kc - qc + 15, 0, 30), (128, 5, 128))
    return ridx, cidx, valid


def _const_inputs():
    if "consts" in _NC_CACHE:
        return _NC_CACHE["consts"]
    ridx, cidx, valid = _na_index_tables()
    ii = np.arange(128)[None, :].astype(np.float64)
    jj = np.arange(128)[:, None].astype(np.float64)
    ret_const = np.stack([np.maximum(ii - jj, 0) + 0 * jj, np.maximum(jj - ii, 0) + 0 * ii, (ii >= jj) * 1.0, (jj >= ii) * 1.0,
                          (ii + 1) + 0 * jj, (128 - ii) + 0 * jj], axis=1).astype(np.float32)
    ret_pcol = np.stack([np.broadcast_to(127 - jj, (128, 64)), np.broadcast_to(jj, (128, 64))], axis=1).astype(np.float32)
    tpos = np.arange(SEQ)
    inv_freq = 10000.0 ** (-np.arange(16, dtype=np.float32) / 16)
    ang = np.concatenate([(tpos // 64).astype(np.float32)[:, None] * inv_freq, (tpos % 64).astype(np.float32)[:, None] * inv_freq],
                         axis=-1).astype(np.float32)
    rope = np.stack([np.tile(np.cos(ang), (1, 8)), np.tile(np.sin(ang), (1, 8))], axis=1).astype(np.float32)
    mm_, i_ = np.arange(64)[:, None], np.arange(64)[None, :]
    Yf, Yb, SUf, SUb = (mm_ <= i_) * 1.0, (mm_ >= i_) * 1.0, (mm_ > i_) * 1.0, (mm_ < i_) * 1.0
    gdn_const = np.stack([Yf, Yb, Yf, Yb, SUf, SUb, SUf, SUb] + [np.eye(64)] * 4 + [np.ones((64, 64))], axis=1).astype(np.float32)
    pp_ = np.arange(128)
    gdn_bd = ((pp_[:, None] // 64) == (pp_[None, :] // 64)).astype(np.float32)
    c = {"gdn_const": np.ascontiguousarray(gdn_const), "gdn_bd": gdn_bd, "ret_const": np.ascontiguousarray(ret_const), "ret_pcol": np.ascontiguousarray(ret_pcol), "rope": rope,
         "na_mask": np.where(valid, 0.0, -1e30).astype(np.float32).transpose(1, 0, 2, 3).reshape(128, 5, 640).copy(),
         "_na_ridx": ridx, "_na_cidx": cidx}
    _NC_CACHE["consts"] = c
    return c


def _prep_core_inputs(b, inputs):
    xin = np.ascontiguousarray(np.concatenate([inputs["ctx"][b], inputs["x"][b]], axis=0))
    cvec = np.ascontiguousarray(np.stack([inputs["c"][b], inputs["c_ctx"]], axis=0))
    m = {"xin": xin, "cvec": cvec}
    for k in ("ada_w", "ada_b", "norm1_g", "norm2_g", "w_in", "w_out", "ffn_w1", "ffn_w3", "ffn_w2", "moe_router", "moe_w1",
              "moe_w3", "moe_w2", "final_g"):
        m[k] = np.ascontiguousarray(inputs[k])
    c = _const_inputs()
    for k in ("gdn_a_log", "gdn_dt_bias", "gdn_norm_g"):
        m[k] = np.ascontiguousarray(inputs[k])
    m["gdn_convw"] = np.ascontiguousarray(inputs["conv_w"].reshape(DEPTH, 5, 3, 4, 64).transpose(0, 3, 4, 2, 1))
    for k in ("na_mask", "ret_const", "ret_pcol", "rope", "gdn_const", "gdn_bd"):
        m[k] = c[k]
    m["ret_decay"] = np.ascontiguousarray(inputs["ret_decay"])
    rp = inputs["na_rpb"]
    gat = rp[:, :, c["_na_ridx"], c["_na_cidx"]]
    m["na_bias"] = np.ascontiguousarray(gat.transpose(0, 1, 3, 2, 4, 5).reshape(DEPTH, 8, 128, 5, 640))
    return m


def kernel(**inputs):
    inputs = {k: np.asarray(v) for k, v in inputs.items()}
    if "nc" not in _NC_CACHE:
        _NC_CACHE["nc"] = build_program()
    nc = _NC_CACHE["nc"]
    in_maps = [_prep_core_inputs(b, inputs) for b in range(8)]
    res = run_bass_kernel_spmd(nc, in_maps, core_ids=list(range(8)))
    return np.stack([r["out"] for r in res.results], axis=0)
```

```python
import os
import numpy as np
import ml_dtypes
import concourse.bass as bass
import concourse.mybir as mybir
from contextlib import ExitStack
from concourse.bass_utils import run_bass_kernel_spmd

F32 = mybir.dt.float32
BF16 = mybir.dt.bfloat16
F32R = mybir.dt.float32r
ALU = mybir.AluOpType
AF = mybir.ActivationFunctionType
AX = mybir.AxisListType

EPOCH = 16000
N_DMA_SEMS = 40

D = 1024
SEQ = 4096
CTX = 256
NTOK = SEQ + CTX
NT = NTOK // 128
DEPTH = 2
D_IN = 3600
D_FF = 2816
NE = 8
EPS = 1e-6


class Buf:
    __slots__ = ("name", "last_w", "reads", "excl")

    def __init__(self, name="", excl=False):
        self.name = name
        self.last_w = None
        self.reads = []
        self.excl = excl


class Eng:
    def __init__(self, name):
        self.name = name
        self.ops = []
        self.n = 0
        self.sems = []
        self.waited = {}


class Ker:
    def __init__(self, nc, stack):
        self.nc = nc
        self.stack = stack
        self.eng = {n: Eng(n) for n in ("pe", "act", "dve", "pool", "sp")}
        self.dma_sems = []
        self.dma_sem_val = []
        self.dma_rr = 0
        for i in range(N_DMA_SEMS):
            s = stack.enter_context(nc.semaphore(f"dq{i}"))
            self.dma_sems.append(s)
            self.dma_sem_val.append(0)

    def _need(self, e, waits, tok):
        if tok is None:
            return
        sem, val, key = tok[0], tok[1], tok[2]
        if e.waited.get(key, 0) >= val:
            return
        e.waited[key] = val
        waits.append((sem, val))

    def _deps(self, e, reads, writes, pe_acc=False):
        waits = []
        for b in reads:
            self._need(e, waits, b.last_w)
            if b.excl:
                for t in b.reads:
                    if t[3] != e.name:
                        self._need(e, waits, t)
        for b in writes:
            if not (pe_acc and b.last_w is not None and b.last_w[3] == "pe"):
                self._need(e, waits, b.last_w)
            for t in b.reads:
                self._need(e, waits, t)
        return waits

    def _commit(self, tok, reads, writes):
        for b in reads:
            b.reads.append(tok)
            if len(b.reads) > 12:
                d = {}
                for t in b.reads:
                    if t[2] not in d or d[t[2]][1] < t[1]:
                        d[t[2]] = t
                b.reads = list(d.values())
        for b in writes:
            b.last_w = tok
            b.reads = []

    def op(self, engname, fn, reads=(), writes=(), pe_acc=False):
        e = self.eng[engname]
        waits = self._deps(e, reads, writes, pe_acc)
        ep = e.n // EPOCH
        while len(e.sems) <= ep:
            e.sems.append(self.stack.enter_context(self.nc.semaphore(f"s_{engname}{len(e.sems)}")))
        sem = e.sems[ep]
        val = e.n % EPOCH + 1
        tok = (sem, val, (engname, ep), engname)
        e.n += 1
        e.ops.append((waits, fn, (sem, 1)))
        self._commit(tok, reads, writes)
        return tok

    def dma(self, qname, out_ap, in_ap, reads=(), writes=(), **kw):
        e = self.eng[qname]
        waits = self._deps(e, reads, writes)
        i = self.dma_rr
        self.dma_rr = (self.dma_rr + 1) % N_DMA_SEMS
        sem = self.dma_sems[i]
        if self.dma_sem_val[i] > 0:
            self._need(e, waits, (sem, self.dma_sem_val[i], ("dq", i), "dma"))
        self.dma_sem_val[i] += 16
        val = self.dma_sem_val[i]
        tok = (sem, val, ("dq", i), "dma")

        def fn(h, out_ap=out_ap, in_ap=in_ap, kw=kw):
            return h.dma_start(out=out_ap, in_=in_ap, **kw)
        e.ops.append((waits, fn, (sem, 16)))
        self._commit(tok, reads, writes)
        return tok

    def barrier(self):
        toks = []
        for n, e in self.eng.items():
            if e.n > 0:
                ep = (e.n - 1) // EPOCH
                toks.append((e.sems[ep], (e.n - 1) % EPOCH + 1, (n, ep), n))
        for i in range(N_DMA_SEMS):
            if self.dma_sem_val[i] > 0:
                toks.append((self.dma_sems[i], self.dma_sem_val[i], ("dq", i), "dma"))
        for n, e in self.eng.items():
            waits = []
            for t in toks:
                self._need(e, waits, t)
            e.ops.append((waits, None, None))

    def finish(self, final_tokens):
        e = self.eng["sp"]
        waits = []
        for t in final_tokens:
            self._need(e, waits, t)
        e.ops.append((waits, None, None))

    def emit(self):
        nc = self.nc
        with nc.Block() as block:
            def run(e):
                def body(h):
                    for waits, fn, inc in e.ops:
                        for (sem, val) in waits:
                            h.wait_ge(sem, val)
                        if fn is not None:
                            ins = fn(h)
                            if inc is not None:
                                ins.then_inc(inc[0], inc[1])
                return body
            block.tensor(run(self.eng["pe"]))
            block.scalar(run(self.eng["act"]))
            block.vector(run(self.eng["dve"]))
            block.gpsimd(run(self.eng["pool"]))
            block.sync(run(self.eng["sp"]))


_CUR = {"K": None}


class SS(ExitStack):
    def __exit__(self, *a):
        if _CUR["K"] is not None and a[0] is None:
            _CUR["K"].barrier()
        return super().__exit__(*a)


class Tile:
    def __init__(self, t, nslots=1):
        self.t = t
        self.b = [Buf() for _ in range(nslots)]

    def __getitem__(self, k):
        return self.t[k]


class Gen:
    def __init__(self, nc, stack):
        self.nc = nc
        self.st = stack
        self.K = Ker(nc, stack)
        self.psum = []
        self.ps_rr = 0
        for i in range(8):
            t = stack.enter_context(nc.psum_tensor(f"ps{i}", [128, 512], F32))
            tl = Tile(t)
            tl.b[0].excl = True
            self.psum.append(tl)

    def ps(self):
        p = self.psum[self.ps_rr]
        self.ps_rr = (self.ps_rr + 1) % 8
        return p

    def sb(self, name, shape, dt, nslots=1, stack=None):
        self.uid = getattr(self, "uid", 0) + 1
        t = (stack or self.st).enter_context(self.nc.sbuf_tensor(f"{name}_{self.uid}", shape, dt))
        return Tile(t, nslots)

    def mm(self, out, lhsT, rhs, start, stop, R, W):
        return self.K.op("pe", lambda h: h.matmul(out, lhsT, rhs, start=start, stop=stop), reads=R, writes=W,
                         pe_acc=not start)

    def tr(self, out, in_, ident, R, W):
        return self.K.op("pe", lambda h: h.transpose(out, in_, ident), reads=R, writes=W, pe_acc=True)

    def act(self, out, in_, func, R, W, bias=None, scale=1.0, accum_out=None, eng="act"):
        kw = {}
        if bias is not None:
            kw["bias"] = bias
        if accum_out is not None:
            kw["accum_out"] = accum_out
        return self.K.op(eng, lambda h: h.activation(out=out, in_=in_, func=func, scale=scale, **kw), reads=R, writes=W)

    def tt(self, out, in0, in1, op, R, W, eng="dve"):
        return self.K.op(eng, lambda h: h.tensor_tensor(out=out, in0=in0, in1=in1, op=op), reads=R, writes=W)

    def ts(self, out, in0, s1, op0, R, W, s2=None, op1=None, eng="dve", accum_out=None):
        kw = {}
        if op1 is not None:
            kw["op1"] = op1
        if accum_out is not None:
            kw["accum_out"] = accum_out
        return self.K.op(eng, lambda h: h.tensor_scalar(out=out, in0=in0, scalar1=s1, scalar2=s2, op0=op0, **kw),
                         reads=R, writes=W)

    def stt(self, out, in0, scalar, in1, op0, op1, R, W, eng="dve"):
        return self.K.op(eng, lambda h: h.scalar_tensor_tensor(out=out, in0=in0, scalar=scalar, in1=in1, op0=op0, op1=op1),
                         reads=R, writes=W)

    def cp(self, out, in_, R, W, eng="dve"):
        if eng == "act":
            return self.K.op("act", lambda h: h.copy(out=out, in_=in_), reads=R, writes=W)
        return self.K.op(eng, lambda h: h.tensor_copy(out=out, in_=in_), reads=R, writes=W)

    def memset(self, ap, val, W, eng="pool"):
        return self.K.op(eng, lambda h: h.memset(ap, val), writes=W)

    def recip(self, out, in_, R, W):
        return self.K.op("dve", lambda h: h.reciprocal(out=out, in_=in_), reads=R, writes=W)

    def dma(self, out, in_, R=(), W=(), q="sp", **kw):
        return self.K.dma(q, out, in_, reads=R, writes=W, **kw)


class Ctx:
    pass


def phase_adaln(C, nlayers):
    g, I = C.g, C.I
    with SS() as s1:
        craw = g.sb("craw", [128, 2, 8], F32, stack=s1)
        for r in range(2):
            g.dma(craw[:, r, :], I["cvec"][r].rearrange("(p k) -> p k", k=8), W=[craw.b[0]])
        scT = g.sb("scT", [128, 8, 2], F32, stack=s1)
        g.act(scT[:].rearrange("p k r -> p r k"), craw[:], AF.Silu, [craw.b[0]], [scT.b[0]])
        adab = g.sb("adab", [2, 6 * D], F32, stack=s1)
        modsb = g.sb("modsb", [2, 6 * D], F32, stack=s1)
        wch = g.sb("wch", [128, 2, 8, 512], F32, nslots=2, stack=s1)
        it = 0
        for l in range(nlayers):
            g.dma(adab[:], I["ada_b"][l:l + 1, :].partition_broadcast(2), W=[adab.b[0]])
            for n in range(12):
                sl = it % 2
                it += 1
                g.dma(wch[:, sl], I["ada_w"][l].rearrange("(p k) n -> p k n", k=8)[:, :, n * 512:(n + 1) * 512],
                      W=[wch.b[sl]])
                p = g.ps()
                for k in range(8):
                    g.mm(p[0:2, :], scT[:, k, :], wch[:, sl, k, :], k == 0, k == 7, [scT.b[0], wch.b[sl]], [p.b[0]])
                g.tt(modsb[:, n * 512:(n + 1) * 512], p[0:2, :], adab[:, n * 512:(n + 1) * 512], ALU.add,
                     [p.b[0], adab.b[0]], [modsb.b[0]])
            g.dma(C.MOD[l], modsb[:], R=[modsb.b[0]], W=[C.B_MOD[l]])


def norm_mod_tiles(C, l, which, s1):
    g, I = C.g, C.I
    so, co = (0, D) if which == 1 else (3 * D, 4 * D)
    gname = "norm1_g" if which == 1 else "norm2_g"
    Abc = g.sb("Abc", [128, 2, D], F32, stack=s1)
    Sbc = g.sb("Sbc", [128, 2, D], F32, stack=s1)
    gbc = g.sb("gbc", [128, D], F32, stack=s1)
    g.dma(gbc[:], I[gname][l:l + 1, :].partition_broadcast(128), W=[gbc.b[0]])
    for r in range(2):
        g.dma(Abc[:, r, :], C.MOD[l, r:r + 1, co:co + D].partition_broadcast(128), R=[C.B_MOD[l]], W=[Abc.b[0]])
        g.dma(Sbc[:, r, :], C.MOD[l, r:r + 1, so:so + D].partition_broadcast(128), R=[C.B_MOD[l]], W=[Sbc.b[0]])
        g.stt(Abc[:, r, :], Abc[:, r, :], 1.0, gbc[:], ALU.add, ALU.mult, [Abc.b[0], gbc.b[0]], [Abc.b[0]])
    return Abc, Sbc


def norm_mod_transpose(C, xt_ap, xt_b, r, Abc, Sbc, T, sl, dstHT, dstB):
    g = C.g
    sq, ssq, hf, hb, hT = T
    g.act(sq[:], xt_ap, AF.Square, [xt_b], [sq.b[0], ssq.b[sl]], accum_out=ssq[:, sl:sl + 1])
    g.act(ssq[:, sl:sl + 1], ssq[:, sl:sl + 1], AF.Sqrt, [ssq.b[sl]], [ssq.b[sl]], bias=C.epsc[:, 0:1], scale=1.0 / D)
    g.recip(ssq[:, sl:sl + 1], ssq[:, sl:sl + 1], [ssq.b[sl]], [ssq.b[sl]])
    g.stt(hf[:, sl], xt_ap, ssq[:, sl:sl + 1], Abc[:, r, :], ALU.mult, ALU.mult,
          [xt_b, ssq.b[sl], Abc.b[0]], [hf.b[sl]])
    g.tt(hb[:, sl], hf[:, sl], Sbc[:, r, :], ALU.add, [hf.b[sl], Sbc.b[0]], [hb.b[sl]], eng="pool")
    p = g.ps()
    pb = p[:].bitcast(BF16)
    for k in range(8):
        g.tr(pb[:, k * 128:(k + 1) * 128], hb[:, sl, k * 128:(k + 1) * 128], C.ident[:],
             [hb.b[sl], C.ident.b[0]], [p.b[0]])
    g.cp(hT[:, sl], pb, [p.b[0]], [hT.b[sl]], eng="act")
    g.dma(dstHT, hT[:, sl], R=[hT.b[sl]], W=[dstB])


def nm_scratch(C, s1):
    g = C.g
    sq = g.sb("sq", [128, D], F32, stack=s1)
    ssq = g.sb("ssq", [128, 2], F32, nslots=2, stack=s1)
    hf = g.sb("hf", [128, 2, D], F32, nslots=2, stack=s1)
    hb = g.sb("hb", [128, 2, D], BF16, nslots=2, stack=s1)
    hT = g.sb("hT", [128, 2, D], BF16, nslots=2, stack=s1)
    return (sq, ssq, hf, hb, hT)


def phase_norm1(C, l):
    g, I = C.g, C.I
    xsrc = I["xin"] if l == 0 else C.XS
    with SS() as s1:
        Abc, Sbc = norm_mod_tiles(C, l, 1, s1)
        xt = g.sb("xt", [128, 2, D], F32, nslots=2, stack=s1)
        T = nm_scratch(C, s1)
        for t in range(NT):
            sl = t % 2
            r = 1 if t < 2 else 0
            g.dma(xt[:, sl], xsrc[t * 128:(t + 1) * 128, :], R=[C.B_XS[t]] if l > 0 else [], W=[xt.b[sl]])
            norm_mod_transpose(C, xt[:, sl], xt.b[sl], r, Abc, Sbc, T, sl, C.HT[t], C.B_HT[t])


def load_ht_all(C, s1):
    g = C.g
    hts = g.sb("hts", [128, 8, NTOK], BF16, nslots=NT, stack=s1)
    for t in range(NT):
        g.dma(hts[:, :, t * 128:(t + 1) * 128], C.HT[t].rearrange("p (k c) -> p k c", k=8), R=[C.B_HT[t]],
              W=[hts.b[t]])
    return hts


def load_w_bf16(C, name, l, c0, c1, tname, s1, krows=8):
    g = C.g
    w = g.sb(tname, [128, krows, c1 - c0], BF16, stack=s1)
    src = C.I[name][l].rearrange("(k p) n -> p k n", p=128)
    for k0 in range(0, krows, 4):
        k1 = min(krows, k0 + 4)
        g.dma(w[:, k0:k1, :], src[:, k0:k1, c0:c1], W=[w.b[0]], q="pool")
    return w


def phase_na(C, l, need_ctx):
    g, I = C.g, C.I
    QC, KC, VC = 2064, 2576, 3088
    with SS() as s1:
        hts = load_ht_all(C, s1)
        wna = load_w_bf16(C, "w_in", l, QC, D_IN, "wna", s1)
        mask = g.sb("namask", [128, 5, 640], F32, stack=s1)
        g.dma(mask[:], I["na_mask"], W=[mask.b[0]])
        braw = g.sb("nabraw", [128, 5, 640], F32, stack=s1)
        bias = g.sb("nabias", [128, 2, 5, 640], BF16, nslots=2, stack=s1)
        qT = g.sb("naqT", [128, NTOK], BF16, nslots=9, stack=s1)
        kT = g.sb("nakT", [128, NTOK], BF16, nslots=9, stack=s1)
        vx = g.sb("navx", [128, NT, 2, 65], BF16, nslots=NT, stack=s1)
        pT = g.sb("napT", [128, 2, 7, 128], BF16, nslots=2, stack=s1)
        rden = g.sb("narden", [128, 2, 2], F32, nslots=2, stack=s1)
        ona = g.sb("naout", [128, 2, 128], BF16, nslots=2, stack=s1)
        g.memset(vx[:], 1.0, vx.b)
        groups = [(0, 256)] + [(256 + 512 * i, 512) for i in range(8)]
        unit = 0
        for hp in range(4):
            for h2 in range(2):
                h = 2 * hp + h2
                g.dma(braw[:], I["na_bias"][l, h], W=[braw.b[0]])
                g.tt(braw[:], braw[:], mask[:], ALU.add, [braw.b[0], mask.b[0]], [braw.b[0]], eng="pool")
                g.act(bias[:, h2], braw[:], AF.Copy, [braw.b[0]], [bias.b[h2]], scale=8.0)
            for gi, (t0, n) in enumerate(groups):
                tl = list(range(t0 // 128, (t0 + n) // 128))
                for (dst, c0) in ((qT, hp * 128), (kT, (KC - QC) + hp * 128)):
                    p = g.ps()
                    for k in range(8):
                        g.mm(p[:, 0:n], wna[:, k, c0:c0 + 128], hts[:, k, t0:t0 + n], k == 0, k == 7,
                             [wna.b[0]] + [hts.b[t] for t in tl], [p.b[0]])
                    g.cp(dst[:, t0:t0 + n], p[:, 0:n], [p.b[0]], [dst.b[gi]], eng="act" if dst is qT else "dve")
                p = g.ps()
                c0 = (VC - QC) + hp * 128
                for j, t in enumerate(tl):
                    for k in range(8):
                        g.mm(p[:, j * 128:(j + 1) * 128], hts[:, k, t * 128:(t + 1) * 128], wna[:, k, c0:c0 + 128],
                             k == 0, k == 7, [wna.b[0], hts.b[t]], [p.b[0]])
                g.cp(vx[:, tl[0]:tl[-1] + 1, :, 0:64],
                     p[:, 0:n // 128 * 128].rearrange("p (a b c) -> p a b c", b=2, c=64),
                     [p.b[0]], [vx.b[t] for t in tl], eng="dve")

            def tok_group(t):
                return 0 if t < 2 else 1 + (t - 2) // 4

            def attend(qt, local_tiles, cls, h2list=(0, 1)):
                nonlocal unit
                sl = unit % 2
                unit += 1
                po = g.ps()
                for h2 in h2list:
                    base = h2 * 64
                    keyt = list(local_tiles) + [0, 1]
                    nk = len(keyt)
                    banks = [g.ps(), g.ps()] if nk > 4 else [g.ps()]
                    for ci, kt in enumerate(keyt):
                        pb = banks[ci // 4]
                        reg = pb[:, (ci % 4) * 128:(ci % 4 + 1) * 128]
                        has_b = ci < len(local_tiles)
                        g.mm(reg, kT[base:base + 64, kt * 128:(kt + 1) * 128], qT[base:base + 64, qt * 128:(qt + 1) * 128],
                             True, not has_b, [kT.b[tok_group(kt)], qT.b[tok_group(qt)]], [pb.b[0]])
                        if has_b:
                            g.mm(reg, C.ident[:], bias[:, h2, cls, ci * 128:(ci + 1) * 128], False, True,
                                 [C.ident.b[0], bias.b[h2]], [pb.b[0]])
                    for bi, pb in enumerate(banks):
                        n = min(4, nk - 4 * bi) * 128
                        g.act(pT[:, sl, 4 * bi:4 * bi + n // 128, :].rearrange("p a b -> p (a b)") if False else
                              pT[:, sl].rearrange("p a b -> p (a b)")[:, bi * 512:bi * 512 + n],
                              pb[:, 0:n], AF.Exp, [pb.b[0]], [pT.b[sl]], scale=0.125)
                    for ci, kt in enumerate(keyt):
                        g.mm(po[:, h2 * 65:(h2 + 1) * 65], pT[:, sl, ci, :], vx[:, kt, h2, :], ci == 0, ci == nk - 1,
                             [pT.b[sl], vx.b[kt]], [po.b[0]])
                    g.recip(rden[:, sl, h2:h2 + 1], po[:, h2 * 65 + 64:h2 * 65 + 65], [po.b[0]], [rden.b[sl]])
                    g.ts(ona[:, sl, h2 * 64:(h2 + 1) * 64], po[:, h2 * 65:h2 * 65 + 64], rden[:, sl, h2:h2 + 1], ALU.mult,
                         [po.b[0], rden.b[sl]], [ona.b[sl]])
                g.dma(C.O[qt * 128:(qt + 1) * 128, 512 + hp * 128:512 + (hp + 1) * 128], ona[:, sl, :],
                      R=[ona.b[sl]], W=[C.B_O[qt]])

            for m in range(32):
                kp0 = na_kp0(m)
                attend(2 + m, [2 + kp0 + j for j in range(5)], na_cls(m))
            if need_ctx:
                for qt in range(2):
                    attend(qt, [], 0)


def silu_from(g, out, outW, x, xR, tmp, tmpB):
    g.act(tmp, x, AF.Exp, xR, [tmpB], scale=-1.0)
    g.ts(tmp, tmp, 1.0, ALU.add, [tmpB], [tmpB])
    g.recip(tmp, tmp, [tmpB], [tmpB])
    g.tt(out, x, tmp, ALU.mult, list(xR) + [tmpB], outW)


def phase_ret(C, l, need_ctx):
    g, I = C.g, C.I
    with SS() as s1:
        lgb = g.sb("lgb", [128, 8], F32, stack=s1)
        g.dma(lgb[:], I["ret_decay"][l:l + 1].rearrange("o a b -> o (a b)").partition_broadcast(128), W=[lgb.b[0]])
        g.act(lgb[:], lgb[:], AF.Exp, [lgb.b[0]], [lgb.b[0]], scale=-float(np.log(2.0)))
        g.act(lgb[:], lgb[:], AF.Ln, [lgb.b[0]], [lgb.b[0]], scale=-1.0, bias=C.onec[:, 0:1])
        cst = g.sb("retc", [128, 6, 128], F32, stack=s1)
        g.dma(cst[:], I["ret_const"], W=[cst.b[0]])
        pcol = g.sb("retpc", [128, 2, 64], F32, stack=s1)
        g.dma(pcol[:], I["ret_pcol"], W=[pcol.b[0]])
        c128 = g.sb("retc128", [128, 128], F32, stack=s1)
        g.memset(c128[:], 128.0, [c128.b[0]])
        BD = g.sb("retBD", [128, 2, 128], F32, stack=s1)
        g.memset(BD[:], 0.0, [BD.b[0]])
        g.memset(BD[0:64, :, 0:64], 1.0, [BD.b[0]])
        g.memset(BD[64:128, :, 64:128], 1.0, [BD.b[0]])
        MT = g.sb("retMT", [128, 2, 2, 128], BF16, stack=s1)
        g.memset(MT[:], 0.0, [MT.b[0]])
        g.memset(MT[0:64, 0], 1.0, [MT.b[0]])
        g.memset(MT[64:128, 1], 1.0, [MT.b[0]])
        D2 = g.sb("retD2", [128, 4, 128], F32, stack=s1)
        tmpd = g.sb("rettmp", [128, 128], F32, stack=s1)
        QF = g.sb("retQF", [128, 2, 128], F32, stack=s1)
        QB = g.sb("retQB", [128, 2, 128], F32, stack=s1)
        KF = g.sb("retKF", [128, 256], F32, stack=s1)
        KB = g.sb("retKB", [128, 256], F32, stack=s1)
        CDF = g.sb("retCDF", [128, 2, 128], F32, stack=s1)
        CDB = g.sb("retCDB", [128, 2, 128], F32, stack=s1)
        R0 = [lgb.b[0], cst.b[0]]
        for h in range(4):
            f, b = lgb[:, h:h + 1], lgb[:, 4 + h:5 + h]
            g.act(D2[:, h, :], cst[:, 0, :], AF.Exp, R0, [D2.b[0]], scale=f)
            g.tt(D2[:, h, :], D2[:, h, :], cst[:, 2, :], ALU.mult, [D2.b[0], cst.b[0]], [D2.b[0]])
            g.act(tmpd[:], cst[:, 1, :], AF.Exp, R0, [tmpd.b[0]], scale=b)
            g.tt(tmpd[:], tmpd[:], cst[:, 3, :], ALU.mult, [tmpd.b[0], cst.b[0]], [tmpd.b[0]])
            g.tt(D2[:, h, :], D2[:, h, :], tmpd[:], ALU.add, [D2.b[0], tmpd.b[0]], [D2.b[0]])
            g.ts(D2[:, h, :], D2[:, h, :], 0.125, ALU.mult, [D2.b[0]], [D2.b[0]])
            pr, bs = h // 2, (h % 2) * 64
            g.act(QF[bs:bs + 64, pr, :], cst[bs:bs + 64, 4, :], AF.Exp, R0, [QF.b[0]], scale=lgb[bs:bs + 64, h:h + 1])
            g.act(QB[bs:bs + 64, pr, :], cst[bs:bs + 64, 5, :], AF.Exp, R0, [QB.b[0]], scale=lgb[bs:bs + 64, 4 + h:5 + h])
            g.act(KF[:, h * 64:(h + 1) * 64], pcol[:, 0, :], AF.Exp, [lgb.b[0], pcol.b[0]], [KF.b[0]], scale=f)
            g.act(KB[:, h * 64:(h + 1) * 64], pcol[:, 1, :], AF.Exp, [lgb.b[0], pcol.b[0]], [KB.b[0]], scale=b)
            g.act(CDF[bs:bs + 64, pr, :], c128[bs:bs + 64, :], AF.Exp, [lgb.b[0], c128.b[0]], [CDF.b[0]],
                  scale=lgb[bs:bs + 64, h:h + 1])
            g.act(CDB[bs:bs + 64, pr, :], c128[bs:bs + 64, :], AF.Exp, [lgb.b[0], c128.b[0]], [CDB.b[0]],
                  scale=lgb[bs:bs + 64, 4 + h:5 + h])
        g.ts(KF[:], KF[:], 0.125, ALU.mult, [KF.b[0]], [KF.b[0]])
        g.ts(KB[:], KB[:], 0.125, ALU.mult, [KB.b[0]], [KB.b[0]])

        import os
        RS = int(os.environ.get("RET_STOP", "9"))
        if RS <= 1:
            return
        qkT = g.sb("retqkT", [128, NT, 4, 128], BF16, nslots=NT, stack=s1)
        vall = g.sb("retv", [128, NT, 256], BF16, nslots=NT, stack=s1)
        sg = g.sb("retsg", [128, NT, 256], BF16, nslots=NT, stack=s1)
        SinF = g.sb("retSinF", [128, NT, 2, 128], BF16, nslots=NT, stack=s1)
        SinB = g.sb("retSinB", [128, NT, 2, 128], BF16, nslots=NT, stack=s1)
        with SS() as s2:
            wret = load_w_bf16(C, "w_in", l, 0, 1024, "wret", s2)
            kdf = g.sb("retkdf", [128, NT, 256], BF16, nslots=NT, stack=s2)
            kdb = g.sb("retkdb", [128, NT, 256], BF16, nslots=NT, stack=s2)
            ht = g.sb("retht", [128, 2, 8, 128], BF16, nslots=2, stack=s2)
            rope = g.sb("retrope", [128, 2, 2, 256], F32, nslots=2, stack=s2)
            qk32 = g.sb("retqk32", [128, 512], F32, stack=s2)
            ra = g.sb("retra", [128, 4, 256], F32, nslots=2, stack=s2)
            qkr = g.sb("retqkr", [128, 2, 512], BF16, nslots=2, stack=s2)
            for t in range(NT):
                sl = t % 2
                g.dma(ht[:, sl], C.HT[t].rearrange("p (k c) -> p k c", k=8), R=[C.B_HT[t]], W=[ht.b[sl]])
                p0, p1 = g.ps(), g.ps()
                for (p, n0) in ((p0, 0), (p1, 512)):
                    for k in range(8):
                        g.mm(p[:, :], ht[:, sl, k, :], wret[:, k, n0:n0 + 512], k == 0, k == 7,
                             [ht.b[sl], wret.b[0]], [p.b[0]])
                SUB = int(os.environ.get("RET_SUB", "9"))
                if SUB <= 0:
                    continue
                if os.environ.get("RET_V", "1") == "1":
                    g.cp(vall[:, t, :], p1[:, 0:256], [p1.b[0]], [vall.b[t]], eng="dve")
                if os.environ.get("RET_G", "1") == "1":
                    silu_from(g, sg[:, t, :], [sg.b[t]], p1[:, 256:512], [p1.b[0]], qk32[:, 0:256], qk32.b[0])
                if SUB <= 1:
                    continue
                if t >= 2:
                    g.dma(rope[:, sl], I["rope"][(t - 2) * 128:(t - 1) * 128], W=[rope.b[sl]])
                    g.cp(qk32[:], p0[:, :], [p0.b[0]], [qk32.b[0]], eng="act")
                    v4 = qk32[:].rearrange("p (h a c) -> p h a c", a=2, c=32)
                    t1, t2 = v4[:, :, 0, :], v4[:, :, 1, :]
                    cs = rope[:, sl, 0, :].rearrange("p (h c) -> p h c", c=32)
                    sn = rope[:, sl, 1, :].rearrange("p (h c) -> p h c", c=32)
                    rv = [ra[:, i, :].rearrange("p (h c) -> p h c", c=32) for i in range(4)]
                    RR = [qk32.b[0], rope.b[sl]]
                    g.tt(rv[0], t1, cs, ALU.mult, RR, [ra.b[0]], eng="dve")
                    g.tt(rv[1], t2, sn, ALU.mult, RR, [ra.b[0]], eng="pool")
                    g.tt(rv[2], t1, sn, ALU.mult, RR, [ra.b[1]], eng="dve")
                    g.tt(rv[3], t2, cs, ALU.mult, RR, [ra.b[1]], eng="pool")
                    o4 = qkr[:, sl, :].rearrange("p (h a c) -> p h a c", a=2, c=32)
                    g.tt(o4[:, :, 0, :], rv[0], rv[1], ALU.subtract, [ra.b[0]], [qkr.b[sl]], eng="dve")
                    g.tt(o4[:, :, 1, :], rv[2], rv[3], ALU.add, [ra.b[1]], [qkr.b[sl]], eng="pool")
                else:
                    g.cp(qkr[:, sl, :], p0[:, :], [p0.b[0]], [qkr.b[sl]], eng="act")
                if SUB <= 2:
                    continue
                g.tt(kdf[:, t, :], qkr[:, sl, 256:512], KF[:], ALU.mult, [qkr.b[sl], KF.b[0]], [kdf.b[t]], eng="dve")
                g.tt(kdb[:, t, :], qkr[:, sl, 256:512], KB[:], ALU.mult, [qkr.b[sl], KB.b[0]], [kdb.b[t]], eng="pool")
                if SUB <= 3:
                    continue
                pt = g.ps()
                ptb = pt[:].bitcast(BF16)
                for c4 in range(4):
                    g.tr(ptb[:, c4 * 128:(c4 + 1) * 128], qkr[:, sl, c4 * 128:(c4 + 1) * 128], C.ident[:],
                         [qkr.b[sl], C.ident.b[0]], [pt.b[0]])
                g.cp(qkT[:, t].rearrange("p a b -> p (a b)"), ptb[:, 0:512], [pt.b[0]], [qkT.b[t]], eng="act")

            if RS <= 2:
                return
            S = g.sb("retS", [128, 2, 2, 128], F32, nslots=2, stack=s2)
            tS = g.sb("rettS", [128, 2, 2, 128], F32, nslots=2, stack=s2)
            g.memset(S[:], 0.0, S.b)
            order_f = list(range(NT))
            order_b = [1, 0] + list(range(NT - 1, 1, -1))
            for step in range(NT):
                for d, (order, kd, Sin, CDt) in enumerate(((order_f, kdf, SinF, CDF), (order_b, kdb, SinB, CDB))):
                    t = order[step]
                    p = g.ps()
                    for pr in range(2):
                        g.mm(p[:, pr * 128:(pr + 1) * 128], kd[:, t, pr * 128:(pr + 1) * 128], vall[:, t, pr * 128:(pr + 1) * 128],
                             True, True, [kd.b[t], vall.b[t]], [p.b[0]])
                    g.tt(tS[:, d].rearrange("p a b -> p (a b)"), p[:, 0:256], BD[:].rearrange("p a b -> p (a b)"), ALU.mult,
                         [p.b[0], BD.b[0]], [tS.b[d]], eng="dve")
                    g.cp(Sin[:, t], S[:, d], [S.b[d]], [Sin.b[t]], eng="act")
                    g.tt(S[:, d], S[:, d], CDt[:], ALU.mult, [S.b[d], CDt.b[0]], [S.b[d]], eng="pool")
                    g.tt(S[:, d], S[:, d], tS[:, d], ALU.add, [S.b[d], tS.b[d]], [S.b[d]], eng="pool")

        if RS <= 3:
            return
        with SS() as s2:
            AT = g.sb("retAT", [128, 2, 4, 128], BF16, nslots=2, stack=s2)
            qm = g.sb("retqm", [128, 2, 4, 128], BF16, nslots=2, stack=s2)
            qsf = g.sb("retqsf", [128, 2, 2, 128], BF16, nslots=2, stack=s2)
            qsb = g.sb("retqsb", [128, 2, 2, 128], BF16, nslots=2, stack=s2)
            o32 = g.sb("reto32", [128, 2, 256], F32, nslots=2, stack=s2)
            osq = g.sb("retosq", [128, 256], F32, stack=s2)
            rs = g.sb("retrs", [128, 2, 4], F32, nslots=2, stack=s2)
            ob = g.sb("retob", [128, 2, 256], BF16, nslots=2, stack=s2)
            for t in range(NT):
                if t < 2 and not need_ctx:
                    continue
                sl = t % 2
                for par in range(2):
                    g.tt(qm[:, sl, 2 * par:2 * par + 2, :], qkT[:, t, 0:2, :], MT[:, par], ALU.mult, [qkT.b[t], MT.b[0]],
                         [qm.b[sl]], eng="pool")
                ps_ = g.ps()
                for h in range(4):
                    pr, par = h // 2, h % 2
                    g.mm(ps_[:, h * 128:(h + 1) * 128], qkT[:, t, 2 + pr, :], qm[:, sl, 2 * par + pr, :], True, True,
                         [qkT.b[t], qm.b[sl]], [ps_.b[0]])
                g.tt(AT[:, sl].rearrange("p a b -> p (a b)"), ps_[:, :], D2[:].rearrange("p a b -> p (a b)"), ALU.mult,
                     [ps_.b[0], D2.b[0]], [AT.b[sl]], eng="dve")
                g.tt(qsf[:, sl], qkT[:, t, 0:2, :], QF[:], ALU.mult, [qkT.b[t], QF.b[0]], [qsf.b[sl]], eng="pool")
                g.tt(qsb[:, sl], qkT[:, t, 0:2, :], QB[:], ALU.mult, [qkT.b[t], QB.b[0]], [qsb.b[sl]], eng="pool")
                po = g.ps()
                for pr in range(2):
                    reg = po[:, pr * 128:(pr + 1) * 128]
                    g.mm(reg, qsf[:, sl, pr, :], SinF[:, t, pr, :], True, False, [qsf.b[sl], SinF.b[t]], [po.b[0]])
                    g.mm(reg, qsb[:, sl, pr, :], SinB[:, t, pr, :], False, False, [qsb.b[sl], SinB.b[t]], [po.b[0]])
                    for par in range(2):
                        h = 2 * pr + par
                        g.mm(po[:, h * 64:(h + 1) * 64], AT[:, sl, h, :], vall[:, t, h * 64:(h + 1) * 64], False, par == 1,
                             [AT.b[sl], vall.b[t]], [po.b[0]])
                g.cp(o32[:, sl, :], po[:, 0:256], [po.b[0]], [o32.b[sl]], eng="act")
                g.tt(osq[:], o32[:, sl, :], o32[:, sl, :], ALU.mult, [o32.b[sl]], [osq.b[0]], eng="pool")
                g.K.op("dve", lambda h_, o_=rs[:, sl, :], i_=osq[:].rearrange("p (h c) -> p h c", c=64):
                       h_.tensor_reduce(out=o_, in_=i_, axis=AX.X, op=ALU.add), reads=[osq.b[0]], writes=[rs.b[sl]])
                g.act(rs[:, sl, :], rs[:, sl, :], AF.Sqrt, [rs.b[sl]], [rs.b[sl]], bias=C.epsc[:, 0:1], scale=1.0 / 64)
                g.recip(rs[:, sl, :], rs[:, sl, :], [rs.b[sl]], [rs.b[sl]])
                for h in range(4):
                    g.stt(ob[:, sl, h * 64:(h + 1) * 64], o32[:, sl, h * 64:(h + 1) * 64], rs[:, sl, h:h + 1],
                          sg[:, t, h * 64:(h + 1) * 64], ALU.mult, ALU.mult, [o32.b[sl], rs.b[sl], sg.b[t]], [ob.b[sl]])
                g.dma(C.O[t * 128:(t + 1) * 128, 0:256], ob[:, sl, :], R=[ob.b[sl]], W=[C.B_O[t]])


def phase_gdn(C, l, need_ctx):
    g, I = C.g, C.I
    NCH = NTOK // 64
    order = [list(range(NCH)), [3, 2, 1, 0] + list(range(NCH - 1, 3, -1))]
    id64 = C.ident[0:64, 0:64]
    with SS() as s1:
        cst = g.sb("gdnc", [64, 13, 64], F32, stack=s1)
        g.dma(cst[:], I["gdn_const"], W=[cst.b[0]])
        CB = cst.b[0]
        Y4, SU4, I4 = cst[:, 0:4, :], cst[:, 4:8, :], cst[:, 8:12, :]
        idf, ones = cst[:, 8, :], cst[:, 12, :]
        one64 = C.onec[0:64, 0:1]
        eps64 = C.epsc[0:64, 0:1]
        ab = g.sb("gab", [64, 16, NCH], F32, stack=s1)
        with SS() as s2:
            ht = g.sb("ght", [128, 2, 8, 128], BF16, nslots=2, stack=s2)
            wab = load_w_bf16(C, "w_in", l, 2048, 2064, "wab", s2)
            p = None
            for t in range(NT):
                sl = t % 2
                g.dma(ht[:, sl], C.HT[t].rearrange("p (k c) -> p k c", k=8), R=[C.B_HT[t]], W=[ht.b[sl]])
                for half in range(2):
                    c = 2 * t + half
                    if c % 32 == 0:
                        p = g.ps()
                        c0 = c
                    reg = p[0:64, (c - c0) * 16:(c - c0 + 1) * 16]
                    for k in range(8):
                        g.mm(reg, ht[:, sl, k, half * 64:(half + 1) * 64], wab[:, k, 0:16], k == 0, k == 7,
                             [ht.b[sl], wab.b[0]], [p.b[0]])
                    if c % 32 == 31 or c == NCH - 1:
                        n = c - c0 + 1
                        g.cp(ab[:, :, c0:c0 + n].rearrange("p k c -> p c k"),
                             p[0:64, 0:n * 16].rearrange("p (c k) -> p c k", k=16), [p.b[0]], [ab.b[0]], eng="dve")
        prm = g.sb("gprm", [64, 2, 8], F32, stack=s1)
        g.dma(prm[:, 0, :], I["gdn_a_log"][l:l + 1].rearrange("o a b -> o (a b)").partition_broadcast(64), W=[prm.b[0]])
        g.dma(prm[:, 1, :], I["gdn_dt_bias"][l:l + 1].rearrange("o a b -> o (a b)").partition_broadcast(64), W=[prm.b[0]])
        sc = {}
        for nm in ("G", "beta", "nbeta", "egi", "erem", "egl", "begi"):
            sc[nm] = g.sb("g" + nm, [64, 8, NCH], F32, stack=s1)
        G, beta, nbeta, egi, erem, egl, begi = [sc[k] for k in ("G", "beta", "nbeta", "egi", "erem", "egl", "begi")]
        bc8 = lambda ap: ap.unsqueeze(2).to_broadcast([64, 8, NCH])
        g.tt(G[:], ab[:, 0:8, :], bc8(prm[:, 1, :]), ALU.add, [ab.b[0], prm.b[0]], [G.b[0]])
        g.act(G[:], G[:], AF.Exp, [G.b[0]], [G.b[0]])
        g.act(G[:], G[:], AF.Ln, [G.b[0]], [G.b[0]], bias=one64)
        g.act(prm[:, 0, :], prm[:, 0, :], AF.Exp, [prm.b[0]], [prm.b[0]])
        g.ts(prm[:, 0, :], prm[:, 0, :], -1.0, ALU.mult, [prm.b[0]], [prm.b[0]])
        g.tt(G[:], G[:], bc8(prm[:, 0, :]), ALU.mult, [G.b[0], prm.b[0]], [G.b[0]])
        g.act(beta[:], ab[:, 8:16, :], AF.Exp, [ab.b[0]], [beta.b[0]], scale=-1.0)
        g.ts(beta[:], beta[:], 1.0, ALU.add, [beta.b[0]], [beta.b[0]])
        g.recip(beta[:], beta[:], [beta.b[0]], [beta.b[0]])
        g.ts(nbeta[:], beta[:], -1.0, ALU.mult, [beta.b[0]], [nbeta.b[0]])
        for d in range(2):
            rhs = G[:, d * 4:(d + 1) * 4, :].rearrange("p a c -> p (a c)")
            for (dst, lhs) in ((egi, cst[:, d, :]), (erem, cst[:, 4 + d, :]), (egl, ones)):
                p = g.ps()
                g.mm(p[0:64, 0:4 * NCH], lhs, rhs, True, True, [CB, G.b[0]], [p.b[0]])
                g.act(dst[:, d * 4:(d + 1) * 4, :].rearrange("p a c -> p (a c)"), p[0:64, 0:4 * NCH], AF.Exp, [p.b[0]],
                      [dst.b[0]])
        g.tt(begi[:], beta[:], egi[:], ALU.mult, [beta.b[0], egi.b[0]], [begi.b[0]])
        ngb = g.sb("gngb", [64, 64], F32, stack=s1)
        g.dma(ngb[:], I["gdn_norm_g"][l:l + 1, :].partition_broadcast(64), W=[ngb.b[0]])

        for hp in range(2):
            with SS() as s2:
                qT = g.sb("gqT", [64, 2, NTOK], BF16, nslots=2, stack=s2)
                kT = g.sb("gkT", [64, 2, NTOK], BF16, nslots=2, stack=s2)
                ktm = g.sb("gktm", [64, 2, NCH, 64], BF16, nslots=2, stack=s2)
                vtm = g.sb("gvtm", [64, 2, NCH, 64], BF16, nslots=2, stack=s2)
                with SS() as s3:
                    wg = load_w_bf16(C, "w_in", l, 1024, 2048, "wg", s3)
                    raw = g.sb("graw", [128, NTOK + 8], F32, stack=s3)
                    acc = g.sb("gacc", [128, NTOK], F32, stack=s3)
                    o128 = g.sb("go128", [128, NTOK], BF16, stack=s3)
                    hg = g.sb("ghg", [128, 2, 8, 512], BF16, nslots=2, stack=s3)
                    cw = g.sb("gcw", [128, 3, 5], F32, stack=s3)
                    sq = g.sb("gsq", [128, 2, 512], F32, nslots=2, stack=s3)
                    bd1 = g.sb("gbd1", [128, 128], F32, stack=s3)
                    g.dma(bd1[:], I["gdn_bd"], W=[bd1.b[0]])
                    g.memset(raw[:], 0.0, [raw.b[0]])
                    g.dma(cw[:], I["gdn_convw"][l, 2 * hp:2 * hp + 2].rearrange("h d t k -> (h d) t k"), W=[cw.b[0]])
                    groups = [(0, 256)] + [(256 + 512 * i, 512) for i in range(8)]
                    git = 0
                    for typ in range(3):
                        wc0 = typ * 256 + hp * 128
                        for (t0, n) in groups:
                            sl = git % 2
                            git += 1
                            for q in range(n // 128):
                                t = t0 // 128 + q
                                g.dma(hg[:, sl, :, q * 128:(q + 1) * 128], C.HT[t].rearrange("p (k c) -> p k c", k=8),
                                      R=[C.B_HT[t]], W=[hg.b[sl]])
                            p = g.ps()
                            for k in range(8):
                                g.mm(p[:, 0:n], wg[:, k, wc0:wc0 + 128], hg[:, sl, k, 0:n], k == 0, k == 7,
                                     [wg.b[0], hg.b[sl]], [p.b[0]])
                            off = t0 + 2 if t0 < 256 else t0 + 6
                            g.cp(raw[:, off:off + n], p[:, 0:n], [p.b[0]], [raw.b[0]], eng="act")
                        for (a0, n, r0) in ((0, 256, 0), (256, 2048, 260), (2304, 2048, 2308)):
                            for k in range(5):
                                src = raw[:, r0 + k:r0 + k + n]
                                if k == 0:
                                    g.ts(acc[:, a0:a0 + n], src, cw[:, typ, 0:1], ALU.mult, [raw.b[0], cw.b[0]], [acc.b[0]])
                                else:
                                    g.stt(acc[:, a0:a0 + n], src, cw[:, typ, k:k + 1], acc[:, a0:a0 + n], ALU.mult, ALU.add,
                                          [raw.b[0], cw.b[0], acc.b[0]], [acc.b[0]])
                        g.act(acc[:], acc[:], AF.Silu, [acc.b[0]], [acc.b[0]])
                        if typ == 2:
                            g.cp(o128[:], acc[:], [acc.b[0]], [o128.b[0]], eng="pool")
                        else:
                            for gi2, (t0, n) in enumerate(groups):
                                ss = gi2 % 2
                                g.tt(sq[:, ss, 0:n], acc[:, t0:t0 + n], acc[:, t0:t0 + n], ALU.mult, [acc.b[0]], [sq.b[ss]],
                                     eng="pool")
                                p = g.ps()
                                g.mm(p[:, 0:n], bd1[:], sq[:, ss, 0:n], True, True, [bd1.b[0], sq.b[ss]], [p.b[0]])
                                g.act(sq[:, ss, 0:n], p[:, 0:n], AF.Sqrt, [p.b[0]], [sq.b[ss]], bias=C.epsc[:, 0:1])
                                g.recip(sq[:, ss, 0:n], sq[:, ss, 0:n], [sq.b[ss]], [sq.b[ss]])
                                g.stt(o128[:, t0:t0 + n], acc[:, t0:t0 + n], 0.125 if typ == 0 else 1.0, sq[:, ss, 0:n],
                                      ALU.mult, ALU.mult, [acc.b[0], sq.b[ss]], [o128.b[0]])
                            dst = qT if typ == 0 else kT
                            for hh in range(2):
                                g.dma(dst[:, hh, :], o128[hh * 64:(hh + 1) * 64, :], R=[o128.b[0]], W=[dst.b[hh]])
                        if typ >= 1:
                            dst_tm = ktm if typ == 1 else vtm
                            for c0 in range(0, NCH, 8):
                                nn = min(8, NCH - c0)
                                p = g.ps()
                                pb = p[:].bitcast(BF16)
                                for ci in range(nn):
                                    c = c0 + ci
                                    g.tr(pb[0:64, ci * 128:(ci + 1) * 128], o128[:, c * 64:(c + 1) * 64], C.ident[:],
                                         [o128.b[0], C.ident.b[0]], [p.b[0]])
                                g.cp(dst_tm[:, :, c0:c0 + nn, :], pb[0:64, 0:nn * 128].rearrange("p (c h d) -> p h c d", h=2, d=64),
                                     [p.b[0]], [dst_tm.b[0], dst_tm.b[1]], eng="act")
                    sgt = g.sb("gsgt", [64, 2, NCH, 64], BF16, stack=s3)
                    ht2 = g.sb("ght2", [128, 2, 8, 128], BF16, nslots=2, stack=s3)
                    sg32 = g.sb("gsg32", [64, 512], F32, stack=s3)
                    p = None
                    for t in range(NT):
                        sl = t % 2
                        g.dma(ht2[:, sl], C.HT[t].rearrange("p (k c) -> p k c", k=8), R=[C.B_HT[t]], W=[ht2.b[sl]])
                        for half in range(2):
                            c = 2 * t + half
                            if c % 4 == 0:
                                p = g.ps()
                                c0 = c
                            reg = p[0:64, (c - c0) * 128:(c - c0 + 1) * 128]
                            for k in range(8):
                                g.mm(reg, ht2[:, sl, k, half * 64:(half + 1) * 64], wg[:, k, 768 + hp * 128:768 + (hp + 1) * 128],
                                     k == 0, k == 7, [ht2.b[sl], wg.b[0]], [p.b[0]])
                            if c % 4 == 3:
                                g.cp(sg32[:], p[0:64, :], [p.b[0]], [sg32.b[0]], eng="act")
                                g.act(sgt[:, :, c0:c0 + 4, :], sg32[:].rearrange("p (c h d) -> p h c d", h=2, d=64), AF.Silu,
                                      [sg32.b[0]], [sgt.b[0]])
                    g.dma(C.SGT, sgt[:], R=[sgt.b[0]], W=[C.B_SGT])

                Oacc = g.sb("gOacc", [64, 2, NCH, 64], F32, nslots=2, stack=s2)
                with SS() as s3:
                    NSET, RS_ = 4, 5

                    def tn(nm, dt, ns, w=64, extra=()):
                        return g.sb(nm, [64, ns] + list(extra) + [4, w] if not extra else [64, ns] + list(extra), dt, nslots=ns, stack=s3)
                    X4 = tn("gX4", F32, NSET)
                    E8 = g.sb("gE8", [64, NSET, 4, 2, 64], F32, nslots=NSET, stack=s3)
                    DT4, DS4, P0, P0T, TT = [tn(n_, F32, NSET) for n_ in ("gDT4", "gDS4", "gP0", "gP0T", "gTT")]
                    PP = g.sb("gPP", [64, NSET * 2, 4, 2, 64], F32, nslots=NSET * 2, stack=s3)
                    TTb, vb4, kbeg4 = [tn(n_, BF16, NSET) for n_ in ("gTTb", "gvb4", "gkbeg4")]
                    attn4, wTb, kdec4 = [tn(n_, BF16, RS_) for n_ in ("gattn4", "gwTb", "gkdec4")]
                    U4 = tn("gU4", F32, RS_)
                    S = g.sb("gS", [64, 4, 64], F32, stack=s3)
                    Sb = g.sb("gSb", [64, 4, 64], BF16, stack=s3)
                    vn4 = g.sb("gvn4", [64, 4, 64], BF16, stack=s3)
                    qs4 = g.sb("gqs4", [64, 4, 64], F32, stack=s3)
                    g.memset(S[:], 0.0, [S.b[0]])
                    g.memset(Sb[:], 0.0, [Sb.b[0]])
                    idr = g.sb("gidr", [64, 64], F32, stack=s3)
                    g.cp(idr[:].bitcast(F32R), idf, [CB], [idr.b[0]])
                    RR_ = lambda ap: ap.bitcast(F32R)
                    visited = set()
                    units = [(hh, d) for hh in range(2) for d in range(2)]

                    def col(tile_, u, step):
                        hh, d = units[u]
                        c = order[d][step]
                        dh = d * 4 + 2 * hp + hh
                        return tile_[:, dh, c:c + 1]

                    def indep(step):
                        ts_ = step % NSET
                        sl = step % RS_
                        cs = [order[d][step] for (hh, d) in units]
                        for u in range(4):
                            g.ts(X4[:, ts_, u, :], SU4[:, u, :], col(G, u, step), ALU.mult, [CB, G.b[0]], [X4.b[ts_]])
                        yield
                        pa, pbk = g.ps(), g.ps()
                        pav = pa[0:64, :].rearrange("p (u t i) -> p u t i", u=4, t=2)
                        pbv = pbk[0:64, :].rearrange("p (u t i) -> p u t i", u=4, t=2)
                        for u, (hh, d) in enumerate(units):
                            g.mm(pav[:, u, 0, :], X4[:, ts_, u, :], cst[:, d, :], True, True, [X4.b[ts_], CB], [pa.b[0]])
                            g.mm(pav[:, u, 1, :], cst[:, d, :], X4[:, ts_, u, :], True, True, [X4.b[ts_], CB], [pa.b[0]])
                        for u, (hh, d) in enumerate(units):
                            c = cs[u]
                            kc, qc = kT[:, hh, c * 64:(c + 1) * 64], qT[:, hh, c * 64:(c + 1) * 64]
                            g.mm(pbv[:, u, 0, :], kc, qc, True, True, [kT.b[hh], qT.b[hh]], [pbk.b[0]])
                            g.mm(pbv[:, u, 1, :], kc, kc, True, True, [kT.b[hh]], [pbk.b[0]])
                        yield
                        g.act(E8[:, ts_].rearrange("p u t i -> p (u t i)"), pa[0:64, :], AF.Exp, [pa.b[0]], [E8.b[ts_]])
                        yield
                        g.tt(DT4[:, ts_], E8[:, ts_, :, 0, :], Y4, ALU.mult, [E8.b[ts_], CB], [DT4.b[ts_]], eng="pool")
                        g.tt(DS4[:, ts_], E8[:, ts_, :, 1, :], SU4, ALU.mult, [E8.b[ts_], CB], [DS4.b[ts_]], eng="pool")
                        yield
                        for u in range(4):
                            g.stt(RR_(P0[:, ts_, u, :]), pbv[:, u, 1, :], col(nbeta, u, step), DS4[:, ts_, u, :], ALU.mult, ALU.mult,
                                  [pbk.b[0], nbeta.b[0], DS4.b[ts_]], [P0.b[ts_]])
                        g.tt(attn4[:, sl], pbv[:, :, 0, :], DT4[:, ts_], ALU.mult, [pbk.b[0], DT4.b[ts_]], [attn4.b[sl]])
                        yield
                        pc = g.ps()
                        pcv = pc[0:64, 0:256].rearrange("p (u i) -> p u i", u=4)
                        for u in range(4):
                            g.mm(pcv[:, u, :], RR_(P0[:, ts_, u, :]), RR_(idr[:]), True, True, [P0.b[ts_], idr.b[0]], [pc.b[0]])
                        yield
                        g.cp(RR_(P0T[:, ts_].rearrange("p u i -> p (u i)")), pc[0:64, 0:256], [pc.b[0]], [P0T.b[ts_]], eng="act")
                        yield
                        g.tt(RR_(TT[:, ts_]), P0T[:, ts_], I4, ALU.add, [P0T.b[ts_], CB], [TT.b[ts_]], eng="pool")
                        Pk, PkT, PkB = P0[:, ts_], P0T[:, ts_], [P0.b[ts_], P0T.b[ts_]]

                        def sq_mm(k, Pk, PkT, PkB):
                            pd = g.ps()
                            pdv = pd[0:64, :].rearrange("p (u t i) -> p u t i", u=4, t=2)
                            for u in range(4):
                                g.mm(pdv[:, u, 0, :], RR_(PkT[:, u, :]), RR_(Pk[:, u, :]), True, True, PkB, [pd.b[0]])
                                if k < 4:
                                    g.mm(pdv[:, u, 1, :], RR_(Pk[:, u, :]), RR_(PkT[:, u, :]), True, True, PkB, [pd.b[0]])
                            return pd

                        pd = sq_mm(0, Pk, PkT, PkB)
                        yield
                        for k in range(5):
                            ps_ = ts_ * 2 + k % 2
                            g.cp(RR_(PP[:, ps_].rearrange("p u t i -> p (u t i)")), pd[0:64, :], [pd.b[0]], [PP.b[ps_]], eng="act")
                            yield
                            Pk, PkT, PkB = PP[:, ps_, :, 0, :], PP[:, ps_, :, 1, :], [PP.b[ps_]]
                            if k < 4:
                                pd = sq_mm(k + 1, Pk, PkT, PkB)
                            pe_ = g.ps()
                            pev = pe_[0:64, 0:256].rearrange("p (u i) -> p u i", u=4)
                            for u in range(4):
                                g.mm(pev[:, u, :], RR_(Pk[:, u, :]), RR_(TT[:, ts_, u, :]), True, True, PkB + [TT.b[ts_]], [pe_.b[0]])
                            yield
                            g.tt(RR_(TT[:, ts_]), TT[:, ts_], pev, ALU.add, [TT.b[ts_], pe_.b[0]], [TT.b[ts_]])
                        g.cp(TTb[:, ts_], TT[:, ts_], [TT.b[ts_]], [TTb.b[ts_]], eng="act")
                        for u, (hh, d) in enumerate(units):
                            c = cs[u]
                            g.ts(vb4[:, ts_, u, :], vtm[:, hh, c, :], col(beta, u, step), ALU.mult, [vtm.b[hh], beta.b[0]], [vb4.b[ts_]])
                            g.ts(kbeg4[:, ts_, u, :], ktm[:, hh, c, :], col(begi, u, step), ALU.mult, [ktm.b[hh], begi.b[0]],
                                 [kbeg4.b[ts_]])
                            g.ts(kdec4[:, sl, u, :], ktm[:, hh, c, :], col(erem, u, step), ALU.mult, [ktm.b[hh], erem.b[0]],
                                 [kdec4.b[sl]])
                        yield
                        pf = g.ps()
                        pfv = pf[0:64, :].rearrange("p (u t i) -> p u t i", u=4, t=2)
                        for u in range(4):
                            g.mm(pfv[:, u, 0, :], TTb[:, ts_, u, :], vb4[:, ts_, u, :], True, True, [TTb.b[ts_], vb4.b[ts_]], [pf.b[0]])
                            g.mm(pfv[:, u, 1, :], kbeg4[:, ts_, u, :], TTb[:, ts_, u, :], True, True, [TTb.b[ts_], kbeg4.b[ts_]],
                                 [pf.b[0]])
                        yield
                        g.cp(U4[:, sl], pfv[:, :, 0, :], [pf.b[0]], [U4.b[sl]], eng="act")
                        g.cp(wTb[:, sl], pfv[:, :, 1, :], [pf.b[0]], [wTb.b[sl]], eng="act")

                    def dep(step):
                        sl = step % RS_
                        cs = [order[d][step] for (hh, d) in units]
                        pg = g.ps()
                        pgv = pg[0:64, :].rearrange("p (u t i) -> p u t i", u=4, t=2)
                        for u, (hh, d) in enumerate(units):
                            c = cs[u]
                            g.mm(pgv[:, u, 0, :], wTb[:, sl, u, :], Sb[:, u, :], True, True, [wTb.b[sl], Sb.b[0]], [pg.b[0]])
                            g.mm(pgv[:, u, 1, :], qT[:, hh, c * 64:(c + 1) * 64], Sb[:, u, :], True, True, [qT.b[hh], Sb.b[0]],
                                 [pg.b[0]])
                        g.tt(vn4[:], U4[:, sl], pgv[:, :, 0, :], ALU.subtract, [U4.b[sl], pg.b[0]], [vn4.b[0]])
                        for u in range(4):
                            g.act(qs4[:, u, :], pgv[:, u, 1, :], AF.Copy, [pg.b[0], egi.b[0]], [qs4.b[0]], scale=col(egi, u, step))
                        ph = g.ps()
                        phv = ph[0:64, :].rearrange("p (u t i) -> p u t i", u=4, t=2)
                        for u in range(4):
                            g.mm(phv[:, u, 0, :], kdec4[:, sl, u, :], vn4[:, u, :], True, True, [kdec4.b[sl], vn4.b[0]], [ph.b[0]])
                            g.mm(phv[:, u, 1, :], attn4[:, sl, u, :], vn4[:, u, :], True, True, [attn4.b[sl], vn4.b[0]], [ph.b[0]])
                        for u in range(4):
                            g.stt(S[:, u, :], S[:, u, :], col(egl, u, step), phv[:, u, 0, :], ALU.mult, ALU.add,
                                  [S.b[0], egl.b[0], ph.b[0]], [S.b[0]])
                        g.cp(Sb[:], S[:], [S.b[0]], [Sb.b[0]], eng="act")
                        for u, (hh, d) in enumerate(units):
                            c = cs[u]
                            if (hh, c) not in visited:
                                visited.add((hh, c))
                                g.tt(Oacc[:, hh, c, :], qs4[:, u, :], phv[:, u, 1, :], ALU.add, [qs4.b[0], ph.b[0]], [Oacc.b[hh]])
                            else:
                                g.tt(qs4[:, u, :], qs4[:, u, :], phv[:, u, 1, :], ALU.add, [qs4.b[0], ph.b[0]], [qs4.b[0]])
                                g.tt(Oacc[:, hh, c, :], Oacc[:, hh, c, :], qs4[:, u, :], ALU.add, [qs4.b[0], Oacc.b[hh]],
                                     [Oacc.b[hh]], eng="pool")

                    NS = int(os.environ.get("GDN_STEPS", str(NCH)))
                    gens = []
                    done = set()
                    nxt_i, nxt_d = 0, 0
                    while nxt_d < NS:
                        while len(gens) < NSET and nxt_i < NS and nxt_i < nxt_d + RS_:
                            gens.append((nxt_i, indep(nxt_i)))
                            nxt_i += 1
                        still = []
                        for (st_, ge_) in gens:
                            try:
                                next(ge_)
                                still.append((st_, ge_))
                            except StopIteration:
                                done.add(st_)
                        gens = still
                        while nxt_d in done:
                            dep(nxt_d)
                            nxt_d += 1
                    if C.DBG is not None and hp == 0 and NS < NCH:
                        o_ = 0
                        for nm_, tl_, ap_ in (("S", S, S[:]), ("qs4", qs4, qs4[:])):
                            g.dma(C.DBG[:, o_:o_ + 256].rearrange("p (u i) -> p u i", u=4), ap_, R=tl_.b, W=[C.B_DBG], q="pool")
                            o_ += 256

                if C.DBG is not None and hp == 0 and "GDN_STEPS" not in os.environ:
                    o_ = 0
                    for tl_ in (G, beta, egi, erem, egl):
                        g.dma(C.DBG[:, o_:o_ + 8 * NCH], tl_[:].rearrange("p a c -> p (a c)"), R=[tl_.b[0]], W=[C.B_DBG], q="pool")
                        o_ += 8 * NCH
                    g.dma(C.DBG[:, 3000:3512], qT[:, 0, 0:512], R=[qT.b[0]], W=[C.B_DBG], q="pool")
                    g.dma(C.DBG[:, 3512:4024], kT[:, 0, 0:512], R=[kT.b[0]], W=[C.B_DBG], q="pool")
                    g.dma(C.DBG[:, 4024:4536], ktm[:, 0, 0:8, :].rearrange("p c d -> p (c d)"), R=[ktm.b[0]], W=[C.B_DBG], q="pool")
                    g.dma(C.DBG[:, 4536:5048], vtm[:, 0, 0:8, :].rearrange("p c d -> p (c d)"), R=[vtm.b[0]], W=[C.B_DBG], q="pool")
                    g.dma(C.DBG[:, 5048:5560], Oacc[:, 0, 0:8, :].rearrange("p c d -> p (c d)"), R=[Oacc.b[0]], W=[C.B_DBG], q="pool")
                    g.dma(C.DBG[:, 5560:6072], Oacc[:, 0, 60:68, :].rearrange("p c d -> p (c d)"), R=[Oacc.b[0]], W=[C.B_DBG], q="pool")
                    g.dma(C.DBG[:, 6072:6584], sgt[:, 0, 0:8, :].rearrange("p c d -> p (c d)"), R=[sgt.b[0]], W=[C.B_DBG], q="pool")
                with SS() as s3:
                    osq = g.sb("gosq", [64, 2 * NCH, 64], F32, stack=s3)
                    sgt = g.sb("gsgt2", [64, 2, NCH, 64], BF16, stack=s3)
                    g.dma(sgt[:], C.SGT, R=[C.B_SGT], W=[sgt.b[0]])
                    rs = g.sb("grs", [64, 2 * NCH], F32, stack=s3)
                    ob = vtm
                    OB = [Oacc.b[0], Oacc.b[1]]
                    Ov = Oacc[:].rearrange("p h c d -> p (h c) d")
                    g.tt(osq[:], Ov, Ov, ALU.mult, OB, [osq.b[0]], eng="pool")
                    g.K.op("dve", lambda h_: h_.tensor_reduce(out=rs[:], in_=osq[:], axis=AX.X, op=ALU.add), reads=[osq.b[0]],
                           writes=[rs.b[0]])
                    g.act(rs[:], rs[:], AF.Sqrt, [rs.b[0]], [rs.b[0]], bias=eps64, scale=1.0 / 64)
                    g.recip(rs[:], rs[:], [rs.b[0]], [rs.b[0]])
                    g.tt(osq[:], Ov, rs[:].unsqueeze(2).to_broadcast([64, 2 * NCH, 64]), ALU.mult, OB + [rs.b[0]], [osq.b[0]])
                    g.tt(osq[:], osq[:], ngb[:].unsqueeze(1).to_broadcast([64, 2 * NCH, 64]), ALU.mult, [osq.b[0], ngb.b[0]],
                         [osq.b[0]])
                    g.tt(ob[:].rearrange("p h c d -> p (h c) d"), osq[:], sgt[:].rearrange("p h c d -> p (h c) d"), ALU.mult,
                         [osq.b[0], sgt.b[0]], [ob.b[0], ob.b[1]])
                    for hh in range(2):
                        h = 2 * hp + hh
                        g.dma(C.O.rearrange("(c p) n -> p c n", p=64)[:, :, 256 + h * 64:256 + (h + 1) * 64], ob[:, hh],
                              R=[ob.b[0], ob.b[1]], W=C.B_O)


def phase_wout(C, l, need_ctx):
    g, I = C.g, C.I
    xsrc = I["xin"] if l == 0 else C.XS
    with SS() as s1:
        wout = load_w_bf16(C, "w_out", l, 0, D, "wout", s1)
        Abc, Sbc = norm_mod_tiles(C, l, 2, s1)
        G1 = g.sb("G1bc", [128, 2, D], F32, stack=s1)
        for r in range(2):
            g.dma(G1[:, r, :], C.MOD[l, r:r + 1, 2 * D:3 * D].partition_broadcast(128), R=[C.B_MOD[l]], W=[G1.b[0]])
        T = nm_scratch(C, s1)
        xt = g.sb("xt", [128, 2, D], F32, nslots=2, stack=s1)
        ot = g.sb("ot", [128, 2, D], BF16, nslots=2, stack=s1)
        oT = g.sb("oT", [128, 2, D], BF16, nslots=2, stack=s1)
        tmp = g.sb("wtmp", [128, D], F32, stack=s1)
        for t in range(NT):
            if t < 2 and not need_ctx:
                continue
            sl = t % 2
            r = 1 if t < 2 else 0
            g.dma(xt[:, sl], xsrc[t * 128:(t + 1) * 128, :], R=[C.B_XS[t]] if l > 0 else [], W=[xt.b[sl]])
            g.dma(ot[:, sl], C.O[t * 128:(t + 1) * 128, :], R=[C.B_O[t]], W=[ot.b[sl]])
            p = g.ps()
            pb = p[:].bitcast(BF16)
            for k in range(8):
                g.tr(pb[:, k * 128:(k + 1) * 128], ot[:, sl, k * 128:(k + 1) * 128], C.ident[:],
                     [ot.b[sl], C.ident.b[0]], [p.b[0]])
            g.cp(oT[:, sl], pb, [p.b[0]], [oT.b[sl]], eng="act")
            for nh in range(2):
                p = g.ps()
                for k in range(8):
                    g.mm(p[:, :], oT[:, sl, k * 128:(k + 1) * 128], wout[:, k, nh * 512:(nh + 1) * 512], k == 0, k == 7,
                         [oT.b[sl], wout.b[0]], [p.b[0]])
                g.tt(tmp[:, nh * 512:(nh + 1) * 512], p[:, :], G1[:, r, nh * 512:(nh + 1) * 512], ALU.mult,
                     [p.b[0], G1.b[0]], [tmp.b[0]], eng="dve")
            g.tt(xt[:, sl], xt[:, sl], tmp[:], ALU.add, [xt.b[sl], tmp.b[0]], [xt.b[sl]], eng="pool")
            g.dma(C.XS[t * 128:(t + 1) * 128, :], xt[:, sl], R=[xt.b[sl]], W=[C.B_XS[t]])
            norm_mod_transpose(C, xt[:, sl], xt.b[sl], r, Abc, Sbc, T, sl, C.H2T[t], C.B_H2T[t])


def phase_ffn(C, l, need_ctx, last):
    g, I = C.g, C.I
    moe = (l % 2 == 1)
    j = l // 2
    E = NE if moe else 1
    if moe:
        W1 = lambda e: I["moe_w1"][j, e]
        W3 = lambda e: I["moe_w3"][j, e]
        W2 = lambda e: I["moe_w2"][j, e]
    else:
        W1 = lambda e: I["ffn_w1"][j]
        W3 = lambda e: I["ffn_w3"][j]
        W2 = lambda e: I["ffn_w2"][j]
    tiles = [t for t in range(NT) if t >= 2 or need_ctx]
    groups = []
    if need_ctx:
        groups.append([0, 1])
    for i in range(4):
        groups.append([2 + 8 * i + q for q in range(8)])
    with SS() as s1:
        G2 = g.sb("G2bc", [128, 2, D], F32, stack=s1)
        for r in range(2):
            g.dma(G2[:, r, :], C.MOD[l, r:r + 1, 5 * D:6 * D].partition_broadcast(128), R=[C.B_MOD[l]], W=[G2.b[0]])
        if last:
            fg = g.sb("fgbc", [128, D], F32, stack=s1)
            g.dma(fg[:], I["final_g"].rearrange("(o d) -> o d", o=1).partition_broadcast(128), W=[fg.b[0]])
        if moe:
            wr = g.sb("wrouter", [128, 8, NE], BF16, stack=s1)
            g.dma(wr[:], I["moe_router"][j].rearrange("(k p) e -> p k e", p=128), W=[wr.b[0]], q="pool")
        h2 = g.sb("h2", [128, 1, 8, 1024], BF16, nslots=1, stack=s1)
        aT = g.sb("aT", [128, 22, 1024], BF16, nslots=22, stack=s1)
        Y = g.sb("Yacc", [128, 8, D], F32, nslots=8, stack=s1)
        w13 = g.sb("w13", [128, 3, 2, 8, 256], BF16, nslots=3, stack=s1)
        w2 = g.sb("w2", [128, 2, 11, D], BF16, nslots=2, stack=s1)
        sil = g.sb("sil", [128, 2, 512], F32, nslots=2, stack=s1)
        gates = g.sb("gates", [128, 8, NE], F32, nslots=8, stack=s1)
        rt = g.sb("rt", [128, 8, NE], F32, stack=s1)
        rc = g.sb("rc", [128, 8], F32, stack=s1)
        xt = g.sb("xt", [128, 2, D], F32, nslots=2, stack=s1)
        ft = g.sb("ftmp", [128, D], F32, stack=s1)
        fs = g.sb("fssq", [128, 2], F32, nslots=2, stack=s1)
        w13_it = 0
        w2_it = 0
        for gi_, tl in enumerate(groups):
            hs = 0
            n = 128 * len(tl)
            for q, t in enumerate(tl):
                g.dma(h2[:, hs, :, q * 128:(q + 1) * 128], C.H2T[t].rearrange("p (k c) -> p k c", k=8), R=[C.B_H2T[t]],
                      W=[h2.b[hs]])
            for q, t in enumerate(tl):
                if not moe:
                    g.memset(gates[:, q, :], 1.0, [gates.b[q]])
                    continue
                p = g.ps()
                for k in range(8):
                    g.mm(p[:, 0:NE], h2[:, hs, k, q * 128:(q + 1) * 128], wr[:, k, :], k == 0, k == 7, [h2.b[hs], wr.b[0]],
                         [p.b[0]])
                RT, RC = [rt.b[0]], [rc.b[0]]
                lg, eq1, msk, eq2 = rt[:, 0, :], rt[:, 1, :], rt[:, 2, :], rt[:, 3, :]
                g.cp(lg, p[:, 0:NE], [p.b[0]], RT, eng="dve")
                red = lambda o_, i_: g.K.op("dve", lambda h_: h_.tensor_reduce(out=o_, in_=i_, axis=AX.X, op=ALU.max),
                                            reads=RT + RC, writes=RC)
                red(rc[:, 0:1], lg)
                g.ts(eq1, lg, rc[:, 0:1], ALU.is_equal, RT + RC, RT)
                g.stt(msk, eq1, -1e30, lg, ALU.mult, ALU.add, RT, RT)
                red(rc[:, 1:2], msk)
                g.ts(eq2, msk, rc[:, 1:2], ALU.is_equal, RT + RC, RT)
                g.tt(rc[:, 2:3], rc[:, 1:2], rc[:, 0:1], ALU.subtract, RC, RC)
                g.act(rc[:, 3:4], rc[:, 2:3], AF.Exp, RC, RC)
                g.ts(rc[:, 4:5], rc[:, 3:4], 1.0, ALU.add, RC, RC)
                g.recip(rc[:, 4:5], rc[:, 4:5], RC, RC)
                g.tt(rc[:, 5:6], rc[:, 3:4], rc[:, 4:5], ALU.mult, RC, RC)
                g.ts(eq1, eq1, rc[:, 4:5], ALU.mult, RT + RC, RT)
                g.stt(gates[:, q, :], eq2, rc[:, 5:6], eq1, ALU.mult, ALU.add, RT + RC, [gates.b[q]])
            for e in range(E):
                w1v = W1(e).rearrange("(k p) n -> p k n", p=128)
                w3v = W3(e).rearrange("(k p) n -> p k n", p=128)
                w2v = W2(e).rearrange("(c p) n -> p c n", p=128)
                w2s = [0, 1]
                sit = 0
                for cb in range(11):
                    wsl = w13_it % 3
                    w13_it += 1
                    g.dma(w13[:, wsl, 0], w1v[:, :, cb * 256:(cb + 1) * 256], W=[w13.b[wsl]], q="pool")
                    g.dma(w13[:, wsl, 1], w3v[:, :, cb * 256:(cb + 1) * 256], W=[w13.b[wsl]], q="pool")
                    if cb == 2:
                        for hf_ in range(2):
                            g.dma(w2[:, hf_], w2v[:, hf_ * 11:(hf_ + 1) * 11, :], W=[w2.b[hf_]], q="pool")
                    for c2 in range(2):
                        c = 2 * cb + c2
                        for n0 in range(0, n, 512):
                            nn = min(512, n - n0)
                            ss = sit % 2
                            sit += 1
                            p1, p3 = g.ps(), g.ps()
                            for (pp, wi) in ((p1, 0), (p3, 1)):
                                for k in range(8):
                                    g.mm(pp[:, 0:nn], w13[:, wsl, wi, k, c2 * 128:(c2 + 1) * 128], h2[:, hs, k, n0:n0 + nn], k == 0,
                                         k == 7, [w13.b[wsl], h2.b[hs]], [pp.b[0]])
                            g.act(sil[:, ss, 0:nn], p1[:, 0:nn], AF.Silu, [p1.b[0]], [sil.b[ss]])
                            g.tt(aT[:, c, n0:n0 + nn], p3[:, 0:nn], sil[:, ss, 0:nn], ALU.mult, [p3.b[0], sil.b[ss]], [aT.b[c]],
                                 eng="dve")
                for q, t in enumerate(tl):
                    for nh in range(2):
                        p = g.ps()
                        for c in range(22):
                            g.mm(p[:, :], aT[:, c, q * 128:(q + 1) * 128], w2[:, w2s[c // 11], c % 11, nh * 512:(nh + 1) * 512],
                                 c == 0, c == 21, [aT.b[c], w2.b[w2s[c // 11]]], [p.b[0]])
                        yv = Y[:, q, nh * 512:(nh + 1) * 512]
                        if e == 0:
                            g.ts(yv, p[:, :], gates[:, q, e:e + 1], ALU.mult, [p.b[0], gates.b[q]], [Y.b[q]])
                        else:
                            g.stt(yv, p[:, :], gates[:, q, e:e + 1], yv, ALU.mult, ALU.add, [p.b[0], gates.b[q], Y.b[q]], [Y.b[q]])
            for q, t in enumerate(tl):
                sl = t % 2
                r = 1 if t < 2 else 0
                g.dma(xt[:, sl], C.XS[t * 128:(t + 1) * 128, :], R=[C.B_XS[t]], W=[xt.b[sl]])
                g.tt(Y[:, q, :], Y[:, q, :], G2[:, r, :], ALU.mult, [Y.b[q], G2.b[0]], [Y.b[q]], eng="pool")
                g.tt(xt[:, sl], xt[:, sl], Y[:, q, :], ALU.add, [xt.b[sl], Y.b[q]], [xt.b[sl]], eng="pool")
                if not last:
                    g.dma(C.XS[t * 128:(t + 1) * 128, :], xt[:, sl], R=[xt.b[sl]], W=[C.B_XS[t]])
                else:
                    g.act(ft[:], xt[:, sl], AF.Square, [xt.b[sl]], [ft.b[0], fs.b[sl]], accum_out=fs[:, sl:sl + 1])
                    g.act(fs[:, sl:sl + 1], fs[:, sl:sl + 1], AF.Sqrt, [fs.b[sl]], [fs.b[sl]], bias=C.epsc[:, 0:1], scale=1.0 / D)
                    g.recip(fs[:, sl:sl + 1], fs[:, sl:sl + 1], [fs.b[sl]], [fs.b[sl]])
                    g.stt(xt[:, sl], xt[:, sl], fs[:, sl:sl + 1], fg[:], ALU.mult, ALU.mult, [xt.b[sl], fs.b[sl], fg.b[0]],
                          [xt.b[sl]])
                    C.out_tokens.append(g.dma(C.out[(t - 2) * 128:(t - 1) * 128, :], xt[:, sl], R=[xt.b[sl]], W=[C.B_OUT[t]]))


def phase_gdnzero(C, l):
    g = C.g
    with SS() as s1:
        z = g.sb("gz", [128, 256], BF16, stack=s1)
        g.memset(z[:], 0.0, [z.b[0]])
        for t in range(NT):
            g.dma(C.O[t * 128:(t + 1) * 128, 256:512], z[:], R=[z.b[0]], W=[C.B_O[t]])


def build_program(dbg=None, nlayers=DEPTH, phases=("norm1", "ret", "gdn", "na", "wout", "ffn")):
    nc = bass.Bass("TRN2", target_bir_lowering=False)
    C = Ctx()
    I = C.I = {}

    def inp(name, shape, dt=F32):
        I[name] = nc.dram_tensor(name, list(shape), dt, kind="ExternalInput").ap()

    def scratch(name, shape, dt):
        return nc.dram_tensor(name, list(shape), dt, kind="ExternalOutput" if (dbg and name in dbg.split("+")) else "Internal").ap()

    inp("xin", [NTOK, D])
    inp("cvec", [2, D])
    inp("ada_w", [DEPTH, D, 6 * D])
    inp("ada_b", [DEPTH, 6 * D])
    inp("norm1_g", [DEPTH, D])
    inp("norm2_g", [DEPTH, D])
    inp("w_in", [DEPTH, D, D_IN])
    inp("ret_decay", [DEPTH, 2, 4])
    inp("ret_const", [128, 6, 128])
    inp("ret_pcol", [128, 2, 64])
    inp("rope", [SEQ, 2, 256])
    inp("w_out", [DEPTH, D, D])
    inp("ffn_w1", [1, D, D_FF])
    inp("ffn_w3", [1, D, D_FF])
    inp("ffn_w2", [1, D_FF, D])
    inp("moe_router", [1, D, NE])
    inp("moe_w1", [1, NE, D, D_FF])
    inp("moe_w3", [1, NE, D, D_FF])
    inp("moe_w2", [1, NE, D_FF, D])
    inp("final_g", [D])
    inp("gdn_const", [64, 13, 64])
    inp("gdn_bd", [128, 128])
    inp("gdn_a_log", [DEPTH, 2, 4])
    inp("gdn_dt_bias", [DEPTH, 2, 4])
    inp("gdn_norm_g", [DEPTH, 64])
    inp("gdn_convw", [DEPTH, 4, 64, 3, 5])
    inp("na_mask", [128, 5, 640])
    inp("na_bias", [DEPTH, 8, 128, 5, 640])
    C.out = nc.dram_tensor("out", [SEQ, D], F32, kind="ExternalOutput").ap()
    C.out_tokens = []
    C.B_OUT = [Buf() for _ in range(NT)]
    C.MOD = scratch("MOD", [DEPTH, 2, 6 * D], F32)
    C.HT = scratch("HT", [NT, 128, D], BF16)
    C.XS = scratch("XS", [NTOK, D], F32)
    C.O = scratch("O", [NTOK, D], BF16)
    C.H2T = scratch("H2T", [NT, 128, D], BF16)
    C.B_H2T = [Buf() for _ in range(NT)]
    C.SGT = scratch("SGT", [64, 2, NTOK // 64, 64], BF16)
    C.B_SGT = Buf()
    C.DBG = scratch("DBG", [64, 8192], F32) if (dbg and "DBG" in dbg) else None
    C.B_DBG = Buf()
    C.B_MOD = [Buf() for _ in range(DEPTH)]
    C.B_HT = [Buf() for _ in range(NT)]
    C.B_XS = [Buf() for _ in range(NT)]
    C.B_O = [Buf() for _ in range(NT)]

    with ExitStack() as st:
        g = C.g = Gen(nc, st)
        K = g.K
        _CUR["K"] = K
        ident = C.ident = g.sb("ident", [128, 128], BF16)
        g.memset(ident[:], 1.0, [ident.b[0]])
        K.op("pool", lambda h: h.affine_select(out=ident[:], in_=ident[:], pattern=[[-1, 128]],
                                                compare_op=ALU.is_equal, fill=0.0, base=0, channel_multiplier=1),
             reads=[ident.b[0]], writes=[ident.b[0]])
        C.epsc = g.sb("epsc", [128, 1], F32)
        g.memset(C.epsc[:], EPS, [C.epsc.b[0]])
        C.onec = g.sb("onec", [128, 1], F32)
        g.memset(C.onec[:], 1.0, [C.onec.b[0]])

        phase_adaln(C, nlayers)
        for l in range(nlayers):
            need_ctx = l < DEPTH - 1
            if "norm1" in phases:
                phase_norm1(C, l)
            if "ret" in phases:
                phase_ret(C, l, need_ctx)
            if "gdn" in phases:
                phase_gdn(C, l, need_ctx)
            if "na" in phases:
                phase_na(C, l, need_ctx)
            if "gdnzero" in phases:
                phase_gdnzero(C, l)
            if "wout" in phases:
                phase_wout(C, l, need_ctx)
            if "ffn" in phases:
                phase_ffn(C, l, need_ctx, l == nlayers - 1 and nlayers == DEPTH)

        fin = [b.last_w for b in C.B_HT + C.B_O + C.B_XS + C.B_H2T + [C.B_DBG] if b.last_w is not None] + C.out_tokens
        K.finish(fin)
        K.emit()
    return nc


_NC_CACHE = {}
NA_M_REP = [0, 1, 2, 30, 31]


def na_cls(m):
    return {0: 0, 1: 1, 30: 3, 31: 4}.get(m, 2)


def na_kp0(m):
    return min(max(m - 2, 0), 27)


def _na_index_tables():
    kk = np.arange(128)[:, None, None]
    j = np.arange(5)[None, :, None]
    qq = np.arange(128)[None, None, :]
    ki, kc = kk // 64, kk % 64
    qi, qc = qq // 64, qq % 64
    ridx = np.zeros((5, 128, 5, 128), np.int64)
    cidx = np.zeros((5, 128, 5, 128), np.int64)
    valid = np.zeros((5, 128, 5, 128), bool)
    for c, m in enumerate(NA_M_REP):
        kr = 2 * (na_kp0(m) + j) + ki
        r = 2 * m + qi
        r0 = np.clip(r - 4, 0, 56)
        ws = np.clip(qc - 8, 0, 48)
        v = (kr >= r0) & (kr < r0 + 8) & (kc >= ws) & (kc < ws + 16)
        valid[c] = np.broadcast_to(v, (128, 5, 128))
        ridx[c] = np.broadcast_to(np.clip(kr - r + 7, 0, 14), (128, 5, 128))
        cidx[c] = np.broadcast_to(np.clip(kc - qc + 15, 0, 30), (128, 5, 128))
    return ridx, cidx, valid


def _const_inputs():
    if "consts" in _NC_CACHE:
        return _NC_CACHE["consts"]
    ridx, cidx, valid = _na_index_tables()
    ii = np.arange(128)[None, :].astype(np.float64)
    jj = np.arange(128)[:, None].astype(np.float64)
    ret_const = np.stack([np.maximum(ii - jj, 0) + 0 * jj, np.maximum(jj - ii, 0) + 0 * ii, (ii >= jj) * 1.0, (jj >= ii) * 1.0,
                          (ii + 1) + 0 * jj, (128 - ii) + 0 * jj], axis=1).astype(np.float32)
    ret_pcol = np.stack([np.broadcast_to(127 - jj, (128, 64)), np.broadcast_to(jj, (128, 64))], axis=1).astype(np.float32)
    tpos = np.arange(SEQ)
    inv_freq = 10000.0 ** (-np.arange(16, dtype=np.float32) / 16)
    ang = np.concatenate([(tpos // 64).astype(np.float32)[:, None] * inv_freq, (tpos % 64).astype(np.float32)[:, None] * inv_freq],
                         axis=-1).astype(np.float32)
    rope = np.stack([np.tile(np.cos(ang), (1, 8)), np.tile(np.sin(ang), (1, 8))], axis=1).astype(np.float32)
    mm_, i_ = np.arange(64)[:, None], np.arange(64)[None, :]
    Yf, Yb, SUf, SUb = (mm_ <= i_) * 1.0, (mm_ >= i_) * 1.0, (mm_ > i_) * 1.0, (mm_ < i_) * 1.0
    gdn_const = np.stack([Yf, Yb, Yf, Yb, SUf, SUb, SUf, SUb] + [np.eye(64)] * 4 + [np.ones((64, 64))], axis=1).astype(np.float32)
    pp_ = np.arange(128)
    gdn_bd = ((pp_[:, None] // 64) == (pp_[None, :] // 64)).astype(np.float32)
    c = {"gdn_const": np.ascontiguousarray(gdn_const), "gdn_bd": gdn_bd, "ret_const": np.ascontiguousarray(ret_const), "ret_pcol": np.ascontiguousarray(ret_pcol), "rope": rope,
         "na_mask": np.where(valid, 0.0, -1e30).astype(np.float32).transpose(1, 0, 2, 3).reshape(128, 5, 640).copy(),
         "_na_ridx": ridx, "_na_cidx": cidx}
    _NC_CACHE["consts"] = c
    return c


def _prep_core_inputs(b, inputs):
    xin = np.ascontiguousarray(np.concatenate([inputs["ctx"][b], inputs["x"][b]], axis=0))
    cvec = np.ascontiguousarray(np.stack([inputs["c"][b], inputs["c_ctx"]], axis=0))
    m = {"xin": xin, "cvec": cvec}
    for k in ("ada_w", "ada_b", "norm1_g", "norm2_g", "w_in", "w_out", "ffn_w1", "ffn_w3", "ffn_w2", "moe_router", "moe_w1",
              "moe_w3", "moe_w2", "final_g"):
        m[k] = np.ascontiguousarray(inputs[k])
    c = _const_inputs()
    for k in ("gdn_a_log", "gdn_dt_bias", "gdn_norm_g"):
        m[k] = np.ascontiguousarray(inputs[k])
    m["gdn_convw"] = np.ascontiguousarray(inputs["conv_w"].reshape(DEPTH, 5, 3, 4, 64).transpose(0, 3, 4, 2, 1))
    for k in ("na_mask", "ret_const", "ret_pcol", "rope", "gdn_const", "gdn_bd"):
        m[k] = c[k]
    m["ret_decay"] = np.ascontiguousarray(inputs["ret_decay"])
    rp = inputs["na_rpb"]
    gat = rp[:, :, c["_na_ridx"], c["_na_cidx"]]
    m["na_bias"] = np.ascontiguousarray(gat.transpose(0, 1, 3, 2, 4, 5).reshape(DEPTH, 8, 128, 5, 640))
    return m


def kernel(**inputs):
    inputs = {k: np.asarray(v) for k, v in inputs.items()}
    if "nc" not in _NC_CACHE:
        _NC_CACHE["nc"] = build_program()
    nc = _NC_CACHE["nc"]
    in_maps = [_prep_core_inputs(b, inputs) for b in range(8)]
    res = run_bass_kernel_spmd(nc, in_maps, core_ids=list(range(8)))
    return np.stack([r["out"] for r in res.results], axis=0)
```

```python
import os
import numpy as np
import ml_dtypes
import concourse.bass as bass
import concourse.mybir as mybir
from contextlib import ExitStack
from concourse.bass_utils import run_bass_kernel_spmd

F32 = mybir.dt.float32
BF16 = mybir.dt.bfloat16
F32R = mybir.dt.float32r
ALU = mybir.AluOpType
AF = mybir.ActivationFunctionType
AX = mybir.AxisListType

EPOCH = 16000
N_DMA_SEMS = 40

D = 1024
SEQ = 4096
CTX = 256
NTOK = SEQ + CTX
NT = NTOK // 128
DEPTH = 2
D_IN = 3600
D_FF = 2816
NE = 8
EPS = 1e-6


class Buf:
    __slots__ = ("name", "last_w", "reads", "excl")

    def __init__(self, name="", excl=False):
        self.name = name
        self.last_w = None
        self.reads = []
        self.excl = excl


class Eng:
    def __init__(self, name):
        self.name = name
        self.ops = []
        self.n = 0
        self.sems = []
        self.waited = {}


class Ker:
    def __init__(self, nc, stack):
        self.nc = nc
        self.stack = stack
        self.eng = {n: Eng(n) for n in ("pe", "act", "dve", "pool", "sp")}
        self.dma_sems = []
        self.dma_sem_val = []
        self.dma_rr = 0
        for i in range(N_DMA_SEMS):
            s = stack.enter_context(nc.semaphore(f"dq{i}"))
            self.dma_sems.append(s)
            self.dma_sem_val.append(0)

    def _need(self, e, waits, tok):
        if tok is None:
            return
        sem, val, key = tok[0], tok[1], tok[2]
        if e.waited.get(key, 0) >= val:
            return
        e.waited[key] = val
        waits.append((sem, val))

    def _deps(self, e, reads, writes, pe_acc=False):
        waits = []
        for b in reads:
            self._need(e, waits, b.last_w)
            if b.excl:
                for t in b.reads:
                    if t[3] != e.name:
                        self._need(e, waits, t)
        for b in writes:
            if not (pe_acc and b.last_w is not None and b.last_w[3] == "pe"):
                self._need(e, waits, b.last_w)
            for t in b.reads:
                self._need(e, waits, t)
        return waits

    def _commit(self, tok, reads, writes):
        for b in reads:
            b.reads.append(tok)
            if len(b.reads) > 12:
                d = {}
                for t in b.reads:
                    if t[2] not in d or d[t[2]][1] < t[1]:
                        d[t[2]] = t
                b.reads = list(d.values())
        for b in writes:
            b.last_w = tok
            b.reads = []

    def op(self, engname, fn, reads=(), writes=(), pe_acc=False):
        e = self.eng[engname]
        waits = self._deps(e, reads, writes, pe_acc)
        ep = e.n // EPOCH
        while len(e.sems) <= ep:
            e.sems.append(self.stack.enter_context(self.nc.semaphore(f"s_{engname}{len(e.sems)}")))
        sem = e.sems[ep]
        val = e.n % EPOCH + 1
        tok = (sem, val, (engname, ep), engname)
        e.n += 1
        e.ops.append((waits, fn, (sem, 1)))
        self._commit(tok, reads, writes)
        return tok

    def dma(self, qname, out_ap, in_ap, reads=(), writes=(), **kw):
        e = self.eng[qname]
        waits = self._deps(e, reads, writes)
        i = self.dma_rr
        self.dma_rr = (self.dma_rr + 1) % N_DMA_SEMS
        sem = self.dma_sems[i]
        if self.dma_sem_val[i] > 0:
            self._need(e, waits, (sem, self.dma_sem_val[i], ("dq", i), "dma"))
        self.dma_sem_val[i] += 16
        val = self.dma_sem_val[i]
        tok = (sem, val, ("dq", i), "dma")

        def fn(h, out_ap=out_ap, in_ap=in_ap, kw=kw):
            return h.dma_start(out=out_ap, in_=in_ap, **kw)
        e.ops.append((waits, fn, (sem, 16)))
        self._commit(tok, reads, writes)
        return tok

    def barrier(self):
        toks = []
        for n, e in self.eng.items():
            if e.n > 0:
                ep = (e.n - 1) // EPOCH
                toks.append((e.sems[ep], (e.n - 1) % EPOCH + 1, (n, ep), n))
        for i in range(N_DMA_SEMS):
            if self.dma_sem_val[i] > 0:
                toks.append((self.dma_sems[i], self.dma_sem_val[i], ("dq", i), "dma"))
        for n, e in self.eng.items():
            waits = []
            for t in toks:
                self._need(e, waits, t)
            e.ops.append((waits, None, None))

    def finish(self, final_tokens):
        e = self.eng["sp"]
        waits = []
        for t in final_tokens:
            self._need(e, waits, t)
        e.ops.append((waits, None, None))

    def emit(self):
        nc = self.nc
        with nc.Block() as block:
            def run(e):
                def body(h):
                    for waits, fn, inc in e.ops:
                        for (sem, val) in waits:
                            h.wait_ge(sem, val)
                        if fn is not None:
                            ins = fn(h)
                            if inc is not None:
                                ins.then_inc(inc[0], inc[1])
                return body
            block.tensor(run(self.eng["pe"]))
            block.scalar(run(self.eng["act"]))
            block.vector(run(self.eng["dve"]))
            block.gpsimd(run(self.eng["pool"]))
            block.sync(run(self.eng["sp"]))


_CUR = {"K": None}


class SS(ExitStack):
    def __exit__(self, *a):
        if _CUR["K"] is not None and a[0] is None:
            _CUR["K"].barrier()
        return super().__exit__(*a)


class Tile:
    def __init__(self, t, nslots=1):
        self.t = t
        self.b = [Buf() for _ in range(nslots)]

    def __getitem__(self, k):
        return self.t[k]


class Gen:
    def __init__(self, nc, stack):
        self.nc = nc
        self.st = stack
        self.K = Ker(nc, stack)
        self.psum = []
        self.ps_rr = 0
        for i in range(8):
            t = stack.enter_context(nc.psum_tensor(f"ps{i}", [128, 512], F32))
            tl = Tile(t)
            tl.b[0].excl = True
            self.psum.append(tl)

    def ps(self):
        p = self.psum[self.ps_rr]
        self.ps_rr = (self.ps_rr + 1) % 8
        return p

    def sb(self, name, shape, dt, nslots=1, stack=None):
        self.uid = getattr(self, "uid", 0) + 1
        t = (stack or self.st).enter_context(self.nc.sbuf_tensor(f"{name}_{self.uid}", shape, dt))
        return Tile(t, nslots)

    def mm(self, out, lhsT, rhs, start, stop, R, W):
        return self.K.op("pe", lambda h: h.matmul(out, lhsT, rhs, start=start, stop=stop), reads=R, writes=W,
                         pe_acc=not start)

    def tr(self, out, in_, ident, R, W):
        return self.K.op("pe", lambda h: h.transpose(out, in_, ident), reads=R, writes=W, pe_acc=True)

    def act(self, out, in_, func, R, W, bias=None, scale=1.0, accum_out=None, eng="act"):
        kw = {}
        if bias is not None:
            kw["bias"] = bias
        if accum_out is not None:
            kw["accum_out"] = accum_out
        return self.K.op(eng, lambda h: h.activation(out=out, in_=in_, func=func, scale=scale, **kw), reads=R, writes=W)

    def tt(self, out, in0, in1, op, R, W, eng="dve"):
        return self.K.op(eng, lambda h: h.tensor_tensor(out=out, in0=in0, in1=in1, op=op), reads=R, writes=W)

    def ts(self, out, in0, s1, op0, R, W, s2=None, op1=None, eng="dve", accum_out=None):
        kw = {}
        if op1 is not None:
            kw["op1"] = op1
        if accum_out is not None:
            kw["accum_out"] = accum_out
        return self.K.op(eng, lambda h: h.tensor_scalar(out=out, in0=in0, scalar1=s1, scalar2=s2, op0=op0, **kw),
                         reads=R, writes=W)

    def stt(self, out, in0, scalar, in1, op0, op1, R, W, eng="dve"):
        return self.K.op(eng, lambda h: h.scalar_tensor_tensor(out=out, in0=in0, scalar=scalar, in1=in1, op0=op0, op1=op1),
                         reads=R, writes=W)

    def cp(self, out, in_, R, W, eng="dve"):
        if eng == "act":
            return self.K.op("act", lambda h: h.copy(out=out, in_=in_), reads=R, writes=W)
        return self.K.op(eng, lambda h: h.tensor_copy(out=out, in_=in_), reads=R, writes=W)

    def memset(self, ap, val, W, eng="pool"):
        return self.K.op(eng, lambda h: h.memset(ap, val), writes=W)

    def recip(self, out, in_, R, W):
        return self.K.op("dve", lambda h: h.reciprocal(out=out, in_=in_), reads=R, writes=W)

    def dma(self, out, in_, R=(), W=(), q="sp", **kw):
        return self.K.dma(q, out, in_, reads=R, writes=W, **kw)


class Ctx:
    pass


def phase_adaln(C, nlayers):
    g, I = C.g, C.I
    with SS() as s1:
        craw = g.sb("craw", [128, 2, 8], F32, stack=s1)
        for r in range(2):
            g.dma(craw[:, r, :], I["cvec"][r].rearrange("(p k) -> p k", k=8), W=[craw.b[0]])
        scT = g.sb("scT", [128, 8, 2], F32, stack=s1)
        g.act(scT[:].rearrange("p k r -> p r k"), craw[:], AF.Silu, [craw.b[0]], [scT.b[0]])
        adab = g.sb("adab", [2, 6 * D], F32, stack=s1)
        modsb = g.sb("modsb", [2, 6 * D], F32, stack=s1)
        wch = g.sb("wch", [128, 2, 8, 512], F32, nslots=2, stack=s1)
        it = 0
        for l in range(nlayers):
            g.dma(adab[:], I["ada_b"][l:l + 1, :].partition_broadcast(2), W=[adab.b[0]])
            for n in range(12):
                sl = it % 2
                it += 1
                g.dma(wch[:, sl], I["ada_w"][l].rearrange("(p k) n -> p k n", k=8)[:, :, n * 512:(n + 1) * 512],
                      W=[wch.b[sl]])
                p = g.ps()
                for k in range(8):
                    g.mm(p[0:2, :], scT[:, k, :], wch[:, sl, k, :], k == 0, k == 7, [scT.b[0], wch.b[sl]], [p.b[0]])
                g.tt(modsb[:, n * 512:(n + 1) * 512], p[0:2, :], adab[:, n * 512:(n + 1) * 512], ALU.add,
                     [p.b[0], adab.b[0]], [modsb.b[0]])
            g.dma(C.MOD[l], modsb[:], R=[modsb.b[0]], W=[C.B_MOD[l]])


def norm_mod_tiles(C, l, which, s1):
    g, I = C.g, C.I
    so, co = (0, D) if which == 1 else (3 * D, 4 * D)
    gname = "norm1_g" if which == 1 else "norm2_g"
    Abc = g.sb("Abc", [128, 2, D], F32, stack=s1)
    Sbc = g.sb("Sbc", [128, 2, D], F32, stack=s1)
    gbc = g.sb("gbc", [128, D], F32, stack=s1)
    g.dma(gbc[:], I[gname][l:l + 1, :].partition_broadcast(128), W=[gbc.b[0]])
    for r in range(2):
        g.dma(Abc[:, r, :], C.MOD[l, r:r + 1, co:co + D].partition_broadcast(128), R=[C.B_MOD[l]], W=[Abc.b[0]])
        g.dma(Sbc[:, r, :], C.MOD[l, r:r + 1, so:so + D].partition_broadcast(128), R=[C.B_MOD[l]], W=[Sbc.b[0]])
        g.stt(Abc[:, r, :], Abc[:, r, :], 1.0, gbc[:], ALU.add, ALU.mult, [Abc.b[0], gbc.b[0]], [Abc.b[0]])
    return Abc, Sbc


def norm_mod_transpose(C, xt_ap, xt_b, r, Abc, Sbc, T, sl, dstHT, dstB):
    g = C.g
    sq, ssq, hf, hb, hT = T
    g.act(sq[:], xt_ap, AF.Square, [xt_b], [sq.b[0], ssq.b[sl]], accum_out=ssq[:, sl:sl + 1])
    g.act(ssq[:, sl:sl + 1], ssq[:, sl:sl + 1], AF.Sqrt, [ssq.b[sl]], [ssq.b[sl]], bias=C.epsc[:, 0:1], scale=1.0 / D)
    g.recip(ssq[:, sl:sl + 1], ssq[:, sl:sl + 1], [ssq.b[sl]], [ssq.b[sl]])
    g.stt(hf[:, sl], xt_ap, ssq[:, sl:sl + 1], Abc[:, r, :], ALU.mult, ALU.mult,
          [xt_b, ssq.b[sl], Abc.b[0]], [hf.b[sl]])
    g.tt(hb[:, sl], hf[:, sl], Sbc[:, r, :], ALU.add, [hf.b[sl], Sbc.b[0]], [hb.b[sl]], eng="pool")
    p = g.ps()
    pb = p[:].bitcast(BF16)
    for k in range(8):
        g.tr(pb[:, k * 128:(k + 1) * 128], hb[:, sl, k * 128:(k + 1) * 128], C.ident[:],
             [hb.b[sl], C.ident.b[0]], [p.b[0]])
    g.cp(hT[:, sl], pb, [p.b[0]], [hT.b[sl]], eng="act")
    g.dma(dstHT, hT[:, sl], R=[hT.b[sl]], W=[dstB])


def nm_scratch(C, s1):
    g = C.g
    sq = g.sb("sq", [128, D], F32, stack=s1)
    ssq = g.sb("ssq", [128, 2], F32, nslots=2, stack=s1)
    hf = g.sb("hf", [128, 2, D], F32, nslots=2, stack=s1)
    hb = g.sb("hb", [128, 2, D], BF16, nslots=2, stack=s1)
    hT = g.sb("hT", [128, 2, D], BF16, nslots=2, stack=s1)
    return (sq, ssq, hf, hb, hT)


def phase_norm1(C, l):
    g, I = C.g, C.I
    xsrc = I["xin"] if l == 0 else C.XS
    with SS() as s1:
        Abc, Sbc = norm_mod_tiles(C, l, 1, s1)
        xt = g.sb("xt", [128, 2, D], F32, nslots=2, stack=s1)
        T = nm_scratch(C, s1)
        for t in range(NT):
            sl = t % 2
            r = 1 if t < 2 else 0
            g.dma(xt[:, sl], xsrc[t * 128:(t + 1) * 128, :], R=[C.B_XS[t]] if l > 0 else [], W=[xt.b[sl]])
            norm_mod_transpose(C, xt[:, sl], xt.b[sl], r, Abc, Sbc, T, sl, C.HT[t], C.B_HT[t])


def load_ht_all(C, s1):
    g = C.g
    hts = g.sb("hts", [128, 8, NTOK], BF16, nslots=NT, stack=s1)
    for t in range(NT):
        g.dma(hts[:, :, t * 128:(t + 1) * 128], C.HT[t].rearrange("p (k c) -> p k c", k=8), R=[C.B_HT[t]],
              W=[hts.b[t]])
    return hts


def load_w_bf16(C, name, l, c0, c1, tname, s1, krows=8):
    g = C.g
    w = g.sb(tname, [128, krows, c1 - c0], BF16, stack=s1)
    src = C.I[name][l].rearrange("(k p) n -> p k n", p=128)
    for k0 in range(0, krows, 4):
        k1 = min(krows, k0 + 4)
        g.dma(w[:, k0:k1, :], src[:, k0:k1, c0:c1], W=[w.b[0]], q="pool")
    return w


def phase_na(C, l, need_ctx):
    g, I = C.g, C.I
    QC, KC, VC = 2064, 2576, 3088
    with SS() as s1:
        hts = load_ht_all(C, s1)
        wna = load_w_bf16(C, "w_in", l, QC, D_IN, "wna", s1)
        mask = g.sb("namask", [128, 5, 640], F32, stack=s1)
        g.dma(mask[:], I["na_mask"], W=[mask.b[0]])
        braw = g.sb("nabraw", [128, 5, 640], F32, stack=s1)
        bias = g.sb("nabias", [128, 2, 5, 640], BF16, nslots=2, stack=s1)
        qT = g.sb("naqT", [128, NTOK], BF16, nslots=9, stack=s1)
        kT = g.sb("nakT", [128, NTOK], BF16, nslots=9, stack=s1)
        vx = g.sb("navx", [128, NT, 2, 65], BF16, nslots=NT, stack=s1)
        pT = g.sb("napT", [128, 4, 7, 128], BF16, nslots=4, stack=s1)
        rden = g.sb("narden", [128, 2, 2], F32, nslots=2, stack=s1)
        ona = g.sb("naout", [128, 2, 128], BF16, nslots=2, stack=s1)
        g.memset(vx[:], 1.0, vx.b)
        groups = [(0, 256)] + [(256 + 512 * i, 512) for i in range(8)]
        unit = 0
        for hp in range(4):
            for h2 in range(2):
                h = 2 * hp + h2
                g.dma(braw[:], I["na_bias"][l, h], W=[braw.b[0]])
                g.tt(braw[:], braw[:], mask[:], ALU.add, [braw.b[0], mask.b[0]], [braw.b[0]], eng="pool")
                g.act(bias[:, h2], braw[:], AF.Copy, [braw.b[0]], [bias.b[h2]], scale=8.0)
            for gi, (t0, n) in enumerate(groups):
                tl = list(range(t0 // 128, (t0 + n) // 128))
                for (dst, c0) in ((qT, hp * 128), (kT, (KC - QC) + hp * 128)):
                    p = g.ps()
                    for k in range(8):
                        g.mm(p[:, 0:n], wna[:, k, c0:c0 + 128], hts[:, k, t0:t0 + n], k == 0, k == 7,
                             [wna.b[0]] + [hts.b[t] for t in tl], [p.b[0]])
                    g.cp(dst[:, t0:t0 + n], p[:, 0:n], [p.b[0]], [dst.b[gi]], eng="act" if dst is qT else "dve")
                p = g.ps()
                c0 = (VC - QC) + hp * 128
                for j, t in enumerate(tl):
                    for k in range(8):
                        g.mm(p[:, j * 128:(j + 1) * 128], hts[:, k, t * 128:(t + 1) * 128], wna[:, k, c0:c0 + 128],
                             k == 0, k == 7, [wna.b[0], hts.b[t]], [p.b[0]])
                g.cp(vx[:, tl[0]:tl[-1] + 1, :, 0:64],
                     p[:, 0:n // 128 * 128].rearrange("p (a b c) -> p a b c", b=2, c=64),
                     [p.b[0]], [vx.b[t] for t in tl], eng="dve")

            def tok_group(t):
                return 0 if t < 2 else 1 + (t - 2) // 4

            def attend(qt, local_tiles, cls, h2list=(0, 1)):
                nonlocal unit
                sl = unit % 2
                unit += 1
                keyt = list(local_tiles) + [0, 1]
                nk = len(keyt)
                hb = {}
                for h2 in h2list:
                    base = h2 * 64
                    banks = [g.ps(), g.ps()] if nk > 4 else [g.ps()]
                    hb[h2] = banks
                    for ci, kt in enumerate(keyt):
                        pb = banks[ci // 4]
                        reg = pb[:, (ci % 4) * 128:(ci % 4 + 1) * 128]
                        has_b = ci < len(local_tiles)
                        g.mm(reg, kT[base:base + 64, kt * 128:(kt + 1) * 128], qT[base:base + 64, qt * 128:(qt + 1) * 128],
                             True, not has_b, [kT.b[tok_group(kt)], qT.b[tok_group(qt)]], [pb.b[0]])
                        if has_b:
                            g.mm(reg, C.ident[:], bias[:, h2, cls, ci * 128:(ci + 1) * 128], False, True,
                                 [C.ident.b[0], bias.b[h2]], [pb.b[0]])
                for h2 in h2list:
                    ps_ = sl * 2 + h2
                    for bi, pb in enumerate(hb[h2]):
                        n = min(4, nk - 4 * bi) * 128
                        g.act(pT[:, ps_].rearrange("p a b -> p (a b)")[:, bi * 512:bi * 512 + n],
                              pb[:, 0:n], AF.Exp, [pb.b[0]], [pT.b[ps_]], scale=0.125)
                po = g.ps()
                for h2 in h2list:
                    ps_ = sl * 2 + h2
                    for ci, kt in enumerate(keyt):
                        g.mm(po[:, h2 * 65:(h2 + 1) * 65], pT[:, ps_, ci, :], vx[:, kt, h2, :], ci == 0, ci == nk - 1,
                             [pT.b[ps_], vx.b[kt]], [po.b[0]])
                pov = po[:, 0:130].rearrange("p (h c) -> p h c", c=65)
                g.recip(rden[:, sl, :], pov[:, :, 64], [po.b[0]], [rden.b[sl]])
                for h2 in h2list:
                    g.ts(ona[:, sl, h2 * 64:(h2 + 1) * 64], po[:, h2 * 65:h2 * 65 + 64], rden[:, sl, h2:h2 + 1], ALU.mult,
                         [po.b[0], rden.b[sl]], [ona.b[sl]])
                g.dma(C.O[qt * 128:(qt + 1) * 128, 512 + hp * 128:512 + (hp + 1) * 128], ona[:, sl, :],
                      R=[ona.b[sl]], W=[C.B_O[qt]])

            for m in range(32):
                kp0 = na_kp0(m)
                attend(2 + m, [2 + kp0 + j for j in range(5)], na_cls(m))
            if need_ctx:
                for qt in range(2):
                    attend(qt, [], 0)


def silu_from(g, out, outW, x, xR, tmp, tmpB):
    g.act(tmp, x, AF.Exp, xR, [tmpB], scale=-1.0)
    g.ts(tmp, tmp, 1.0, ALU.add, [tmpB], [tmpB])
    g.recip(tmp, tmp, [tmpB], [tmpB])
    g.tt(out, x, tmp, ALU.mult, list(xR) + [tmpB], outW)


def phase_ret(C, l, need_ctx):
    g, I = C.g, C.I
    with SS() as s1:
        lgb = g.sb("lgb", [128, 8], F32, stack=s1)
        g.dma(lgb[:], I["ret_decay"][l:l + 1].rearrange("o a b -> o (a b)").partition_broadcast(128), W=[lgb.b[0]])
        g.act(lgb[:], lgb[:], AF.Exp, [lgb.b[0]], [lgb.b[0]], scale=-float(np.log(2.0)))
        g.act(lgb[:], lgb[:], AF.Ln, [lgb.b[0]], [lgb.b[0]], scale=-1.0, bias=C.onec[:, 0:1])
        cst = g.sb("retc", [128, 6, 128], F32, stack=s1)
        g.dma(cst[:], I["ret_const"], W=[cst.b[0]])
        pcol = g.sb("retpc", [128, 2, 64], F32, stack=s1)
        g.dma(pcol[:], I["ret_pcol"], W=[pcol.b[0]])
        c128 = g.sb("retc128", [128, 128], F32, stack=s1)
        g.memset(c128[:], 128.0, [c128.b[0]])
        BD = g.sb("retBD", [128, 2, 128], F32, stack=s1)
        g.memset(BD[:], 0.0, [BD.b[0]])
        g.memset(BD[0:64, :, 0:64], 1.0, [BD.b[0]])
        g.memset(BD[64:128, :, 64:128], 1.0, [BD.b[0]])
        MT = g.sb("retMT", [128, 2, 2, 128], BF16, stack=s1)
        g.memset(MT[:], 0.0, [MT.b[0]])
        g.memset(MT[0:64, 0], 1.0, [MT.b[0]])
        g.memset(MT[64:128, 1], 1.0, [MT.b[0]])
        D2 = g.sb("retD2", [128, 4, 128], F32, stack=s1)
        tmpd = g.sb("rettmp", [128, 128], F32, stack=s1)
        QF = g.sb("retQF", [128, 2, 128], F32, stack=s1)
        QB = g.sb("retQB", [128, 2, 128], F32, stack=s1)
        KF = g.sb("retKF", [128, 256], F32, stack=s1)
        KB = g.sb("retKB", [128, 256], F32, stack=s1)
        CDF = g.sb("retCDF", [128, 2, 128], F32, stack=s1)
        CDB = g.sb("retCDB", [128, 2, 128], F32, stack=s1)
        R0 = [lgb.b[0], cst.b[0]]
        for h in range(4):
            f, b = lgb[:, h:h + 1], lgb[:, 4 + h:5 + h]
            g.act(D2[:, h, :], cst[:, 0, :], AF.Exp, R0, [D2.b[0]], scale=f)
            g.tt(D2[:, h, :], D2[:, h, :], cst[:, 2, :], ALU.mult, [D2.b[0], cst.b[0]], [D2.b[0]])
            g.act(tmpd[:], cst[:, 1, :], AF.Exp, R0, [tmpd.b[0]], scale=b)
            g.tt(tmpd[:], tmpd[:], cst[:, 3, :], ALU.mult, [tmpd.b[0], cst.b[0]], [tmpd.b[0]])
            g.tt(D2[:, h, :], D2[:, h, :], tmpd[:], ALU.add, [D2.b[0], tmpd.b[0]], [D2.b[0]])
            g.ts(D2[:, h, :], D2[:, h, :], 0.125, ALU.mult, [D2.b[0]], [D2.b[0]])
            pr, bs = h // 2, (h % 2) * 64
            g.act(QF[bs:bs + 64, pr, :], cst[bs:bs + 64, 4, :], AF.Exp, R0, [QF.b[0]], scale=lgb[bs:bs + 64, h:h + 1])
            g.act(QB[bs:bs + 64, pr, :], cst[bs:bs + 64, 5, :], AF.Exp, R0, [QB.b[0]], scale=lgb[bs:bs + 64, 4 + h:5 + h])
            g.act(KF[:, h * 64:(h + 1) * 64], pcol[:, 0, :], AF.Exp, [lgb.b[0], pcol.b[0]], [KF.b[0]], scale=f)
            g.act(KB[:, h * 64:(h + 1) * 64], pcol[:, 1, :], AF.Exp, [lgb.b[0], pcol.b[0]], [KB.b[0]], scale=b)
            g.act(CDF[bs:bs + 64, pr, :], c128[bs:bs + 64, :], AF.Exp, [lgb.b[0], c128.b[0]], [CDF.b[0]],
                  scale=lgb[bs:bs + 64, h:h + 1])
            g.act(CDB[bs:bs + 64, pr, :], c128[bs:bs + 64, :], AF.Exp, [lgb.b[0], c128.b[0]], [CDB.b[0]],
                  scale=lgb[bs:bs + 64, 4 + h:5 + h])
        g.ts(KF[:], KF[:], 0.125, ALU.mult, [KF.b[0]], [KF.b[0]])
        g.ts(KB[:], KB[:], 0.125, ALU.mult, [KB.b[0]], [KB.b[0]])

        import os
        RS = int(os.environ.get("RET_STOP", "9"))
        if RS <= 1:
            return
        qkT = g.sb("retqkT", [128, NT, 4, 128], BF16, nslots=NT, stack=s1)
        vall = g.sb("retv", [128, NT, 256], BF16, nslots=NT, stack=s1)
        sg = g.sb("retsg", [128, NT, 256], BF16, nslots=NT, stack=s1)
        SinF = g.sb("retSinF", [128, NT, 2, 128], BF16, nslots=NT, stack=s1)
        SinB = g.sb("retSinB", [128, NT, 2, 128], BF16, nslots=NT, stack=s1)
        with SS() as s2:
            wret = load_w_bf16(C, "w_in", l, 0, 1024, "wret", s2)
            kdf = g.sb("retkdf", [128, NT, 256], BF16, nslots=NT, stack=s2)
            kdb = g.sb("retkdb", [128, NT, 256], BF16, nslots=NT, stack=s2)
            ht = g.sb("retht", [128, 2, 8, 128], BF16, nslots=2, stack=s2)
            rope = g.sb("retrope", [128, 2, 2, 256], F32, nslots=2, stack=s2)
            qk32 = g.sb("retqk32", [128, 512], F32, stack=s2)
            ra = g.sb("retra", [128, 4, 256], F32, nslots=2, stack=s2)
            qkr = g.sb("retqkr", [128, 2, 512], BF16, nslots=2, stack=s2)
            for t in range(NT):
                sl = t % 2
                g.dma(ht[:, sl], C.HT[t].rearrange("p (k c) -> p k c", k=8), R=[C.B_HT[t]], W=[ht.b[sl]])
                p0, p1 = g.ps(), g.ps()
                for (p, n0) in ((p0, 0), (p1, 512)):
                    for k in range(8):
                        g.mm(p[:, :], ht[:, sl, k, :], wret[:, k, n0:n0 + 512], k == 0, k == 7,
                             [ht.b[sl], wret.b[0]], [p.b[0]])
                SUB = int(os.environ.get("RET_SUB", "9"))
                if SUB <= 0:
                    continue
                if os.environ.get("RET_V", "1") == "1":
                    g.cp(vall[:, t, :], p1[:, 0:256], [p1.b[0]], [vall.b[t]], eng="dve")
                if os.environ.get("RET_G", "1") == "1":
                    silu_from(g, sg[:, t, :], [sg.b[t]], p1[:, 256:512], [p1.b[0]], qk32[:, 0:256], qk32.b[0])
                if SUB <= 1:
                    continue
                if t >= 2:
                    g.dma(rope[:, sl], I["rope"][(t - 2) * 128:(t - 1) * 128], W=[rope.b[sl]])
                    g.cp(qk32[:], p0[:, :], [p0.b[0]], [qk32.b[0]], eng="act")
                    v4 = qk32[:].rearrange("p (h a c) -> p h a c", a=2, c=32)
                    t1, t2 = v4[:, :, 0, :], v4[:, :, 1, :]
                    cs = rope[:, sl, 0, :].rearrange("p (h c) -> p h c", c=32)
                    sn = rope[:, sl, 1, :].rearrange("p (h c) -> p h c", c=32)
                    rv = [ra[:, i, :].rearrange("p (h c) -> p h c", c=32) for i in range(4)]
                    RR = [qk32.b[0], rope.b[sl]]
                    g.tt(rv[0], t1, cs, ALU.mult, RR, [ra.b[0]], eng="dve")
                    g.tt(rv[1], t2, sn, ALU.mult, RR, [ra.b[0]], eng="pool")
                    g.tt(rv[2], t1, sn, ALU.mult, RR, [ra.b[1]], eng="dve")
                    g.tt(rv[3], t2, cs, ALU.mult, RR, [ra.b[1]], eng="pool")
                    o4 = qkr[:, sl, :].rearrange("p (h a c) -> p h a c", a=2, c=32)
                    g.tt(o4[:, :, 0, :], rv[0], rv[1], ALU.subtract, [ra.b[0]], [qkr.b[sl]], eng="dve")
                    g.tt(o4[:, :, 1, :], rv[2], rv[3], ALU.add, [ra.b[1]], [qkr.b[sl]], eng="pool")
                else:
                    g.cp(qkr[:, sl, :], p0[:, :], [p0.b[0]], [qkr.b[sl]], eng="act")
                if SUB <= 2:
                    continue
                g.tt(kdf[:, t, :], qkr[:, sl, 256:512], KF[:], ALU.mult, [qkr.b[sl], KF.b[0]], [kdf.b[t]], eng="dve")
                g.tt(kdb[:, t, :], qkr[:, sl, 256:512], KB[:], ALU.mult, [qkr.b[sl], KB.b[0]], [kdb.b[t]], eng="pool")
                if SUB <= 3:
                    continue
                pt = g.ps()
                ptb = pt[:].bitcast(BF16)
                for c4 in range(4):
                    g.tr(ptb[:, c4 * 128:(c4 + 1) * 128], qkr[:, sl, c4 * 128:(c4 + 1) * 128], C.ident[:],
                         [qkr.b[sl], C.ident.b[0]], [pt.b[0]])
                g.cp(qkT[:, t].rearrange("p a b -> p (a b)"), ptb[:, 0:512], [pt.b[0]], [qkT.b[t]], eng="act")

            if RS <= 2:
                return
            S = g.sb("retS", [128, 2, 2, 128], F32, nslots=2, stack=s2)
            tS = g.sb("rettS", [128, 2, 2, 128], F32, nslots=2, stack=s2)
            g.memset(S[:], 0.0, S.b)
            order_f = list(range(NT))
            order_b = [1, 0] + list(range(NT - 1, 1, -1))
            for step in range(NT):
                for d, (order, kd, Sin, CDt) in enumerate(((order_f, kdf, SinF, CDF), (order_b, kdb, SinB, CDB))):
                    t = order[step]
                    p = g.ps()
                    for pr in range(2):
                        g.mm(p[:, pr * 128:(pr + 1) * 128], kd[:, t, pr * 128:(pr + 1) * 128], vall[:, t, pr * 128:(pr + 1) * 128],
                             True, True, [kd.b[t], vall.b[t]], [p.b[0]])
                    g.tt(tS[:, d].rearrange("p a b -> p (a b)"), p[:, 0:256], BD[:].rearrange("p a b -> p (a b)"), ALU.mult,
                         [p.b[0], BD.b[0]], [tS.b[d]], eng="dve")
                    g.cp(Sin[:, t], S[:, d], [S.b[d]], [Sin.b[t]], eng="act")
                    g.tt(S[:, d], S[:, d], CDt[:], ALU.mult, [S.b[d], CDt.b[0]], [S.b[d]], eng="pool")
                    g.tt(S[:, d], S[:, d], tS[:, d], ALU.add, [S.b[d], tS.b[d]], [S.b[d]], eng="pool")

        if RS <= 3:
            return
        with SS() as s2:
            AT = g.sb("retAT", [128, 2, 4, 128], BF16, nslots=2, stack=s2)
            qm = g.sb("retqm", [128, 2, 4, 128], BF16, nslots=2, stack=s2)
            qsf = g.sb("retqsf", [128, 2, 2, 128], BF16, nslots=2, stack=s2)
            qsb = g.sb("retqsb", [128, 2, 2, 128], BF16, nslots=2, stack=s2)
            o32 = g.sb("reto32", [128, 2, 256], F32, nslots=2, stack=s2)
            osq = g.sb("retosq", [128, 256], F32, stack=s2)
            rs = g.sb("retrs", [128, 2, 4], F32, nslots=2, stack=s2)
            ob = g.sb("retob", [128, 2, 256], BF16, nslots=2, stack=s2)
            for t in range(NT):
                if t < 2 and not need_ctx:
                    continue
                sl = t % 2
                for par in range(2):
                    g.tt(qm[:, sl, 2 * par:2 * par + 2, :], qkT[:, t, 0:2, :], MT[:, par], ALU.mult, [qkT.b[t], MT.b[0]],
                         [qm.b[sl]], eng="pool")
                ps_ = g.ps()
                for h in range(4):
                    pr, par = h // 2, h % 2
                    g.mm(ps_[:, h * 128:(h + 1) * 128], qkT[:, t, 2 + pr, :], qm[:, sl, 2 * par + pr, :], True, True,
                         [qkT.b[t], qm.b[sl]], [ps_.b[0]])
                g.tt(AT[:, sl].rearrange("p a b -> p (a b)"), ps_[:, :], D2[:].rearrange("p a b -> p (a b)"), ALU.mult,
                     [ps_.b[0], D2.b[0]], [AT.b[sl]], eng="dve")
                g.tt(qsf[:, sl], qkT[:, t, 0:2, :], QF[:], ALU.mult, [qkT.b[t], QF.b[0]], [qsf.b[sl]], eng="pool")
                g.tt(qsb[:, sl], qkT[:, t, 0:2, :], QB[:], ALU.mult, [qkT.b[t], QB.b[0]], [qsb.b[sl]], eng="pool")
                po = g.ps()
                for pr in range(2):
                    reg = po[:, pr * 128:(pr + 1) * 128]
                    g.mm(reg, qsf[:, sl, pr, :], SinF[:, t, pr, :], True, False, [qsf.b[sl], SinF.b[t]], [po.b[0]])
                    g.mm(reg, qsb[:, sl, pr, :], SinB[:, t, pr, :], False, False, [qsb.b[sl], SinB.b[t]], [po.b[0]])
                    for par in range(2):
                        h = 2 * pr + par
                        g.mm(po[:, h * 64:(h + 1) * 64], AT[:, sl, h, :], vall[:, t, h * 64:(h + 1) * 64], False, par == 1,
                             [AT.b[sl], vall.b[t]], [po.b[0]])
                g.cp(o32[:, sl, :], po[:, 0:256], [po.b[0]], [o32.b[sl]], eng="act")
                g.tt(osq[:], o32[:, sl, :], o32[:, sl, :], ALU.mult, [o32.b[sl]], [osq.b[0]], eng="pool")
                g.K.op("dve", lambda h_, o_=rs[:, sl, :], i_=osq[:].rearrange("p (h c) -> p h c", c=64):
                       h_.tensor_reduce(out=o_, in_=i_, axis=AX.X, op=ALU.add), reads=[osq.b[0]], writes=[rs.b[sl]])
                g.act(rs[:, sl, :], rs[:, sl, :], AF.Sqrt, [rs.b[sl]], [rs.b[sl]], bias=C.epsc[:, 0:1], scale=1.0 / 64)
                g.recip(rs[:, sl, :], rs[:, sl, :], [rs.b[sl]], [rs.b[sl]])
                for h in range(4):
                    g.stt(ob[:, sl, h * 64:(h + 1) * 64], o32[:, sl, h * 64:(h + 1) * 64], rs[:, sl, h:h + 1],
                          sg[:, t, h * 64:(h + 1) * 64], ALU.mult, ALU.mult, [o32.b[sl], rs.b[sl], sg.b[t]], [ob.b[sl]])
                g.dma(C.O[t * 128:(t + 1) * 128, 0:256], ob[:, sl, :], R=[ob.b[sl]], W=[C.B_O[t]])


def phase_gdn(C, l, need_ctx):
    g, I = C.g, C.I
    NCH = NTOK // 64
    order = [list(range(NCH)), [3, 2, 1, 0] + list(range(NCH - 1, 3, -1))]
    id64 = C.ident[0:64, 0:64]
    with SS() as s1:
        cst = g.sb("gdnc", [64, 13, 64], F32, stack=s1)
        g.dma(cst[:], I["gdn_const"], W=[cst.b[0]])
        CB = cst.b[0]
        Y4, SU4, I4 = cst[:, 0:4, :], cst[:, 4:8, :], cst[:, 8:12, :]
        idf, ones = cst[:, 8, :], cst[:, 12, :]
        one64 = C.onec[0:64, 0:1]
        eps64 = C.epsc[0:64, 0:1]
        ab = g.sb("gab", [64, 16, NCH], F32, stack=s1)
        with SS() as s2:
            ht = g.sb("ght", [128, 2, 8, 128], BF16, nslots=2, stack=s2)
            wab = load_w_bf16(C, "w_in", l, 2048, 2064, "wab", s2)
            p = None
            for t in range(NT):
                sl = t % 2
                g.dma(ht[:, sl], C.HT[t].rearrange("p (k c) -> p k c", k=8), R=[C.B_HT[t]], W=[ht.b[sl]])
                for half in range(2):
                    c = 2 * t + half
                    if c % 32 == 0:
                        p = g.ps()
                        c0 = c
                    reg = p[0:64, (c - c0) * 16:(c - c0 + 1) * 16]
                    for k in range(8):
                        g.mm(reg, ht[:, sl, k, half * 64:(half + 1) * 64], wab[:, k, 0:16], k == 0, k == 7,
                             [ht.b[sl], wab.b[0]], [p.b[0]])
                    if c % 32 == 31 or c == NCH - 1:
                        n = c - c0 + 1
                        g.cp(ab[:, :, c0:c0 + n].rearrange("p k c -> p c k"),
                             p[0:64, 0:n * 16].rearrange("p (c k) -> p c k", k=16), [p.b[0]], [ab.b[0]], eng="dve")
        prm = g.sb("gprm", [64, 2, 8], F32, stack=s1)
        g.dma(prm[:, 0, :], I["gdn_a_log"][l:l + 1].rearrange("o a b -> o (a b)").partition_broadcast(64), W=[prm.b[0]])
        g.dma(prm[:, 1, :], I["gdn_dt_bias"][l:l + 1].rearrange("o a b -> o (a b)").partition_broadcast(64), W=[prm.b[0]])
        sc = {}
        for nm in ("G", "beta", "nbeta", "egi", "erem", "egl", "begi"):
            sc[nm] = g.sb("g" + nm, [64, 8, NCH], F32, stack=s1)
        G, beta, nbeta, egi, erem, egl, begi = [sc[k] for k in ("G", "beta", "nbeta", "egi", "erem", "egl", "begi")]
        bc8 = lambda ap: ap.unsqueeze(2).to_broadcast([64, 8, NCH])
        g.tt(G[:], ab[:, 0:8, :], bc8(prm[:, 1, :]), ALU.add, [ab.b[0], prm.b[0]], [G.b[0]])
        g.act(G[:], G[:], AF.Exp, [G.b[0]], [G.b[0]])
        g.act(G[:], G[:], AF.Ln, [G.b[0]], [G.b[0]], bias=one64)
        g.act(prm[:, 0, :], prm[:, 0, :], AF.Exp, [prm.b[0]], [prm.b[0]])
        g.ts(prm[:, 0, :], prm[:, 0, :], -1.0, ALU.mult, [prm.b[0]], [prm.b[0]])
        g.tt(G[:], G[:], bc8(prm[:, 0, :]), ALU.mult, [G.b[0], prm.b[0]], [G.b[0]])
        g.act(beta[:], ab[:, 8:16, :], AF.Exp, [ab.b[0]], [beta.b[0]], scale=-1.0)
        g.ts(beta[:], beta[:], 1.0, ALU.add, [beta.b[0]], [beta.b[0]])
        g.recip(beta[:], beta[:], [beta.b[0]], [beta.b[0]])
        g.ts(nbeta[:], beta[:], -1.0, ALU.mult, [beta.b[0]], [nbeta.b[0]])
        for d in range(2):
            rhs = G[:, d * 4:(d + 1) * 4, :].rearrange("p a c -> p (a c)")
            for (dst, lhs) in ((egi, cst[:, d, :]), (erem, cst[:, 4 + d, :]), (egl, ones)):
                p = g.ps()
                g.mm(p[0:64, 0:4 * NCH], lhs, rhs, True, True, [CB, G.b[0]], [p.b[0]])
                g.act(dst[:, d * 4:(d + 1) * 4, :].rearrange("p a c -> p (a c)"), p[0:64, 0:4 * NCH], AF.Exp, [p.b[0]],
                      [dst.b[0]])
        g.tt(begi[:], beta[:], egi[:], ALU.mult, [beta.b[0], egi.b[0]], [begi.b[0]])
        ngb = g.sb("gngb", [64, 64], F32, stack=s1)
        g.dma(ngb[:], I["gdn_norm_g"][l:l + 1, :].partition_broadcast(64), W=[ngb.b[0]])

        for hp in range(2):
            with SS() as s2:
                qT = g.sb("gqT", [64, 2, NTOK], BF16, nslots=2, stack=s2)
                kT = g.sb("gkT", [64, 2, NTOK], BF16, nslots=2, stack=s2)
                ktm = g.sb("gktm", [64, 2, NCH, 64], BF16, nslots=2, stack=s2)
                vtm = g.sb("gvtm", [64, 2, NCH, 64], BF16, nslots=2, stack=s2)
                sgt = g.sb("gsgt", [64, 2, NCH, 64], BF16, stack=s2)
                with SS() as s3:
                    wg = load_w_bf16(C, "w_in", l, 1024, 2048, "wg", s3)
                    raw = g.sb("graw", [128, NTOK + 8], F32, stack=s3)
                    acc = g.sb("gacc", [128, NTOK], F32, stack=s3)
                    o128 = g.sb("go128", [128, NTOK], BF16, stack=s3)
                    hg = g.sb("ghg", [128, 2, 8, 512], BF16, nslots=2, stack=s3)
                    cw = g.sb("gcw", [128, 3, 5], F32, stack=s3)
                    sq = g.sb("gsq", [128, 2, 512], F32, nslots=2, stack=s3)
                    bd1 = g.sb("gbd1", [128, 128], F32, stack=s3)
                    g.dma(bd1[:], I["gdn_bd"], W=[bd1.b[0]])
                    g.memset(raw[:], 0.0, [raw.b[0]])
                    g.dma(cw[:], I["gdn_convw"][l, 2 * hp:2 * hp + 2].rearrange("h d t k -> (h d) t k"), W=[cw.b[0]])
                    groups = [(0, 256)] + [(256 + 512 * i, 512) for i in range(8)]
                    git = 0
                    for typ in range(3):
                        wc0 = typ * 256 + hp * 128
                        for (t0, n) in groups:
                            sl = git % 2
                            git += 1
                            for q in range(n // 128):
                                t = t0 // 128 + q
                                g.dma(hg[:, sl, :, q * 128:(q + 1) * 128], C.HT[t].rearrange("p (k c) -> p k c", k=8),
                                      R=[C.B_HT[t]], W=[hg.b[sl]])
                            p = g.ps()
                            for k in range(8):
                                g.mm(p[:, 0:n], wg[:, k, wc0:wc0 + 128], hg[:, sl, k, 0:n], k == 0, k == 7,
                                     [wg.b[0], hg.b[sl]], [p.b[0]])
                            off = t0 + 2 if t0 < 256 else t0 + 6
                            g.cp(raw[:, off:off + n], p[:, 0:n], [p.b[0]], [raw.b[0]], eng="act")
                        for (a0, n, r0) in ((0, 256, 0), (256, 2048, 260), (2304, 2048, 2308)):
                            for k in range(5):
                                src = raw[:, r0 + k:r0 + k + n]
                                if k == 0:
                                    g.ts(acc[:, a0:a0 + n], src, cw[:, typ, 0:1], ALU.mult, [raw.b[0], cw.b[0]], [acc.b[0]])
                                else:
                                    g.stt(acc[:, a0:a0 + n], src, cw[:, typ, k:k + 1], acc[:, a0:a0 + n], ALU.mult, ALU.add,
                                          [raw.b[0], cw.b[0], acc.b[0]], [acc.b[0]])
                        g.act(acc[:], acc[:], AF.Silu, [acc.b[0]], [acc.b[0]])
                        if typ == 2:
                            g.cp(o128[:], acc[:], [acc.b[0]], [o128.b[0]], eng="pool")
                        else:
                            for gi2, (t0, n) in enumerate(groups):
                                ss = gi2 % 2
                                g.tt(sq[:, ss, 0:n], acc[:, t0:t0 + n], acc[:, t0:t0 + n], ALU.mult, [acc.b[0]], [sq.b[ss]],
                                     eng="pool")
                                p = g.ps()
                                g.mm(p[:, 0:n], bd1[:], sq[:, ss, 0:n], True, True, [bd1.b[0], sq.b[ss]], [p.b[0]])
                                g.act(sq[:, ss, 0:n], p[:, 0:n], AF.Sqrt, [p.b[0]], [sq.b[ss]], bias=C.epsc[:, 0:1])
                                g.recip(sq[:, ss, 0:n], sq[:, ss, 0:n], [sq.b[ss]], [sq.b[ss]])
                                g.stt(o128[:, t0:t0 + n], acc[:, t0:t0 + n], 0.125 if typ == 0 else 1.0, sq[:, ss, 0:n],
                                      ALU.mult, ALU.mult, [acc.b[0], sq.b[ss]], [o128.b[0]])
                            dst = qT if typ == 0 else kT
                            for hh in range(2):
                                g.dma(dst[:, hh, :], o128[hh * 64:(hh + 1) * 64, :], R=[o128.b[0]], W=[dst.b[hh]])
                        if typ >= 1:
                            dst_tm = ktm if typ == 1 else vtm
                            for c0 in range(0, NCH, 8):
                                nn = min(8, NCH - c0)
                                p = g.ps()
                                pb = p[:].bitcast(BF16)
                                for ci in range(nn):
                                    c = c0 + ci
                                    g.tr(pb[0:64, ci * 128:(ci + 1) * 128], o128[:, c * 64:(c + 1) * 64], C.ident[:],
                                         [o128.b[0], C.ident.b[0]], [p.b[0]])
                                g.cp(dst_tm[:, :, c0:c0 + nn, :], pb[0:64, 0:nn * 128].rearrange("p (c h d) -> p h c d", h=2, d=64),
                                     [p.b[0]], [dst_tm.b[0], dst_tm.b[1]], eng="act")
                    ht2 = g.sb("ght2", [128, 2, 8, 128], BF16, nslots=2, stack=s3)
                    sg32 = g.sb("gsg32", [64, 512], F32, stack=s3)
                    p = None
                    for t in range(NT):
                        sl = t % 2
                        g.dma(ht2[:, sl], C.HT[t].rearrange("p (k c) -> p k c", k=8), R=[C.B_HT[t]], W=[ht2.b[sl]])
                        for half in range(2):
                            c = 2 * t + half
                            if c % 4 == 0:
                                p = g.ps()
                                c0 = c
                            reg = p[0:64, (c - c0) * 128:(c - c0 + 1) * 128]
                            for k in range(8):
                                g.mm(reg, ht2[:, sl, k, half * 64:(half + 1) * 64], wg[:, k, 768 + hp * 128:768 + (hp + 1) * 128],
                                     k == 0, k == 7, [ht2.b[sl], wg.b[0]], [p.b[0]])
                            if c % 4 == 3:
                                g.cp(sg32[:], p[0:64, :], [p.b[0]], [sg32.b[0]], eng="act")
                                g.act(sgt[:, :, c0:c0 + 4, :], sg32[:].rearrange("p (c h d) -> p h c d", h=2, d=64), AF.Silu,
                                      [sg32.b[0]], [sgt.b[0]])

                Oacc = g.sb("gOacc", [64, 2, NCH, 64], F32, nslots=2, stack=s2)
                with SS() as s3:
                    NSET, RS_ = 3, 4

                    def tn(nm, dt, ns, w=64, extra=()):
                        return g.sb(nm, [64, ns] + list(extra) + [4, w] if not extra else [64, ns] + list(extra), dt, nslots=ns, stack=s3)
                    X4 = tn("gX4", F32, NSET)
                    E8 = g.sb("gE8", [64, NSET, 4, 2, 64], F32, nslots=NSET, stack=s3)
                    DT4, DS4, P0, P0T, TT = [tn(n_, F32, NSET) for n_ in ("gDT4", "gDS4", "gP0", "gP0T", "gTT")]
                    PP = g.sb("gPP", [64, NSET * 2, 4, 2, 64], F32, nslots=NSET * 2, stack=s3)
                    TTb, vb4, kbeg4 = [tn(n_, BF16, NSET) for n_ in ("gTTb", "gvb4", "gkbeg4")]
                    attn4, wTb, kdec4 = [tn(n_, BF16, RS_) for n_ in ("gattn4", "gwTb", "gkdec4")]
                    U4 = tn("gU4", F32, RS_)
                    S = g.sb("gS", [64, 4, 64], F32, stack=s3)
                    Sb = g.sb("gSb", [64, 4, 64], BF16, stack=s3)
                    vn4 = g.sb("gvn4", [64, 4, 64], BF16, stack=s3)
                    qs4 = g.sb("gqs4", [64, 4, 64], F32, stack=s3)
                    g.memset(S[:], 0.0, [S.b[0]])
                    g.memset(Sb[:], 0.0, [Sb.b[0]])
                    idr = g.sb("gidr", [64, 64], F32, stack=s3)
                    g.cp(idr[:].bitcast(F32R), idf, [CB], [idr.b[0]])
                    RR_ = lambda ap: ap.bitcast(F32R)
                    visited = set()
                    units = [(hh, d) for hh in range(2) for d in range(2)]

                    def col(tile_, u, step):
                        hh, d = units[u]
                        c = order[d][step]
                        dh = d * 4 + 2 * hp + hh
                        return tile_[:, dh, c:c + 1]

                    def indep(step):
                        ts_ = step % NSET
                        sl = step % RS_
                        cs = [order[d][step] for (hh, d) in units]
                        for u in range(4):
                            g.ts(X4[:, ts_, u, :], SU4[:, u, :], col(G, u, step), ALU.mult, [CB, G.b[0]], [X4.b[ts_]])
                        yield
                        pa, pbk = g.ps(), g.ps()
                        pav = pa[0:64, :].rearrange("p (u t i) -> p u t i", u=4, t=2)
                        pbv = pbk[0:64, :].rearrange("p (u t i) -> p u t i", u=4, t=2)
                        for u, (hh, d) in enumerate(units):
                            g.mm(pav[:, u, 0, :], X4[:, ts_, u, :], cst[:, d, :], True, True, [X4.b[ts_], CB], [pa.b[0]])
                            g.mm(pav[:, u, 1, :], cst[:, d, :], X4[:, ts_, u, :], True, True, [X4.b[ts_], CB], [pa.b[0]])
                        for u, (hh, d) in enumerate(units):
                            c = cs[u]
                            kc, qc = kT[:, hh, c * 64:(c + 1) * 64], qT[:, hh, c * 64:(c + 1) * 64]
                            g.mm(pbv[:, u, 0, :], kc, qc, True, True, [kT.b[hh], qT.b[hh]], [pbk.b[0]])
                            g.mm(pbv[:, u, 1, :], kc, kc, True, True, [kT.b[hh]], [pbk.b[0]])
                        yield
                        g.act(E8[:, ts_].rearrange("p u t i -> p (u t i)"), pa[0:64, :], AF.Exp, [pa.b[0]], [E8.b[ts_]])
                        yield
                        g.tt(DT4[:, ts_], E8[:, ts_, :, 0, :], Y4, ALU.mult, [E8.b[ts_], CB], [DT4.b[ts_]], eng="pool")
                        g.tt(DS4[:, ts_], E8[:, ts_, :, 1, :], SU4, ALU.mult, [E8.b[ts_], CB], [DS4.b[ts_]], eng="pool")
                        yield
                        for u in range(4):
                            g.stt(RR_(P0[:, ts_, u, :]), pbv[:, u, 1, :], col(nbeta, u, step), DS4[:, ts_, u, :], ALU.mult, ALU.mult,
                                  [pbk.b[0], nbeta.b[0], DS4.b[ts_]], [P0.b[ts_]])
                        g.tt(attn4[:, sl], pbv[:, :, 0, :], DT4[:, ts_], ALU.mult, [pbk.b[0], DT4.b[ts_]], [attn4.b[sl]])
                        yield
                        pc = g.ps()
                        pcv = pc[0:64, 0:256].rearrange("p (u i) -> p u i", u=4)
                        for u in range(4):
                            g.mm(pcv[:, u, :], RR_(P0[:, ts_, u, :]), RR_(idr[:]), True, True, [P0.b[ts_], idr.b[0]], [pc.b[0]])
                        yield
                        g.cp(RR_(P0T[:, ts_].rearrange("p u i -> p (u i)")), pc[0:64, 0:256], [pc.b[0]], [P0T.b[ts_]], eng="act")
                        yield
                        g.tt(RR_(TT[:, ts_]), P0T[:, ts_], I4, ALU.add, [P0T.b[ts_], CB], [TT.b[ts_]], eng="pool")
                        Pk, PkT, PkB = P0[:, ts_], P0T[:, ts_], [P0.b[ts_], P0T.b[ts_]]

                        def sq_mm(k, Pk, PkT, PkB):
                            pd = g.ps()
                            pdv = pd[0:64, :].rearrange("p (u t i) -> p u t i", u=4, t=2)
                            for u in range(4):
                                g.mm(pdv[:, u, 0, :], RR_(PkT[:, u, :]), RR_(Pk[:, u, :]), True, True, PkB, [pd.b[0]])
                                if k < 4:
                                    g.mm(pdv[:, u, 1, :], RR_(Pk[:, u, :]), RR_(PkT[:, u, :]), True, True, PkB, [pd.b[0]])
                            return pd

                        pd = sq_mm(0, Pk, PkT, PkB)
                        yield
                        for k in range(5):
                            ps_ = ts_ * 2 + k % 2
                            g.cp(RR_(PP[:, ps_].rearrange("p u t i -> p (u t i)")), pd[0:64, :], [pd.b[0]], [PP.b[ps_]], eng="act")
                            yield
                            Pk, PkT, PkB = PP[:, ps_, :, 0, :], PP[:, ps_, :, 1, :], [PP.b[ps_]]
                            if k < 4:
                                pd = sq_mm(k + 1, Pk, PkT, PkB)
                            pe_ = g.ps()
                            pev = pe_[0:64, 0:256].rearrange("p (u i) -> p u i", u=4)
                            for u in range(4):
                                g.mm(pev[:, u, :], RR_(Pk[:, u, :]), RR_(TT[:, ts_, u, :]), True, True, PkB + [TT.b[ts_]], [pe_.b[0]])
                            yield
                            g.tt(RR_(TT[:, ts_]), TT[:, ts_], pev, ALU.add, [TT.b[ts_], pe_.b[0]], [TT.b[ts_]])
                        g.cp(TTb[:, ts_], TT[:, ts_], [TT.b[ts_]], [TTb.b[ts_]], eng="act")
                        for u, (hh, d) in enumerate(units):
                            c = cs[u]
                            g.ts(vb4[:, ts_, u, :], vtm[:, hh, c, :], col(beta, u, step), ALU.mult, [vtm.b[hh], beta.b[0]], [vb4.b[ts_]])
                            g.ts(kbeg4[:, ts_, u, :], ktm[:, hh, c, :], col(begi, u, step), ALU.mult, [ktm.b[hh], begi.b[0]],
                                 [kbeg4.b[ts_]])
                            g.ts(kdec4[:, sl, u, :], ktm[:, hh, c, :], col(erem, u, step), ALU.mult, [ktm.b[hh], erem.b[0]],
                                 [kdec4.b[sl]])
                        yield
                        pf = g.ps()
                        pfv = pf[0:64, :].rearrange("p (u t i) -> p u t i", u=4, t=2)
                        for u in range(4):
                            g.mm(pfv[:, u, 0, :], TTb[:, ts_, u, :], vb4[:, ts_, u, :], True, True, [TTb.b[ts_], vb4.b[ts_]], [pf.b[0]])
                            g.mm(pfv[:, u, 1, :], kbeg4[:, ts_, u, :], TTb[:, ts_, u, :], True, True, [TTb.b[ts_], kbeg4.b[ts_]],
                                 [pf.b[0]])
                        yield
                        g.cp(U4[:, sl], pfv[:, :, 0, :], [pf.b[0]], [U4.b[sl]], eng="act")
                        g.cp(wTb[:, sl], pfv[:, :, 1, :], [pf.b[0]], [wTb.b[sl]], eng="act")

                    def dep(step):
                        sl = step % RS_
                        cs = [order[d][step] for (hh, d) in units]
                        pg = g.ps()
                        pgv = pg[0:64, :].rearrange("p (u t i) -> p u t i", u=4, t=2)
                        for u, (hh, d) in enumerate(units):
                            c = cs[u]
                            g.mm(pgv[:, u, 0, :], wTb[:, sl, u, :], Sb[:, u, :], True, True, [wTb.b[sl], Sb.b[0]], [pg.b[0]])
                            g.mm(pgv[:, u, 1, :], qT[:, hh, c * 64:(c + 1) * 64], Sb[:, u, :], True, True, [qT.b[hh], Sb.b[0]],
                                 [pg.b[0]])
                        g.tt(vn4[:], U4[:, sl], pgv[:, :, 0, :], ALU.subtract, [U4.b[sl], pg.b[0]], [vn4.b[0]])
                        for u in range(4):
                            g.act(qs4[:, u, :], pgv[:, u, 1, :], AF.Copy, [pg.b[0], egi.b[0]], [qs4.b[0]], scale=col(egi, u, step))
                        ph = g.ps()
                        phv = ph[0:64, :].rearrange("p (u t i) -> p u t i", u=4, t=2)
                        for u in range(4):
                            g.mm(phv[:, u, 0, :], kdec4[:, sl, u, :], vn4[:, u, :], True, True, [kdec4.b[sl], vn4.b[0]], [ph.b[0]])
                            g.mm(phv[:, u, 1, :], attn4[:, sl, u, :], vn4[:, u, :], True, True, [attn4.b[sl], vn4.b[0]], [ph.b[0]])
                        for u in range(4):
                            g.stt(S[:, u, :], S[:, u, :], col(egl, u, step), phv[:, u, 0, :], ALU.mult, ALU.add,
                                  [S.b[0], egl.b[0], ph.b[0]], [S.b[0]])
                        g.cp(Sb[:], S[:], [S.b[0]], [Sb.b[0]], eng="act")
                        for u, (hh, d) in enumerate(units):
                            c = cs[u]
                            if (hh, c) not in visited:
                                visited.add((hh, c))
                                g.tt(Oacc[:, hh, c, :], qs4[:, u, :], phv[:, u, 1, :], ALU.add, [qs4.b[0], ph.b[0]], [Oacc.b[hh]])
                            else:
                                g.tt(qs4[:, u, :], qs4[:, u, :], phv[:, u, 1, :], ALU.add, [qs4.b[0], ph.b[0]], [qs4.b[0]])
                                g.tt(Oacc[:, hh, c, :], Oacc[:, hh, c, :], qs4[:, u, :], ALU.add, [qs4.b[0], Oacc.b[hh]],
                                     [Oacc.b[hh]], eng="pool")

                    NS = int(os.environ.get("GDN_STEPS", str(NCH)))
                    gens = []
                    done = set()
                    nxt_i, nxt_d = 0, 0
                    while nxt_d < NS:
                        while len(gens) < NSET and nxt_i < NS and nxt_i < nxt_d + RS_:
                            gens.append((nxt_i, indep(nxt_i)))
                            nxt_i += 1
                        still = []
                        for (st_, ge_) in gens:
                            try:
                                next(ge_)
                                still.append((st_, ge_))
                            except StopIteration:
                                done.add(st_)
                        gens = still
                        while nxt_d in done:
                            dep(nxt_d)
                            nxt_d += 1
                    if C.DBG is not None and hp == 0 and NS < NCH:
                        o_ = 0
                        for nm_, tl_, ap_ in (("S", S, S[:]), ("qs4", qs4, qs4[:])):
                            g.dma(C.DBG[:, o_:o_ + 256].rearrange("p (u i) -> p u i", u=4), ap_, R=tl_.b, W=[C.B_DBG], q="pool")
                            o_ += 256

                if C.DBG is not None and hp == 0 and "GDN_STEPS" not in os.environ:
                    o_ = 0
                    for tl_ in (G, beta, egi, erem, egl):
                        g.dma(C.DBG[:, o_:o_ + 8 * NCH], tl_[:].rearrange("p a c -> p (a c)"), R=[tl_.b[0]], W=[C.B_DBG], q="pool")
                        o_ += 8 * NCH
                    g.dma(C.DBG[:, 3000:3512], qT[:, 0, 0:512], R=[qT.b[0]], W=[C.B_DBG], q="pool")
                    g.dma(C.DBG[:, 3512:4024], kT[:, 0, 0:512], R=[kT.b[0]], W=[C.B_DBG], q="pool")
                    g.dma(C.DBG[:, 4024:4536], ktm[:, 0, 0:8, :].rearrange("p c d -> p (c d)"), R=[ktm.b[0]], W=[C.B_DBG], q="pool")
                    g.dma(C.DBG[:, 4536:5048], vtm[:, 0, 0:8, :].rearrange("p c d -> p (c d)"), R=[vtm.b[0]], W=[C.B_DBG], q="pool")
                    g.dma(C.DBG[:, 5048:5560], Oacc[:, 0, 0:8, :].rearrange("p c d -> p (c d)"), R=[Oacc.b[0]], W=[C.B_DBG], q="pool")
                    g.dma(C.DBG[:, 5560:6072], Oacc[:, 0, 60:68, :].rearrange("p c d -> p (c d)"), R=[Oacc.b[0]], W=[C.B_DBG], q="pool")
                    g.dma(C.DBG[:, 6072:6584], sgt[:, 0, 0:8, :].rearrange("p c d -> p (c d)"), R=[sgt.b[0]], W=[C.B_DBG], q="pool")
                with SS() as s3:
                    osq = g.sb("gosq", [64, 2 * NCH, 64], F32, stack=s3)
                    rs = g.sb("grs", [64, 2 * NCH], F32, stack=s3)
                    ob = vtm
                    OB = [Oacc.b[0], Oacc.b[1]]
                    Ov = Oacc[:].rearrange("p h c d -> p (h c) d")
                    g.tt(osq[:], Ov, Ov, ALU.mult, OB, [osq.b[0]], eng="pool")
                    g.K.op("dve", lambda h_: h_.tensor_reduce(out=rs[:], in_=osq[:], axis=AX.X, op=ALU.add), reads=[osq.b[0]],
                           writes=[rs.b[0]])
                    g.act(rs[:], rs[:], AF.Sqrt, [rs.b[0]], [rs.b[0]], bias=eps64, scale=1.0 / 64)
                    g.recip(rs[:], rs[:], [rs.b[0]], [rs.b[0]])
                    g.tt(osq[:], Ov, rs[:].unsqueeze(2).to_broadcast([64, 2 * NCH, 64]), ALU.mult, OB + [rs.b[0]], [osq.b[0]])
                    g.tt(osq[:], osq[:], ngb[:].unsqueeze(1).to_broadcast([64, 2 * NCH, 64]), ALU.mult, [osq.b[0], ngb.b[0]],
                         [osq.b[0]])
                    g.tt(ob[:].rearrange("p h c d -> p (h c) d"), osq[:], sgt[:].rearrange("p h c d -> p (h c) d"), ALU.mult,
                         [osq.b[0], sgt.b[0]], [ob.b[0], ob.b[1]])
                    for hh in range(2):
                        h = 2 * hp + hh
                        g.dma(C.O.rearrange("(c p) n -> p c n", p=64)[:, :, 256 + h * 64:256 + (h + 1) * 64], ob[:, hh],
                              R=[ob.b[0], ob.b[1]], W=C.B_O)


def phase_wout(C, l, need_ctx):
    g, I = C.g, C.I
    xsrc = I["xin"] if l == 0 else C.XS
    with SS() as s1:
        wout = load_w_bf16(C, "w_out", l, 0, D, "wout", s1)
        Abc, Sbc = norm_mod_tiles(C, l, 2, s1)
        G1 = g.sb("G1bc", [128, 2, D], F32, stack=s1)
        for r in range(2):
            g.dma(G1[:, r, :], C.MOD[l, r:r + 1, 2 * D:3 * D].partition_broadcast(128), R=[C.B_MOD[l]], W=[G1.b[0]])
        T = nm_scratch(C, s1)
        xt = g.sb("xt", [128, 2, D], F32, nslots=2, stack=s1)
        ot = g.sb("ot", [128, 2, D], BF16, nslots=2, stack=s1)
        oT = g.sb("oT", [128, 2, D], BF16, nslots=2, stack=s1)
        tmp = g.sb("wtmp", [128, D], F32, stack=s1)
        for t in range(NT):
            if t < 2 and not need_ctx:
                continue
            sl = t % 2
            r = 1 if t < 2 else 0
            g.dma(xt[:, sl], xsrc[t * 128:(t + 1) * 128, :], R=[C.B_XS[t]] if l > 0 else [], W=[xt.b[sl]])
            g.dma(ot[:, sl], C.O[t * 128:(t + 1) * 128, :], R=[C.B_O[t]], W=[ot.b[sl]])
            p = g.ps()
            pb = p[:].bitcast(BF16)
            for k in range(8):
                g.tr(pb[:, k * 128:(k + 1) * 128], ot[:, sl, k * 128:(k + 1) * 128], C.ident[:],
                     [ot.b[sl], C.ident.b[0]], [p.b[0]])
            g.cp(oT[:, sl], pb, [p.b[0]], [oT.b[sl]], eng="act")
            for nh in range(2):
                p = g.ps()
                for k in range(8):
                    g.mm(p[:, :], oT[:, sl, k * 128:(k + 1) * 128], wout[:, k, nh * 512:(nh + 1) * 512], k == 0, k == 7,
                         [oT.b[sl], wout.b[0]], [p.b[0]])
                g.tt(tmp[:, nh * 512:(nh + 1) * 512], p[:, :], G1[:, r, nh * 512:(nh + 1) * 512], ALU.mult,
                     [p.b[0], G1.b[0]], [tmp.b[0]], eng="dve")
            g.tt(xt[:, sl], xt[:, sl], tmp[:], ALU.add, [xt.b[sl], tmp.b[0]], [xt.b[sl]], eng="pool")
            g.dma(C.XS[t * 128:(t + 1) * 128, :], xt[:, sl], R=[xt.b[sl]], W=[C.B_XS[t]])
            norm_mod_transpose(C, xt[:, sl], xt.b[sl], r, Abc, Sbc, T, sl, C.H2T[t], C.B_H2T[t])


def phase_ffn(C, l, need_ctx, last):
    g, I = C.g, C.I
    moe = (l % 2 == 1)
    j = l // 2
    E = NE if moe else 1
    if moe:
        W1 = lambda e: I["moe_w1"][j, e]
        W3 = lambda e: I["moe_w3"][j, e]
        W2 = lambda e: I["moe_w2"][j, e]
    else:
        W1 = lambda e: I["ffn_w1"][j]
        W3 = lambda e: I["ffn_w3"][j]
        W2 = lambda e: I["ffn_w2"][j]
    tiles = [t for t in range(NT) if t >= 2 or need_ctx]
    groups = []
    if need_ctx:
        groups.append([0, 1])
    for i in range(4):
        groups.append([2 + 8 * i + q for q in range(8)])
    with SS() as s1:
        G2 = g.sb("G2bc", [128, 2, D], F32, stack=s1)
        for r in range(2):
            g.dma(G2[:, r, :], C.MOD[l, r:r + 1, 5 * D:6 * D].partition_broadcast(128), R=[C.B_MOD[l]], W=[G2.b[0]])
        if last:
            fg = g.sb("fgbc", [128, D], F32, stack=s1)
            g.dma(fg[:], I["final_g"].rearrange("(o d) -> o d", o=1).partition_broadcast(128), W=[fg.b[0]])
        if moe:
            wr = g.sb("wrouter", [128, 8, NE], BF16, stack=s1)
            g.dma(wr[:], I["moe_router"][j].rearrange("(k p) e -> p k e", p=128), W=[wr.b[0]], q="pool")
        h2 = g.sb("h2", [128, 1, 8, 1024], BF16, nslots=1, stack=s1)
        aT = g.sb("aT", [128, 22, 1024], BF16, nslots=22, stack=s1)
        Y = g.sb("Yacc", [128, 8, D], F32, nslots=8, stack=s1)
        w13 = g.sb("w13", [128, 3, 2, 8, 256], BF16, nslots=3, stack=s1)
        w2 = g.sb("w2", [128, 2, 11, D], BF16, nslots=2, stack=s1)
        sil = g.sb("sil", [128, 2, 512], F32, nslots=2, stack=s1)
        gates = g.sb("gates", [128, 8, NE], F32, nslots=8, stack=s1)
        rt = g.sb("rt", [128, 8, NE], F32, stack=s1)
        rc = g.sb("rc", [128, 8], F32, stack=s1)
        xt = g.sb("xt", [128, 2, D], F32, nslots=2, stack=s1)
        ft = g.sb("ftmp", [128, D], F32, stack=s1)
        fs = g.sb("fssq", [128, 2], F32, nslots=2, stack=s1)
        w13_it = 0
        w2_it = 0
        for gi_, tl in enumerate(groups):
            hs = 0
            n = 128 * len(tl)
            for q, t in enumerate(tl):
                g.dma(h2[:, hs, :, q * 128:(q + 1) * 128], C.H2T[t].rearrange("p (k c) -> p k c", k=8), R=[C.B_H2T[t]],
                      W=[h2.b[hs]])
            for q, t in enumerate(tl):
                if not moe:
                    g.memset(gates[:, q, :], 1.0, [gates.b[q]])
                    continue
                p = g.ps()
                for k in range(8):
                    g.mm(p[:, 0:NE], h2[:, hs, k, q * 128:(q + 1) * 128], wr[:, k, :], k == 0, k == 7, [h2.b[hs], wr.b[0]],
                         [p.b[0]])
                RT, RC = [rt.b[0]], [rc.b[0]]
                lg, eq1, msk, eq2 = rt[:, 0, :], rt[:, 1, :], rt[:, 2, :], rt[:, 3, :]
                g.cp(lg, p[:, 0:NE], [p.b[0]], RT, eng="dve")
                red = lambda o_, i_: g.K.op("dve", lambda h_: h_.tensor_reduce(out=o_, in_=i_, axis=AX.X, op=ALU.max),
                                            reads=RT + RC, writes=RC)
                red(rc[:, 0:1], lg)
                g.ts(eq1, lg, rc[:, 0:1], ALU.is_equal, RT + RC, RT)
                g.stt(msk, eq1, -1e30, lg, ALU.mult, ALU.add, RT, RT)
                red(rc[:, 1:2], msk)
                g.ts(eq2, msk, rc[:, 1:2], ALU.is_equal, RT + RC, RT)
                g.tt(rc[:, 2:3], rc[:, 1:2], rc[:, 0:1], ALU.subtract, RC, RC)
                g.act(rc[:, 3:4], rc[:, 2:3], AF.Exp, RC, RC)
                g.ts(rc[:, 4:5], rc[:, 3:4], 1.0, ALU.add, RC, RC)
                g.recip(rc[:, 4:5], rc[:, 4:5], RC, RC)
                g.tt(rc[:, 5:6], rc[:, 3:4], rc[:, 4:5], ALU.mult, RC, RC)
                g.ts(eq1, eq1, rc[:, 4:5], ALU.mult, RT + RC, RT)
                g.stt(gates[:, q, :], eq2, rc[:, 5:6], eq1, ALU.mult, ALU.add, RT + RC, [gates.b[q]])
            for e in range(E):
                w1v = W1(e).rearrange("(k p) n -> p k n", p=128)
                w3v = W3(e).rearrange("(k p) n -> p k n", p=128)
                w2v = W2(e).rearrange("(c p) n -> p c n", p=128)
                w2s = [0, 1]
                sit = 0
                for cb in range(11):
                    wsl = w13_it % 3
                    w13_it += 1
                    g.dma(w13[:, wsl, 0], w1v[:, :, cb * 256:(cb + 1) * 256], W=[w13.b[wsl]], q="pool")
                    g.dma(w13[:, wsl, 1], w3v[:, :, cb * 256:(cb + 1) * 256], W=[w13.b[wsl]], q="pool")
                    if cb == 2:
                        for hf_ in range(2):
                            g.dma(w2[:, hf_], w2v[:, hf_ * 11:(hf_ + 1) * 11, :], W=[w2.b[hf_]], q="pool")
                    for c2 in range(2):
                        c = 2 * cb + c2
                        for n0 in range(0, n, 512):
                            nn = min(512, n - n0)
                            ss = sit % 2
                            sit += 1
                            p1, p3 = g.ps(), g.ps()
                            for (pp, wi) in ((p1, 0), (p3, 1)):
                                for k in range(8):
                                    g.mm(pp[:, 0:nn], w13[:, wsl, wi, k, c2 * 128:(c2 + 1) * 128], h2[:, hs, k, n0:n0 + nn], k == 0,
                                         k == 7, [w13.b[wsl], h2.b[hs]], [pp.b[0]])
                            g.act(sil[:, ss, 0:nn], p1[:, 0:nn], AF.Silu, [p1.b[0]], [sil.b[ss]])
                            g.tt(aT[:, c, n0:n0 + nn], p3[:, 0:nn], sil[:, ss, 0:nn], ALU.mult, [p3.b[0], sil.b[ss]], [aT.b[c]],
                                 eng="dve")
                for q, t in enumerate(tl):
                    for nh in range(2):
                        p = g.ps()
                        for c in range(22):
                            g.mm(p[:, :], aT[:, c, q * 128:(q + 1) * 128], w2[:, w2s[c // 11], c % 11, nh * 512:(nh + 1) * 512],
                                 c == 0, c == 21, [aT.b[c], w2.b[w2s[c // 11]]], [p.b[0]])
                        yv = Y[:, q, nh * 512:(nh + 1) * 512]
                        if e == 0:
                            g.ts(yv, p[:, :], gates[:, q, e:e + 1], ALU.mult, [p.b[0], gates.b[q]], [Y.b[q]])
                        else:
                            g.stt(yv, p[:, :], gates[:, q, e:e + 1], yv, ALU.mult, ALU.add, [p.b[0], gates.b[q], Y.b[q]], [Y.b[q]])
            for q, t in enumerate(tl):
                sl = t % 2
                r = 1 if t < 2 else 0
                g.dma(xt[:, sl], C.XS[t * 128:(t + 1) * 128, :], R=[C.B_XS[t]], W=[xt.b[sl]])
                g.tt(Y[:, q, :], Y[:, q, :], G2[:, r, :], ALU.mult, [Y.b[q], G2.b[0]], [Y.b[q]], eng="pool")
                g.tt(xt[:, sl], xt[:, sl], Y[:, q, :], ALU.add, [xt.b[sl], Y.b[q]], [xt.b[sl]], eng="pool")
                if not last:
                    g.dma(C.XS[t * 128:(t + 1) * 128, :], xt[:, sl], R=[xt.b[sl]], W=[C.B_XS[t]])
                else:
                    g.act(ft[:], xt[:, sl], AF.Square, [xt.b[sl]], [ft.b[0], fs.b[sl]], accum_out=fs[:, sl:sl + 1])
                    g.act(fs[:, sl:sl + 1], fs[:, sl:sl + 1], AF.Sqrt, [fs.b[sl]], [fs.b[sl]], bias=C.epsc[:, 0:1], scale=1.0 / D)
                    g.recip(fs[:, sl:sl + 1], fs[:, sl:sl + 1], [fs.b[sl]], [fs.b[sl]])
                    g.stt(xt[:, sl], xt[:, sl], fs[:, sl:sl + 1], fg[:], ALU.mult, ALU.mult, [xt.b[sl], fs.b[sl], fg.b[0]],
                          [xt.b[sl]])
                    C.out_tokens.append(g.dma(C.out[(t - 2) * 128:(t - 1) * 128, :], xt[:, sl], R=[xt.b[sl]], W=[C.B_OUT[t]]))


def phase_gdnzero(C, l):
    g = C.g
    with SS() as s1:
        z = g.sb("gz", [128, 256], BF16, stack=s1)
        g.memset(z[:], 0.0, [z.b[0]])
        for t in range(NT):
            g.dma(C.O[t * 128:(t + 1) * 128, 256:512], z[:], R=[z.b[0]], W=[C.B_O[t]])


def build_program(dbg=None, nlayers=DEPTH, phases=("norm1", "ret", "gdn", "na", "wout", "ffn")):
    nc = bass.Bass("TRN2", target_bir_lowering=False)
    C = Ctx()
    I = C.I = {}

    def inp(name, shape, dt=F32):
        I[name] = nc.dram_tensor(name, list(shape), dt, kind="ExternalInput").ap()

    def scratch(name, shape, dt):
        return nc.dram_tensor(name, list(shape), dt, kind="ExternalOutput" if (dbg and name in dbg.split("+")) else "Internal").ap()

    inp("xin", [NTOK, D])
    inp("cvec", [2, D])
    inp("ada_w", [DEPTH, D, 6 * D])
    inp("ada_b", [DEPTH, 6 * D])
    inp("norm1_g", [DEPTH, D])
    inp("norm2_g", [DEPTH, D])
    inp("w_in", [DEPTH, D, D_IN])
    inp("ret_decay", [DEPTH, 2, 4])
    inp("ret_const", [128, 6, 128])
    inp("ret_pcol", [128, 2, 64])
    inp("rope", [SEQ, 2, 256])
    inp("w_out", [DEPTH, D, D])
    inp("ffn_w1", [1, D, D_FF])
    inp("ffn_w3", [1, D, D_FF])
    inp("ffn_w2", [1, D_FF, D])
    inp("moe_router", [1, D, NE])
    inp("moe_w1", [1, NE, D, D_FF])
    inp("moe_w3", [1, NE, D, D_FF])
    inp("moe_w2", [1, NE, D_FF, D])
    inp("final_g", [D])
    inp("gdn_const", [64, 13, 64])
    inp("gdn_bd", [128, 128])
    inp("gdn_a_log", [DEPTH, 2, 4])
    inp("gdn_dt_bias", [DEPTH, 2, 4])
    inp("gdn_norm_g", [DEPTH, 64])
    inp("gdn_convw", [DEPTH, 4, 64, 3, 5])
    inp("na_mask", [128, 5, 640])
    inp("na_bias", [DEPTH, 8, 128, 5, 640])
    C.out = nc.dram_tensor("out", [SEQ, D], F32, kind="ExternalOutput").ap()
    C.out_tokens = []
    C.B_OUT = [Buf() for _ in range(NT)]
    C.MOD = scratch("MOD", [DEPTH, 2, 6 * D], F32)
    C.HT = scratch("HT", [NT, 128, D], BF16)
    C.XS = scratch("XS", [NTOK, D], F32)
    C.O = scratch("O", [NTOK, D], BF16)
    C.H2T = scratch("H2T", [NT, 128, D], BF16)
    C.B_H2T = [Buf() for _ in range(NT)]
    C.DBG = scratch("DBG", [64, 8192], F32) if (dbg and "DBG" in dbg) else None
    C.B_DBG = Buf()
    C.B_MOD = [Buf() for _ in range(DEPTH)]
    C.B_HT = [Buf() for _ in range(NT)]
    C.B_XS = [Buf() for _ in range(NT)]
    C.B_O = [Buf() for _ in range(NT)]

    with ExitStack() as st:
        g = C.g = Gen(nc, st)
        K = g.K
        _CUR["K"] = K
        ident = C.ident = g.sb("ident", [128, 128], BF16)
        g.memset(ident[:], 1.0, [ident.b[0]])
        K.op("pool", lambda h: h.affine_select(out=ident[:], in_=ident[:], pattern=[[-1, 128]],
                                                compare_op=ALU.is_equal, fill=0.0, base=0, channel_multiplier=1),
             reads=[ident.b[0]], writes=[ident.b[0]])
        C.epsc = g.sb("epsc", [128, 1], F32)
        g.memset(C.epsc[:], EPS, [C.epsc.b[0]])
        C.onec = g.sb("onec", [128, 1], F32)
        g.memset(C.onec[:], 1.0, [C.onec.b[0]])

        phase_adaln(C, nlayers)
        for l in range(nlayers):
            need_ctx = l < DEPTH - 1
            if "norm1" in phases:
                phase_norm1(C, l)
            if "ret" in phases:
                phase_ret(C, l, need_ctx)
            if "gdn" in phases:
                phase_gdn(C, l, need_ctx)
            if "na" in phases:
                phase_na(C, l, need_ctx)
            if "gdnzero" in phases:
                phase_gdnzero(C, l)
            if "wout" in phases:
                phase_wout(C, l, need_ctx)
            if "ffn" in phases:
                phase_ffn(C, l, need_ctx, l == nlayers - 1 and nlayers == DEPTH)

        fin = [b.last_w for b in C.B_HT + C.B_O + C.B_XS + C.B_H2T + [C.B_DBG] if b.last_w is not None] + C.out_tokens
        K.finish(fin)
        K.emit()
    return nc


_NC_CACHE = {}
NA_M_REP = [0, 1, 2, 30, 31]


def na_cls(m):
    return {0: 0, 1: 1, 30: 3, 31: 4}.get(m, 2)


def na_kp0(m):
    return min(max(m - 2, 0), 27)


def _na_index_tables():
    kk = np.arange(128)[:, None, None]
    j = np.arange(5)[None, :, None]
    qq = np.arange(128)[None, None, :]
    ki, kc = kk // 64, kk % 64
    qi, qc = qq // 64, qq % 64
    ridx = np.zeros((5, 128, 5, 128), np.int64)
    cidx = np.zeros((5, 128, 5, 128), np.int64)
    valid = np.zeros((5, 128, 5, 128), bool)
    for c, m in enumerate(NA_M_REP):
        kr = 2 * (na_kp0(m) + j) + ki
        r = 2 * m + qi
        r0 = np.clip(r - 4, 0, 56)
        ws = np.clip(qc - 8, 0, 48)
        v = (kr >= r0) & (kr < r0 + 8) & (kc >= ws) & (kc < ws + 16)
        valid[c] = np.broadcast_to(v, (128, 5, 128))
        ridx[c] = np.broadcast_to(np.clip(kr - r + 7, 0, 14), (128, 5, 128))
        cidx[c] = np.broadcast_to(np.clip(kc - qc + 15, 0, 30), (128, 5, 128))
    return ridx, cidx, valid


def _const_inputs():
    if "consts" in _NC_CACHE:
        return _NC_CACHE["consts"]
    ridx, cidx, valid = _na_index_tables()
    ii = np.arange(128)[None, :].astype(np.float64)
    jj = np.arange(128)[:, None].astype(np.float64)
    ret_const = np.stack([np.maximum(ii - jj, 0) + 0 * jj, np.maximum(jj - ii, 0) + 0 * ii, (ii >= jj) * 1.0, (jj >= ii) * 1.0,
                          (ii + 1) + 0 * jj, (128 - ii) + 0 * jj], axis=1).astype(np.float32)
    ret_pcol = np.stack([np.broadcast_to(127 - jj, (128, 64)), np.broadcast_to(jj, (128, 64))], axis=1).astype(np.float32)
    tpos = np.arange(SEQ)
    inv_freq = 10000.0 ** (-np.arange(16, dtype=np.float32) / 16)
    ang = np.concatenate([(tpos // 64).astype(np.float32)[:, None] * inv_freq, (tpos % 64).astype(np.float32)[:, None] * inv_freq],
                         axis=-1).astype(np.float32)
    rope = np.stack([np.tile(np.cos(ang), (1, 8)), np.tile(np.sin(ang), (1, 8))], axis=1).astype(np.float32)
    mm_, i_ = np.arange(64)[:, None], np.arange(64)[None, :]
    Yf, Yb, SUf, SUb = (mm_ <= i_) * 1.0, (mm_ >= i_) * 1.0, (mm_ > i_) * 1.0, (mm_ < i_) * 1.0
    gdn_const = np.stack([Yf, Yb, Yf, Yb, SUf, SUb, SUf, SUb] + [np.eye(64)] * 4 + [np.ones((64, 64))], axis=1).astype(np.float32)
    pp_ = np.arange(128)
    gdn_bd = ((pp_[:, None] // 64) == (pp_[None, :] // 64)).astype(np.float32)
    c = {"gdn_const": np.ascontiguousarray(gdn_const), "gdn_bd": gdn_bd, "ret_const": np.ascontiguousarray(ret_const), "ret_pcol": np.ascontiguousarray(ret_pcol), "rope": rope,
         "na_mask": np.where(valid, 0.0, -1e30).astype(np.float32).transpose(1, 0, 2, 3).reshape(128, 5, 640).copy(),
         "_na_ridx": ridx, "_na_cidx": cidx}
    _NC_CACHE["consts"] = c
    return c


def _prep_core_inputs(b, inputs):
    xin = np.ascontiguousarray(np.concatenate([inputs["ctx"][b], inputs["x"][b]], axis=0))
    cvec = np.ascontiguousarray(np.stack([inputs["c"][b], inputs["c_ctx"]], axis=0))
    m = {"xin": xin, "cvec": cvec}
    for k in ("ada_w", "ada_b", "norm1_g", "norm2_g", "w_in", "w_out", "ffn_w1", "ffn_w3", "ffn_w2", "moe_router", "moe_w1",
              "moe_w3", "moe_w2", "final_g"):
        m[k] = np.ascontiguousarray(inputs[k])
    c = _const_inputs()
    for k in ("gdn_a_log", "gdn_dt_bias", "gdn_norm_g"):
        m[k] = np.ascontiguousarray(inputs[k])
    m["gdn_convw"] = np.ascontiguousarray(inputs["conv_w"].reshape(DEPTH, 5, 3, 4, 64).transpose(0, 3, 4, 2, 1))
    for k in ("na_mask", "ret_const", "ret_pcol", "rope", "gdn_const", "gdn_bd"):
        m[k] = c[k]
    m["ret_decay"] = np.ascontiguousarray(inputs["ret_decay"])
    rp = inputs["na_rpb"]
    gat = rp[:, :, c["_na_ridx"], c["_na_cidx"]]
    m["na_bias"] = np.ascontiguousarray(gat.transpose(0, 1, 3, 2, 4, 5).reshape(DEPTH, 8, 128, 5, 640))
    return m


def kernel(**inputs):
    inputs = {k: np.asarray(v) for k, v in inputs.items()}
    if "nc" not in _NC_CACHE:
        _NC_CACHE["nc"] = build_program()
    nc = _NC_CACHE["nc"]
    in_maps = [_prep_core_inputs(b, inputs) for b in range(8)]
    res = run_bass_kernel_spmd(nc, in_maps, core_ids=list(range(8)))
    return np.stack([r["out"] for r in res.results], axis=0)
```
